# Optimizing a Trainium2 kernel written in Bass

```python
import jax, jax.numpy as jnp
from jax import lax
import numpy as np


D_MODEL = 1024
BATCH = 8
SEQ = 2048
DEPTH = 2

HEAD_DIM = 64
N_HEADS = D_MODEL // HEAD_DIM
N_MIXERS = 2
N_RWKV = (DEPTH + 1) // 2
N_FOX = DEPTH // 2
RWKV_DECAY_LORA = 64
RWKV_AAA_LORA = 64
RWKV_GATE_LORA = 128
GN_EPS = 64e-5
Q_BLOCK = 128
N_EXPERTS = 16
N_GROUPS = 4
EXPERTS_PER_GROUP = N_EXPERTS // N_GROUPS
TOP_K = 2
D_EXPERT = 512
EXPERT_BLOCK = 128
LN_EPS = 1e-5
ALPHA = (2 * DEPTH) ** 0.25
BETA = (8 * DEPTH) ** -0.25

kernel_name = "hybrid_rwkv7_fox_sharedrouter_moe_deepnorm"


def layer_norm(x, g, b):
    xf = x.astype(jnp.float32)
    mu = jnp.mean(xf, -1, keepdims=True)
    var = jnp.mean(jnp.square(xf - mu), -1, keepdims=True)
    return ((xf - mu) * lax.rsqrt(var + LN_EPS)).astype(x.dtype) * g + b


def rwkv7_step(S, inp):
    r_t, w_t, k_t, v_t, kk_t, a_t = inp
    sa = jnp.einsum('bhij,bhj->bhi', S, kk_t)
    S = (S * w_t[:, :, None, :]
         - sa[..., :, None] * (kk_t * a_t)[..., None, :]
         + v_t[..., :, None] * k_t[..., None, :])
    y = jnp.einsum('bhij,bhj->bhi', S, r_t)
    return S, y


def rwkv7_time_mix(x, mix, wr, wk, wv, wo, w0, w1, w2, a0, a1, a2, g1, g2,
                   k_k, k_a, r_k, gn_g, gn_b):
    B, T, D = x.shape
    H, N = N_HEADS, HEAD_DIM
    xx = jnp.pad(x, ((0, 0), (1, 0), (0, 0)))[:, :T] - x
    xr, xw, xk, xv, xa, xg = (x + xx * mix[j] for j in range(6))
    r = xr @ wr
    k = xk @ wk
    v = xv @ wv
    w = -jax.nn.softplus(-(w0 + jnp.tanh(xw @ w1) @ w2)) - 0.5
    decay = jnp.exp(-jnp.exp(w.astype(jnp.float32))).astype(x.dtype)
    a = jax.nn.sigmoid(a0 + (xa @ a1) @ a2)
    g = jax.nn.sigmoid(xg @ g1) @ g2
    kk = (k * k_k).reshape(B, T, H, N)
    kk_norm = jnp.sqrt(jnp.sum(jnp.square(kk.astype(jnp.float32)), -1, keepdims=True))
    kk = kk / jnp.maximum(kk_norm, 1e-12).astype(x.dtype)
    k = k * (1 + (a - 1) * k_a)
    heads = lambda t: t.reshape(B, T, H, N)
    seq_first = lambda t: jnp.swapaxes(t, 0, 1)
    r_h, k_h, v_h = heads(r), heads(k), heads(v)
    S0 = jnp.zeros((B, H, N, N), x.dtype)
    scan_in = tuple(seq_first(t) for t in (r_h, heads(decay), k_h, v_h, kk, heads(a)))
    _, y = lax.scan(rwkv7_step, S0, scan_in)
    y = jnp.swapaxes(y, 0, 1)
    yf = y.astype(jnp.float32)
    mu = jnp.mean(yf, -1, keepdims=True)
    var = jnp.mean(jnp.square(yf - mu), -1, keepdims=True)
    yn = ((yf - mu) * lax.rsqrt(var + GN_EPS)).astype(x.dtype).reshape(B, T, D) * gn_g + gn_b
    bonus = jnp.sum(r_h * k_h * r_k, -1, keepdims=True) * v_h
    y = yn + bonus.reshape(B, T, D)
    return (y * g) @ wo


def forgetting_attention(x, w_in, b_f, wo):
    B, T, D = x.shape
    H, N = N_HEADS, HEAD_DIM
    proj = x @ w_in
    q, k, v = proj[..., :D], proj[..., D:2 * D], proj[..., 2 * D:3 * D]
    log_f = jax.nn.log_sigmoid((proj[..., 3 * D:] + b_f).astype(jnp.float32))
    c = jnp.swapaxes(jnp.cumsum(log_f, axis=1), 1, 2)
    to_heads = lambda t: t.reshape(B, T, H, N).transpose(0, 2, 1, 3)
    q, k, v = to_heads(q), to_heads(k), to_heads(v)
    nb = T // Q_BLOCK
    qb = q.reshape(B, H, nb, Q_BLOCK, N).transpose(2, 0, 1, 3, 4)
    cb = c.reshape(B, H, nb, Q_BLOCK).transpose(2, 0, 1, 3)
    kpos = jnp.arange(T)
    scale = HEAD_DIM ** -0.5

    def attend(args):
        q_blk, c_blk, i = args
        qpos = i * Q_BLOCK + jnp.arange(Q_BLOCK)
        s = jnp.einsum('bhqd,bhkd->bhqk', q_blk, k).astype(jnp.float32) * scale
        s = s + c_blk[..., :, None] - c[:, :, None, :]
        s = jnp.where(kpos[None, :] <= qpos[:, None], s, -jnp.inf)
        p = jax.nn.softmax(s, axis=-1).astype(v.dtype)
        return jnp.einsum('bhqk,bhkd->bhqd', p, v)

    o = lax.map(attend, (qb, cb, jnp.arange(nb)))
    o = o.transpose(1, 0, 3, 2, 4).reshape(B, T, D)
    return o @ wo


def shared_router_moe(h, router_w, router_bias, w_gate, w_up, w_down):
    B, T, D = h.shape
    n_tok = B * T
    xt = h.reshape(n_tok, D)
    s = jax.nn.sigmoid((xt @ router_w).astype(jnp.float32))
    s_sel = (s + router_bias.astype(jnp.float32)).reshape(n_tok, N_GROUPS, EXPERTS_PER_GROUP)
    grp_score = jnp.sum(lax.top_k(s_sel, 2)[0], -1)
    g_star = jnp.argmax(grp_score, -1)
    in_grp = jnp.take_along_axis(s_sel, g_star[:, None, None], axis=1)[:, 0]
    _, loc = lax.top_k(in_grp, TOP_K)
    e_idx = (g_star[:, None] * EXPERTS_PER_GROUP + loc).astype(jnp.int32)
    gate = jnp.take_along_axis(s, e_idx, axis=1)
    gate = (gate / jnp.sum(gate, -1, keepdims=True)).astype(h.dtype)

    n_assign = n_tok * TOP_K
    e_flat = e_idx.reshape(n_assign)
    tok = jnp.arange(n_assign, dtype=jnp.int32) // TOP_K
    order = jnp.argsort(e_flat)
    e_sorted = e_flat[order]
    counts = jnp.zeros((N_EXPERTS,), jnp.int32).at[e_flat].add(1)
    padded = (counts + EXPERT_BLOCK - 1) // EXPERT_BLOCK * EXPERT_BLOCK
    start = jnp.cumsum(counts) - counts
    pend = jnp.cumsum(padded)
    pstart = pend - padded
    dest = pstart[e_sorted] + jnp.arange(n_assign, dtype=jnp.int32) - start[e_sorted]
    n_blocks = (n_assign + N_EXPERTS * (EXPERT_BLOCK - 1) + EXPERT_BLOCK - 1) // EXPERT_BLOCK
    n_rows = n_blocks * EXPERT_BLOCK
    row_tok = jnp.full((n_rows,), n_tok, jnp.int32).at[dest].set(tok[order])
    row_gate = jnp.zeros((n_rows,), h.dtype).at[dest].set(gate.reshape(n_assign)[order])
    block_exp = jnp.minimum(
        jnp.searchsorted(pend, jnp.arange(n_blocks, dtype=jnp.int32) * EXPERT_BLOCK, side='right'),
        N_EXPERTS - 1)
    x_pad = jnp.concatenate([xt, jnp.zeros((1, D), h.dtype)], axis=0)
    xs = x_pad[row_tok].reshape(n_blocks, EXPERT_BLOCK, D)

    def expert_block(args):
        xb, e = args
        hid = jax.nn.silu(xb @ w_gate[e]) * (xb @ w_up[e])
        return hid @ w_down[e]

    ys = lax.map(expert_block, (xs, block_exp)).reshape(n_rows, D)
    out = jax.ops.segment_sum(ys * row_gate[:, None], row_tok, num_segments=n_tok + 1)[:n_tok]
    return out.reshape(B, T, D)


def setup_inputs(seed: int = 0) -> dict:
    key = jax.random.key(seed)
    ks = jax.random.split(key, 32)
    D, H, N, L_A, L_B = D_MODEL, N_HEADS, HEAD_DIM, N_RWKV, N_FOX
    nrm = lambda i, shape, sc: jax.random.normal(ks[i], shape, jnp.float32) * sc
    unif = lambda i, shape: jax.random.uniform(ks[i], shape, jnp.float32)
    inv = D ** -0.5
    v_cols = jnp.concatenate([jnp.ones((2 * D,)), jnp.full((D,), BETA), jnp.ones((H,))]).astype(jnp.float32)
    return {
        "x": nrm(0, (BATCH, SEQ, D), 1.0),
        "rw_mix": unif(1, (L_A, 6, D)),
        "rw_wr": nrm(2, (L_A, D, D), inv),
        "rw_wk": nrm(3, (L_A, D, D), inv),
        "rw_wv": nrm(4, (L_A, D, D), inv * BETA),
        "rw_wo": nrm(5, (L_A, D, D), inv * BETA),
        "rw_w0": -6.0 + 5.0 * unif(6, (L_A, D)),
        "rw_w1": nrm(7, (L_A, D, RWKV_DECAY_LORA), inv),
        "rw_w2": nrm(8, (L_A, RWKV_DECAY_LORA, D), 0.1 * RWKV_DECAY_LORA ** -0.5),
        "rw_a0": nrm(9, (L_A, D), 0.1),
        "rw_a1": nrm(10, (L_A, D, RWKV_AAA_LORA), inv),
        "rw_a2": nrm(11, (L_A, RWKV_AAA_LORA, D), 0.1 * RWKV_AAA_LORA ** -0.5),
        "rw_g1": nrm(12, (L_A, D, RWKV_GATE_LORA), inv),
        "rw_g2": nrm(13, (L_A, RWKV_GATE_LORA, D), RWKV_GATE_LORA ** -0.5),
        "rw_kk": 0.85 + nrm(14, (L_A, D), 0.05),
        "rw_ka": 1.0 + nrm(15, (L_A, D), 0.05),
        "rw_rk": nrm(16, (L_A, H, N), 0.1),
        "rw_gn_g": 1.0 + nrm(17, (L_A, D), 0.02),
        "rw_gn_b": nrm(18, (L_A, D), 0.02),
        "fx_w_in": nrm(19, (L_B, D, 3 * D + H), inv) * v_cols,
        "fx_b_f": 3.0 + nrm(20, (L_B, H), 0.5),
        "fx_wo": nrm(21, (L_B, D, D), inv * BETA),
        "router_w": nrm(22, (D, N_EXPERTS), inv),
        "router_bias": nrm(23, (N_EXPERTS,), 0.01),
        "moe_w_gate": nrm(24, (DEPTH, N_EXPERTS, D, D_EXPERT), inv * BETA),
        "moe_w_up": nrm(25, (DEPTH, N_EXPERTS, D, D_EXPERT), inv * BETA),
        "moe_w_down": nrm(26, (DEPTH, N_EXPERTS, D_EXPERT, D), D_EXPERT ** -0.5 * BETA),
        "ln_g": 1.0 + nrm(27, (DEPTH, 2, D), 0.02),
        "ln_b": nrm(28, (DEPTH, 2, D), 0.02),
    }


def reference(x, rw_mix, rw_wr, rw_wk, rw_wv, rw_wo, rw_w0, rw_w1, rw_w2, rw_a0, rw_a1,
              rw_a2, rw_g1, rw_g2, rw_kk, rw_ka, rw_rk, rw_gn_g, rw_gn_b,
              fx_w_in, fx_b_f, fx_wo, router_w, router_bias,
              moe_w_gate, moe_w_up, moe_w_down, ln_g, ln_b):
    h = x
    for i in range(DEPTH):
        j = i // N_MIXERS
        if i % N_MIXERS == 0:
            mixed = rwkv7_time_mix(h, rw_mix[j], rw_wr[j], rw_wk[j], rw_wv[j], rw_wo[j],
                                   rw_w0[j], rw_w1[j], rw_w2[j], rw_a0[j], rw_a1[j], rw_a2[j],
                                   rw_g1[j], rw_g2[j], rw_kk[j], rw_ka[j], rw_rk[j],
                                   rw_gn_g[j], rw_gn_b[j])
        else:
            mixed = forgetting_attention(h, fx_w_in[j], fx_b_f[j], fx_wo[j])
        h = layer_norm(ALPHA * h + mixed, ln_g[i, 0], ln_b[i, 0])
        ffn = shared_router_moe(h, router_w, router_bias, moe_w_gate[i], moe_w_up[i], moe_w_down[i])
        h = layer_norm(ALPHA * h + ffn, ln_g[i, 1], ln_b[i, 1])
    return h
```

```python
import numpy as np
from contextlib import ExitStack
import concourse.bass as bass
import concourse.mybir as mybir
from concourse.bass_utils import run_bass_kernel_spmd

F32 = mybir.dt.float32
BF16 = mybir.dt.bfloat16
AF = mybir.ActivationFunctionType
ALU = mybir.AluOpType
AX = mybir.AxisListType


STRICT = False


class Ins:
    __slots__ = ("q", "fn", "deps", "sem", "val", "dkey", "need", "n")

    def __init__(self, q, fn, dkey):
        self.q = q
        self.fn = fn
        self.dkey = dkey
        self.deps = {}
        self.need = False
        self.sem = None
        self.val = 0


class Prog:
    ENG = ("pe", "act", "dve", "pool", "sp")

    def __init__(self, nc):
        self.nc = nc
        self.begin(None)
        self.pool_es = ExitStack()
        self.esems = None
        self.dpool = {}
        self.base = {}

    def begin(self, es):
        self.es = es
        self.streams = {e: [] for e in self.ENG}
        self.lastw = {}
        self.readers = {}
        self.n = 0

    _uid = [0]

    def sb(self, name, shape, dt):
        Prog._uid[0] += 1
        return self.es.enter_context(self.nc.sbuf_tensor("%s_%d" % (name, Prog._uid[0]), list(shape), dt))

    def ps(self, name, shape, dt=F32):
        Prog._uid[0] += 1
        return self.es.enter_context(self.nc.psum_tensor("%s_%d" % (name, Prog._uid[0]), list(shape), dt))

    def capture(self):
        self._cap = []

    def end_capture(self):
        lst, self._cap = self._cap, None
        return lst

    def replay_merged(self, *lists):
        pos = [0] * len(lists)
        total = sum(len(l) for l in lists)
        for _ in range(total):
            best, bf = None, 2.0
            for k, l in enumerate(lists):
                if pos[k] < len(l):
                    f = pos[k] / len(l)
                    if f < bf:
                        best, bf = k, f
            a = lists[best][pos[best]]
            pos[best] += 1
            self.op(*a[0], **a[1])

    def op(self, q, fn, r=(), w=(), dma=None):
        if getattr(self, "_cap", None) is not None:
            self._cap.append(((q, fn), dict(r=list(r), w=list(w), dma=dma)))
            return None
        ins = Ins(q, fn, dma)
        ins.n = self.n
        self.n += 1
        for k in r:
            d = self.lastw.get(k)
            if d is not None:
                ins.deps[d] = "raw"
        for k in w:
            d = self.lastw.get(k)
            if d is not None and d not in ins.deps:
                ins.deps[d] = "waw"
            for d in self.readers.get(k, ()):
                if d not in ins.deps and d is not ins:
                    ins.deps[d] = "war"
        for k in w:
            self.lastw[k] = ins
            self.readers[k] = []
        for k in r:
            self.readers.setdefault(k, []).append(ins)
        self.streams[q].append(ins)
        return ins

    @staticmethod
    def _skip(i, d, kind):
        if d.dkey is not None:
            return False
        if i.dkey is None and d.q == i.q:
            if i.q == "pe":
                return True
            return False if STRICT else kind != "raw"
        return False


    def mm(self, out, lhsT, rhs, start=True, stop=True, r=(), w=()):
        return self.op("pe", lambda e: e.matmul(out, lhsT, rhs, start=start, stop=stop), r=r, w=w)

    def tr(self, out, in_, ident, r=(), w=()):
        return self.op("pe", lambda e: e.transpose(out, in_, ident), r=r, w=w)

    def act(self, out, in_, func, r=(), w=(), bias=None, scale=None):
        kw = {}
        if bias is not None:
            kw["bias"] = bias
        if scale is not None:
            kw["scale"] = scale
        return self.op("act", lambda e: e.activation(out, in_, func, **kw), r=r, w=w)

    def tt(self, q, out, in0, in1, op, r=(), w=()):
        return self.op(q, lambda e: e.tensor_tensor(out, in0, in1, op), r=r, w=w)

    def ts(self, q, out, in0, s1, s2, op0, op1=None, r=(), w=()):
        if op1 is None:
            return self.op(q, lambda e: e.tensor_scalar(out, in0, s1, None, op0), r=r, w=w)
        return self.op(q, lambda e: e.tensor_scalar(out, in0, s1, s2, op0, op1), r=r, w=w)

    def stt(self, q, out, in0, scalar, in1, op0, op1, r=(), w=()):
        return self.op(q, lambda e: e.scalar_tensor_tensor(out, in0, scalar, in1, op0, op1), r=r, w=w)

    def cp(self, q, out, in_, r=(), w=()):
        if q == "act":
            return self.op(q, lambda e: e.copy(out, in_), r=r, w=w)
        return self.op(q, lambda e: e.tensor_copy(out, in_), r=r, w=w)

    def red(self, out, in_, op=None, r=(), w=()):
        op = op or ALU.add
        return self.op("dve", lambda e: e.tensor_reduce(out, in_, AX.X, op), r=r, w=w)

    def rcp(self, out, in_, r=(), w=()):
        return self.op("dve", lambda e: e.reciprocal(out, in_), r=r, w=w)

    def dma(self, q, out, in_, dkey, r=(), w=(), slow=False):
        if slow:
            return self.op(q, lambda e: e.dma_start(out=out, in_=in_, allow_slow_non_contiguous=True), r=r, w=w, dma=dkey)
        return self.op(q, lambda e: e.dma_start(out=out, in_=in_), r=r, w=w, dma=dkey)

    def fence(self, keys):
        return self.op("sp", None, r=keys)

    def sem(self, name):
        Prog._uid[0] += 1
        return self.es.enter_context(self.nc.semaphore("%s_%d" % (name, Prog._uid[0])))

    def emit(self):
        nc = self.nc
        es = self.es
        for q in self.ENG:
            for i in self.streams[q]:
                for d, kind in i.deps.items():
                    if not self._skip(i, d, kind):
                        d.need = True
        if self.esems is None:
            self.esems = {}
            for q in ("pe", "act", "dve", "pool"):
                Prog._uid[0] += 1
                self.esems[q] = self.pool_es.enter_context(nc.semaphore("s_%s_%d" % (q, Prog._uid[0])))
                self.base[id(self.esems[q])] = 0
        sems = self.esems
        dsem = {}
        dcnt = {}
        nused = {}
        for q in self.ENG:
            cnt = self.base[id(sems[q])] if q in sems else 0
            for i in self.streams[q]:
                if i.dkey is not None:
                    if i.dkey not in dsem:
                        dp = self.dpool.setdefault(q, [])
                        used = nused.setdefault(q, 0)
                        nused[q] = used + 1
                        if used >= len(dp):
                            Prog._uid[0] += 1
                            sm_ = self.pool_es.enter_context(nc.semaphore("d%s_%d" % (q, Prog._uid[0])))
                            dp.append(sm_)
                            self.base[id(sm_)] = 0
                        dsem[i.dkey] = dp[used]
                        dcnt[i.dkey] = self.base[id(dp[used])]
                    dcnt[i.dkey] += 16
                    i.sem = dsem[i.dkey]
                    i.val = dcnt[i.dkey]
                elif i.need:
                    cnt += 1
                    i.sem = sems[q]
                    i.val = cnt
            if q in sems:
                self.base[id(sems[q])] = cnt
        for k_, sm_ in dsem.items():
            self.base[id(sm_)] = dcnt[k_]
        block = es.enter_context(nc.Block())

        def run(q, eng):
            waited = {}
            for i in self.streams[q]:
                for d, kind in i.deps.items():
                    if self._skip(i, d, kind):
                        continue
                    sid = id(d.sem)
                    if waited.get(sid, 0) >= d.val:
                        continue
                    eng.wait_ge(d.sem, d.val)
                    waited[sid] = d.val
                if i.fn is None:
                    continue
                if i.dkey is not None and i.val - 16 > 0:
                    sid = id(i.sem)
                    if waited.get(sid, 0) < i.val - 16:
                        eng.wait_ge(i.sem, i.val - 16)
                        waited[sid] = i.val - 16
                bi = i.fn(eng)
                if i.dkey is not None:
                    bi.then_inc(i.sem, 16)
                elif i.need:
                    bi.then_inc(i.sem, 1)

        @block.tensor
        def _(e):
            run("pe", e)

        @block.scalar
        def _(e):
            run("act", e)

        @block.vector
        def _(e):
            run("dve", e)

        @block.gpsimd
        def _(e):
            run("pool", e)

        @block.sync
        def _(e):
            run("sp", e)


T = 2048
D = 1024
NT = 16
ALPHA = 4.0 ** 0.25
LN_EPS = 1e-5
GN_EPS = 64e-5


def K(name, *idx):
    return (name,) + idx


def consts(p):
    c = {}
    ident = p.sb("ident", [128, 128], F32)
    identb = p.sb("identb", [128, 128], BF16)
    p.op("pool", lambda e: e.memset(ident[:], 0.0), w=["ident"])
    p.op("pool", lambda e: e.affine_select(out=ident[:], in_=ident[:], pattern=[[-1, 128]],
                                           compare_op=ALU.not_equal, fill=1.0, base=0, channel_multiplier=1),
         r=["ident"], w=["ident"])
    p.op("pool", lambda e: e.tensor_copy(identb[:], ident[:]), r=["ident"], w=["identb"])
    c["ident"] = ident
    c["identb"] = identb
    return c


def layer_norm_tiles(p, pre, acc, g_d, b_d, out_d, tmp):
    nc = p.nc
    gbc = p.sb(pre + "gbc", [128, D], F32)
    bbc = p.sb(pre + "bbc", [128, D], F32)
    p.op("sp", lambda e: e.dma_start(out=gbc[:], in_=g_d.partition_broadcast(128)), w=[K(pre, "gbc")], dma=pre + "gbc")
    p.op("sp", lambda e: e.dma_start(out=bbc[:], in_=b_d.partition_broadcast(128)), w=[K(pre, "bbc")], dma=pre + "bbc")
    s1 = p.sb(pre + "s1", [128, NT], F32)
    s2 = p.sb(pre + "s2", [128, NT], F32)
    st = p.sb(pre + "st", [128, 4, NT], F32)
    for i in range(NT):
        p.op("dve", (lambda i: lambda e: e.reduce_sum(s1[:, i:i + 1], acc[:, i, :], axis=AX.X))(i),
             r=[K("acc", i)], w=[K(pre, "s1", i)])
        sl = tmp[i % 2]
        p.op("act", (lambda i, sl: lambda e: e.activation(sl[:], acc[:, i, :], AF.Square))(i, sl),
             r=[K("acc", i)], w=[K(pre, "sq", i % 2)])
        p.op("dve", (lambda i, sl: lambda e: e.reduce_sum(s2[:, i:i + 1], sl[:], axis=AX.X))(i, sl),
             r=[K(pre, "sq", i % 2)], w=[K(pre, "s2", i)])
    allk1 = [K(pre, "s1", i) for i in range(NT)]
    allk2 = [K(pre, "s2", i) for i in range(NT)]
    mean, m2, var, rstd = st[:, 0, :], st[:, 1, :], st[:, 2, :], st[:, 3, :]
    p.op("dve", lambda e: e.tensor_scalar(mean, s1[:], 1.0 / D, None, ALU.mult), r=allk1, w=[K(pre, "mean")])
    p.op("dve", lambda e: e.tensor_tensor(m2, mean, mean, ALU.mult), r=[K(pre, "mean")], w=[K(pre, "m2")])
    p.op("dve", lambda e: e.scalar_tensor_tensor(var, s2[:], 1.0 / D, m2, ALU.mult, ALU.subtract),
         r=allk2 + [K(pre, "m2")], w=[K(pre, "var")])
    p.op("dve", lambda e: e.tensor_scalar(var, var, LN_EPS, None, ALU.add), r=[K(pre, "var")], w=[K(pre, "var")])
    p.op("act", lambda e: e.activation(var, var, AF.Sqrt), r=[K(pre, "var")], w=[K(pre, "var")])
    p.op("dve", lambda e: e.reciprocal(rstd, var), r=[K(pre, "var")], w=[K(pre, "rstd")])
    p.op("dve", lambda e: e.scalar_tensor_tensor(m2, mean, -1.0, rstd, ALU.mult, ALU.mult),
         r=[K(pre, "mean"), K(pre, "rstd")], w=[K(pre, "m2")])
    for i in range(NT):
        p.op("act", (lambda i: lambda e: e.activation(acc[:, i, :], acc[:, i, :], AF.Identity,
                                                      bias=m2[:, i:i + 1], scale=rstd[:, i:i + 1]))(i),
             r=[K("acc", i), K(pre, "m2"), K(pre, "rstd")], w=[K("acc", i)])
        p.op("dve", (lambda i: lambda e: e.tensor_tensor(acc[:, i, :], acc[:, i, :], gbc[:], ALU.mult))(i),
             r=[K("acc", i), K(pre, "gbc")], w=[K("acc", i)])
        p.op("dve" if i % 2 == 0 else "pool", (lambda i: lambda e: e.tensor_tensor(acc[:, i, :], acc[:, i, :], bbc[:], ALU.add))(i),
             r=[K("acc", i), K(pre, "bbc")], w=[K("acc", i)])
        p.op("sp", (lambda i: lambda e: e.dma_start(out=out_d[i * 128:(i + 1) * 128, :], in_=acc[:, i, :]))(i),
             r=[K("acc", i)], w=[K(pre, "out", i)], dma=pre + "st%d" % (i % 4))
    return [K(pre, "out", i) for i in range(NT)]


def stage_moe(p, pre, h_in, h_out, rw_d, rb_d, wg_d, wu_d, wd_d, lng_d, lnb_d, acc=None):
    es = ExitStack()
    with es:
        p.begin(es)
        c = consts(p)
        preloaded = acc is not None
        if not preloaded:
            acc = p.sb(pre + "acc", [128, NT, D], F32)
        hFM = p.sb(pre + "hFM", [128, 8, T], BF16)
        hF32 = [p.sb(pre + "hF32_%d" % j, [128, 8, 128], F32) for j in range(2)]
        rw = p.sb(pre + "rw", [128, 8, 16], F32)
        rbb = p.sb(pre + "rbb", [128, 16], F32)
        lg = p.sb(pre + "lg", [128, NT, 16], F32)
        tmp = [p.sb(pre + "tmp%d" % j, [128, D], F32) for j in range(2)]
        ps_tr = [p.ps(pre + "ps_tr%d" % j, [128, 512]) for j in range(2)]
        ps_g = [p.ps(pre + "ps_g%d" % j, [128, 512]) for j in range(2)]
        ps_u = [p.ps(pre + "ps_u%d" % j, [128, 512]) for j in range(2)]
        ps_d = [p.ps(pre + "ps_d%d" % j, [128, 512]) for j in range(2)]
        wg = [p.sb(pre + "wg%d" % j, [128, 8, 512], BF16) for j in range(2)]
        wu = [p.sb(pre + "wu%d" % j, [128, 8, 512], BF16) for j in range(2)]
        wd = [p.sb(pre + "wd%d" % j, [128, 4, 1024], BF16) for j in range(2)]
        hid = [p.sb(pre + "hid%d" % j, [128, 4, 512], BF16) for j in range(2)]
        sg = [p.sb(pre + "sg%d" % j, [128, 512], F32) for j in range(2)]

        for i in range(NT if not preloaded else 0):
            p.op("sp", (lambda i: lambda e: e.dma_start(out=acc[:, i, :], in_=h_in[i * 128:(i + 1) * 128, :]))(i),
                 w=[K("acc", i)], dma=pre + "ld%d" % (i % 4))
        p.op("sp", lambda e: e.dma_start(out=rw[:], in_=rw_d.rearrange("(c p) e -> p c e", p=128)), w=[K(pre, "rw")], dma=pre + "rw")
        p.op("sp", lambda e: e.dma_start(out=rbb[:], in_=rb_d.partition_broadcast(128)), w=[K(pre, "rbb")], dma=pre + "rbb")

        def load_w(e_, after=()):
            s = e_ % 2
            p.op("pool", lambda e: e.dma_start(out=wg[s][:], in_=wg_d[e_].rearrange("(c p) f -> p c f", p=128)),
                 r=list(after), w=[K(pre, "wg", s)], dma=pre + "wg%d" % s)
            p.op("pool", lambda e: e.dma_start(out=wu[s][:], in_=wu_d[e_].rearrange("(c p) f -> p c f", p=128)),
                 r=list(after), w=[K(pre, "wu", s)], dma=pre + "wu%d" % s)
            p.op("pool", lambda e: e.dma_start(out=wd[s][:], in_=wd_d[e_].rearrange("(c p) f -> p c f", p=128)),
                 r=list(after), w=[K(pre, "wd", s)], dma=pre + "wd%d" % s)
        load_w(0)
        load_w(1)

        for i in range(NT):
            for half in range(2):
                pt = ps_tr[half]
                for cc in range(4):
                    c8 = half * 4 + cc
                    p.op("pe", (lambda i, c8, cc, pt: lambda e: e.transpose(pt[:, cc * 128:(cc + 1) * 128], acc[:, i, c8 * 128:(c8 + 1) * 128], c["ident"][:]))(i, c8, cc, pt),
                         r=[K("acc", i), "ident"], w=[K(pre, "ps_tr", half)])
                f32t = hF32[i % 2]
                p.op("act", (lambda i, half, pt, f32t: lambda e: e.copy(f32t[:, half * 4:(half + 1) * 4, :], pt[:].rearrange("p (c t) -> p c t", c=4)))(i, half, pt, f32t),
                     r=[K(pre, "ps_tr", half)], w=[K(pre, "hF32", i % 2, half), K(pre, "ps_tr", half)])
                p.op("dve", (lambda i, half, pt: lambda e: e.tensor_copy(hFM[:, half * 4:(half + 1) * 4, i * 128:(i + 1) * 128], pt[:].rearrange("p (c t) -> p c t", c=4)))(i, half, pt),
                     r=[K(pre, "ps_tr", half)], w=[K(pre, "hFM", i // 4), K(pre, "ps_tr", half)])
            pl = ps_d[i % 2]
            for c8 in range(8):
                p.op("pe", (lambda i, c8, pl: lambda e: e.matmul(pl[:, 0:16], hF32[i % 2][:, c8, :], rw[:, c8, :], start=(c8 == 0), stop=(c8 == 7)))(i, c8, pl),
                     r=[K(pre, "hF32", i % 2, c8 // 4), K(pre, "rw")], w=[K(pre, "ps_d", i % 2)])
            p.op("act", (lambda i, pl: lambda e: e.copy(lg[:, i, :], pl[:, 0:16]))(i, pl), r=[K(pre, "ps_d", i % 2)], w=[K(pre, "lg")])
            p.op("act", (lambda i: lambda e: e.mul(acc[:, i, :], acc[:, i, :], ALPHA))(i), r=[K("acc", i)], w=[K("acc", i)])

        S = p.sb(pre + "S", [128, NT, 16], F32)
        SS = p.sb(pre + "SS", [128, NT, 16], F32)
        CMP = p.sb(pre + "CMP", [128, NT * 4, 4, 4], F32)
        RK = p.sb(pre + "RK", [128, NT * 4, 4], F32)
        GS = p.sb(pre + "GS", [128, NT, 4], F32)
        GM = p.sb(pre + "GM", [128, NT], F32)
        G = p.sb(pre + "G", [128, NT, 16], F32)
        kr = K(pre, "router")
        p.op("act", lambda e: e.activation(S[:], lg[:], AF.Sigmoid), r=[K(pre, "lg")], w=[K(pre, "S")])
        p.op("dve", lambda e: e.tensor_tensor(SS[:], S[:], rbb[:].unsqueeze(1).to_broadcast([128, NT, 16]), ALU.add),
             r=[K(pre, "S"), K(pre, "rbb")], w=[kr])
        ssv = SS[:].rearrange("p i (g a) -> p (i g) a", g=4)
        in_b = ssv.unsqueeze(2).to_broadcast([128, NT * 4, 4, 4])
        in_a = ssv.unsqueeze(3).to_broadcast([128, NT * 4, 4, 4])
        p.op("dve", lambda e: e.tensor_tensor(CMP[:], in_b, in_a, ALU.is_gt), r=[kr], w=[kr])
        p.op("dve", lambda e: e.tensor_reduce(RK[:], CMP[:], AX.X, ALU.add), r=[kr], w=[kr])
        p.op("dve", lambda e: e.tensor_scalar(RK[:], RK[:], 1.5, None, ALU.is_lt), r=[kr], w=[kr])
        p.op("dve", lambda e: e.tensor_tensor(CMP[:, :, 0, :], ssv, RK[:], ALU.mult), r=[kr], w=[kr])
        p.op("dve", lambda e: e.tensor_reduce(GS[:].rearrange("p i g -> p (i g)"), CMP[:, :, 0, :], AX.X, ALU.add), r=[kr], w=[kr])
        p.op("dve", lambda e: e.tensor_reduce(GM[:], GS[:], AX.X, ALU.max), r=[kr], w=[kr])
        p.op("dve", lambda e: e.tensor_tensor(GS[:], GS[:], GM[:].unsqueeze(2).to_broadcast([128, NT, 4]), ALU.is_ge), r=[kr], w=[kr])
        p.op("dve", lambda e: e.tensor_tensor(G[:].rearrange("p i (g a) -> p i g a", g=4), RK[:].rearrange("p (i g) a -> p i g a", g=4),
                                              GS[:].unsqueeze(3).to_broadcast([128, NT, 4, 4]), ALU.mult), r=[kr], w=[kr])
        p.op("dve", lambda e: e.tensor_tensor(G[:], G[:], S[:], ALU.mult), r=[kr, K(pre, "S")], w=[kr])
        p.op("dve", lambda e: e.tensor_reduce(GM[:], G[:], AX.X, ALU.add), r=[kr], w=[kr])
        p.op("dve", lambda e: e.reciprocal(GM[:], GM[:]), r=[kr], w=[kr])
        p.op("dve", lambda e: e.tensor_tensor(G[:], G[:], GM[:].unsqueeze(2).to_broadcast([128, NT, 16]), ALU.mult), r=[kr], w=[kr])

        gbc = p.sb(pre + "gbc", [128, D], F32)
        bbc = p.sb(pre + "bbc", [128, D], F32)
        st4 = [p.sb(pre + "st4_%d" % j, [128, 8, 4], F32) for j in range(2)]
        p.dma("sp", gbc[:], lng_d.partition_broadcast(128), pre + "gbc", w=[K(pre + "ln", "gbc")])
        p.dma("sp", bbc[:], lnb_d.partition_broadcast(128), pre + "bbc", w=[K(pre + "ln", "bbc")])
        outk = []
        pendln = []
        pd4 = [(ps_d[0], K(pre, "ps_d", 0)), (ps_d[1], K(pre, "ps_d", 1)), (ps_tr[0], K(pre, "ps_tr", 0)), (ps_tr[1], K(pre, "ps_tr", 1))]
        n = 0
        for e_ in range(16):
            s = e_ % 2
            for tb in range(4):
                if e_ == 15:
                    p.capture()
                hb = hid[tb % 2]
                for hc in range(4):
                    j = n % 2
                    n += 1
                    for c8 in range(8):
                        p.op("pe", (lambda s, hc, c8, tb, j: lambda e: e.matmul(ps_g[j][:], wg[s][:, c8, hc * 128:(hc + 1) * 128], hFM[:, c8, tb * 512:(tb + 1) * 512], start=(c8 == 0), stop=(c8 == 7)))(s, hc, c8, tb, j),
                             r=[K(pre, "wg", s), K(pre, "hFM", tb)], w=[K(pre, "ps_g", j)])
                    for c8 in range(8):
                        p.op("pe", (lambda s, hc, c8, tb, j: lambda e: e.matmul(ps_u[j][:], wu[s][:, c8, hc * 128:(hc + 1) * 128], hFM[:, c8, tb * 512:(tb + 1) * 512], start=(c8 == 0), stop=(c8 == 7)))(s, hc, c8, tb, j),
                             r=[K(pre, "wu", s), K(pre, "hFM", tb)], w=[K(pre, "ps_u", j)])
                    p.op("act", (lambda j: lambda e: e.activation(sg[j][:], ps_g[j][:], AF.Silu))(j),
                         r=[K(pre, "ps_g", j)], w=[K(pre, "sg", j)])
                    p.op("dve", (lambda j, hb, hc: lambda e: e.tensor_tensor(hb[:, hc, :], sg[j][:], ps_u[j][:], ALU.mult))(j, hb, hc),
                         r=[K(pre, "sg", j), K(pre, "ps_u", j)], w=[K(pre, "hid", tb % 2, hc)])
                for tt in range(4):
                    i = tb * 4 + tt
                    for half in range(2):
                        j = (tt * 2 + half) % 4
                        pdj, kdj = pd4[j]
                        for hc in range(4):
                            p.op("pe", (lambda s, hc, tt, half, pdj, hb: lambda e: e.matmul(pdj[:], hb[:, hc, tt * 128:(tt + 1) * 128], wd[s][:, hc, half * 512:(half + 1) * 512], start=(hc == 0), stop=(hc == 3)))(s, hc, tt, half, pdj, hb),
                                 r=[K(pre, "hid", tb % 2, hc), K(pre, "wd", s)], w=[kdj])
                        p.op("dve", (lambda i, half, pdj, e_: lambda e: e.scalar_tensor_tensor(acc[:, i, half * 512:(half + 1) * 512], pdj[:], G[:, i, e_:e_ + 1], acc[:, i, half * 512:(half + 1) * 512], ALU.mult, ALU.add))(i, half, pdj, e_),
                             r=[kdj, kr, K("acc", i)], w=[K("acc", i), kdj])
                if e_ == 15:
                    la = p.end_capture()
                    p.replay_merged(la, pendln)
                    zs = [acc[:, tb * 4 + t, :] for t in range(4)]
                    zks = [K("acc", tb * 4 + t) for t in range(4)]
                    outs = [h_out[(tb * 4 + t) * 128:(tb * 4 + t + 1) * 128, :] for t in range(4)]
                    p.capture()
                    outk += ln_group(p, pre + "ln", zs, zks, gbc, bbc, outs, tb, st4[tb % 2], tmp)
                    pendln = p.end_capture()
                    if tb == 3:
                        p.replay_merged(pendln)
            if e_ + 2 < 16:
                load_w(e_ + 2)
        p.op("sp", None, r=outk)
        p.emit()
    p.nc.all_engine_barrier()


def ln_tile(p, pre, z, gbc, bbc, out_rows, i, zk, st, sq):
    j = i % 2
    ks = K(pre, "lnst", j)
    p.op("dve", lambda e: e.reduce_sum(st[:, 0:1], z, axis=AX.X), r=[zk], w=[ks])
    p.op("act", lambda e: e.activation(sq[:], z, AF.Square), r=[zk], w=[K(pre, "lnsq", j)])
    p.op("dve", lambda e: e.reduce_sum(st[:, 1:2], sq[:], axis=AX.X), r=[K(pre, "lnsq", j), ks], w=[ks])
    p.op("dve", lambda e: e.tensor_scalar(st[:, 0:2], st[:, 0:2], 1.0 / D, None, ALU.mult), r=[ks], w=[ks])
    p.op("dve", lambda e: e.tensor_tensor(st[:, 2:3], st[:, 0:1], st[:, 0:1], ALU.mult), r=[ks], w=[ks])
    p.op("dve", lambda e: e.tensor_tensor(st[:, 3:4], st[:, 1:2], st[:, 2:3], ALU.subtract), r=[ks], w=[ks])
    p.op("dve", lambda e: e.tensor_scalar(st[:, 3:4], st[:, 3:4], LN_EPS, None, ALU.add), r=[ks], w=[ks])
    p.op("act", lambda e: e.activation(st[:, 4:5], st[:, 3:4], AF.Sqrt), r=[ks], w=[ks])
    p.op("dve", lambda e: e.reciprocal(st[:, 5:6], st[:, 4:5]), r=[ks], w=[ks])
    p.op("dve", lambda e: e.scalar_tensor_tensor(st[:, 6:7], st[:, 0:1], -1.0, st[:, 5:6], ALU.mult, ALU.mult), r=[ks], w=[ks])
    p.op("act", lambda e: e.activation(z, z, AF.Identity, bias=st[:, 6:7], scale=st[:, 5:6]), r=[zk, ks], w=[zk])
    p.op("dve", lambda e: e.tensor_tensor(z, z, gbc[:], ALU.mult), r=[zk, K(pre, "gbc")], w=[zk])
    p.op("pool", lambda e: e.tensor_tensor(z, z, bbc[:], ALU.add), r=[zk, K(pre, "bbc")], w=[zk])
    ok = K(pre, "out", i)
    p.op("sp", lambda e: e.dma_start(out=out_rows, in_=z), r=[zk], w=[ok], dma=pre + "st%d" % j)
    return ok


import os
CUT = int(os.environ.get('FOX_CUT', '0'))
CUT2 = int(os.environ.get('FOX_CUT2', '0'))


def ln_group(p, pre, zs, zks, gbc, bbc, outs, gi, st, sq):
    n = len(zs)
    j = gi % 2
    ks = K(pre, "lnst", j)
    for t in range(n):
        sqt = sq[t % len(sq)]
        kq = K(pre, "lnsq", t % len(sq))
        p.op("act", (lambda sqt, z, o_: lambda e: e.activation(sqt[:], z, AF.Identity, accum_out=o_))(sqt, zs[t], st[:, 0, t:t + 1]), r=[zks[t], ks], w=[kq, ks])
        p.op("act", (lambda sqt, z, o_: lambda e: e.activation(sqt[:], z, AF.Square, accum_out=o_))(sqt, zs[t], st[:, 1, t:t + 1]), r=[zks[t], ks], w=[kq, ks])
    p.ts("dve", st[:, 0:2, 0:n], st[:, 0:2, 0:n], 1.0 / D, None, ALU.mult, r=[ks], w=[ks])
    p.tt("dve", st[:, 2, 0:n], st[:, 0, 0:n], st[:, 0, 0:n], ALU.mult, r=[ks], w=[ks])
    p.tt("dve", st[:, 3, 0:n], st[:, 1, 0:n], st[:, 2, 0:n], ALU.subtract, r=[ks], w=[ks])
    p.ts("dve", st[:, 3, 0:n], st[:, 3, 0:n], LN_EPS, None, ALU.add, r=[ks], w=[ks])
    p.act(st[:, 4, 0:n], st[:, 3, 0:n], AF.Ln, r=[ks], w=[ks])
    p.act(st[:, 5, 0:n], st[:, 4, 0:n], AF.Exp, scale=-0.5, r=[ks], w=[ks])
    p.stt("dve", st[:, 6, 0:n], st[:, 0, 0:n], -1.0, st[:, 5, 0:n], ALU.mult, ALU.mult, r=[ks], w=[ks])
    oks = []
    for t in range(n):
        z = zs[t]
        p.act(z, z, AF.Identity, bias=st[:, 6, t:t + 1], scale=st[:, 5, t:t + 1], r=[zks[t], ks], w=[zks[t]])
        p.tt("dve", z, z, gbc[:], ALU.mult, r=[zks[t], K(pre, "gbc")], w=[zks[t]])
        ok = K(pre, "out", gi, t)
        if isinstance(outs[t], tuple):
            p.tt("pool", outs[t][1], z, bbc[:], ALU.add, r=[zks[t], K(pre, "bbc")], w=[ok])
        else:
            p.tt("pool", z, z, bbc[:], ALU.add, r=[zks[t], K(pre, "bbc")], w=[zks[t]])
            p.dma("sp", outs[t], z, pre + "st%d_%d" % (j, t), r=[zks[t]], w=[ok])
        oks.append(ok)
    return oks


def stage_fox(p, pre, h_in, h_out, win_d, bf_d, wo_d, lng_d, lnb_d, dbg=None, acc=None):
    nc = p.nc
    HG = 4
    NG = 16 // HG
    outer = ExitStack()
    with outer:
        p.es = outer
        hFM = p.sb(pre + "hFM", [128, 8, T], BF16)
        csplit = p.sb(pre + "csplit", [80, T], BF16)
        ckTM = p.sb(pre + "ckTM", [128, NT, 16], F32)
        with ExitStack() as es:
            p.begin(es)
            c = consts(p)
            ht = [p.sb(pre + "ht%d" % j, [128, D], F32) for j in range(2)]
            hF32 = [p.sb(pre + "hF32_%d" % j, [128, 8, 128], F32) for j in range(2)]
            wf = p.sb(pre + "wf", [128, 8, 16], F32)
            nbf = p.sb(pre + "nbf", [16, 1], F32)
            fT = p.sb(pre + "fT", [16, T], F32)
            A = p.sb(pre + "A", [16, T], F32)
            B = p.sb(pre + "B", [16, T], F32)
            hi = p.sb(pre + "hi", [16, T], BF16)
            ps_tr = [p.ps(pre + "ps_tr%d" % j, [128, 512]) for j in range(2)]
            ps_f = [p.ps(pre + "ps_f%d" % j, [128, 512]) for j in range(2)]
            p.op("sp", lambda e: e.dma_start(out=wf[:], in_=win_d[:, 3072:3088].rearrange("(c p) e -> p c e", p=128)), w=["wf"], dma=pre + "wf")
            p.op("sp", lambda e: e.dma_start(out=nbf[:], in_=bf_d.rearrange("(h o) -> h o", o=1)), w=["nbf"], dma=pre + "nbf")
            p.op("dve", lambda e: e.tensor_scalar(nbf[:], nbf[:], -1.0, None, ALU.mult), r=["nbf"], w=["nbf"])
            p.op("pool", lambda e: e.memset(csplit[:], 0.0), w=["csplit"])
            for i in range(NT):
                hti = ht[i % 2]
                p.op("sp", (lambda i, hti: lambda e: e.dma_start(out=hti[:], in_=h_in[i * 128:(i + 1) * 128, :]))(i, hti),
                     w=[K("ht", i % 2)], dma=pre + "ht%d" % (i % 2))
                for half in range(2):
                    pt = ps_tr[half]
                    for cc in range(4):
                        c8 = half * 4 + cc
                        p.op("pe", (lambda hti, c8, cc, pt: lambda e: e.transpose(pt[:, cc * 128:(cc + 1) * 128], hti[:, c8 * 128:(c8 + 1) * 128], c["ident"][:]))(hti, c8, cc, pt),
                             r=[K("ht", i % 2), "ident"], w=[K("ps_tr", half)])
                    f32t = hF32[i % 2]
                    p.op("act", (lambda half, pt, f32t: lambda e: e.copy(f32t[:, half * 4:(half + 1) * 4, :], pt[:].rearrange("p (c t) -> p c t", c=4)))(half, pt, f32t),
                         r=[K("ps_tr", half)], w=[K("hF32", i % 2, half), K("ps_tr", half)])
                    p.op("dve", (lambda i, half, pt: lambda e: e.tensor_copy(hFM[:, half * 4:(half + 1) * 4, i * 128:(i + 1) * 128], pt[:].rearrange("p (c t) -> p c t", c=4)))(i, half, pt),
                         r=[K("ps_tr", half)], w=[K("hFM", i), K("ps_tr", half)])
                if CUT == 1:
                    continue
                pf = ps_f[(i // 4) % 2]
                for c8 in range(8):
                    p.op("pe", (lambda i, c8, pf: lambda e: e.matmul(pf[0:16, (i % 4) * 128:(i % 4 + 1) * 128], wf[:, c8, :], hF32[i % 2][:, c8, :], start=(c8 == 0), stop=(c8 == 7)))(i, c8, pf),
                         r=[K("hF32", i % 2, c8 // 4), "wf"], w=[K("ps_f", (i // 4) % 2)])
                if i % 4 == 3:
                    tb = i // 4
                    p.op("act", (lambda tb, pf: lambda e: e.activation(fT[:, tb * 512:(tb + 1) * 512], pf[0:16, :], AF.Exp, bias=nbf[:], scale=-1.0))(tb, pf),
                         r=[K("ps_f", tb % 2), "nbf"], w=[K("fT", tb), K("ps_f", tb % 2)])
                    p.op("act", (lambda tb: lambda e: e.activation(A[:, tb * 512:(tb + 1) * 512], fT[:, tb * 512:(tb + 1) * 512], AF.Ln, bias=1.0))(tb),
                         r=[K("fT", tb)], w=[id(A)])
            cur, oth = A, B
            s = 1
            while s < T and CUT not in (1, 2):
                p.op("dve", (lambda cur, oth, s: lambda e: e.tensor_tensor(oth[:, s:T], cur[:, s:T], cur[:, 0:T - s], ALU.add))(cur, oth, s),
                     r=[id(cur)], w=[id(oth)])
                p.op("act", (lambda cur, oth, s: lambda e: e.copy(oth[:, 0:s], cur[:, 0:s]))(cur, oth, s),
                     r=[id(cur)], w=[id(oth)])
                cur, oth = oth, cur
                s *= 2
            csum = cur
            for i in range(NT if CUT not in (1, 2, 3) else 0):
                p.op("pe", (lambda i: lambda e: e.transpose(ps_tr[0][:, i * 16:(i + 1) * 16], csum[:, i * 128:(i + 1) * 128], c["ident"][0:16, 0:16]))(i),
                     r=[id(csum), "ident"], w=[K("ps_tr", 0)])
            p.op("act", lambda e: e.copy(ckTM[:].rearrange("p i h -> p (i h)"), ps_tr[0][:, 0:256]), r=[K("ps_tr", 0)], w=["ckTM", K("ps_tr", 0)])
            m8 = oth
            p.op("dve", lambda e: e.tensor_scalar(m8[:], csum[:], -8.0, None, ALU.mult), r=[id(csum)], w=[id(m8)])
            for j in range(3 if CUT not in (1, 2, 3, 4) else 0):
                dst = csplit[32 * j:32 * j + 16, :]
                p.op("act", (lambda dst: lambda e: e.copy(dst, m8[:]))(dst), r=[id(m8)], w=["csplit"])
                if j < 2:
                    p.op("act", lambda e: e.copy(hi[:], m8[:]), r=[id(m8)], w=["hi"])
                    p.op("act", lambda e: e.copy(fT[:], hi[:]), r=["hi"], w=["hif"])
                    p.op("dve", lambda e: e.tensor_tensor(m8[:], m8[:], fT[:], ALU.subtract), r=[id(m8), "hif"], w=[id(m8)])
            if dbg is not None:
                p.op("sp", lambda e: e.dma_start(out=dbg[0:128, 0:256], in_=ckTM[:].rearrange("p i h -> p (i h)")), r=["ckTM"], w=["dbg"], dma="dbg")
                p.op("sp", None, r=["dbg"])
            p.op("sp", None, r=["csplit", "ckTM"] + [K("hFM", i) for i in range(NT)])
            p.emit()
        nc.all_engine_barrier()
        if dbg is not None:
            return
        p.es = outer
        OT = p.sb(pre + "OT", [128, 8, T], BF16)
        wo = p.sb(pre + "wo", [128, 8, D], BF16)
        with ExitStack() as es:
            p.begin(es)
            KTe = [p.sb(pre + "KTe%d" % h, [67, T], BF16) for h in range(HG)]
            QTe = [[p.sb(pre + "QTe%d_%d" % (j, h), [67, 512], BF16) for h in range(HG)] for j in range(2)]
            Vext = p.sb(pre + "Vext", [128, NT, HG, 128], BF16)
            wq = p.sb(pre + "wq", [128, 8, HG * 64], BF16)
            wk = p.sb(pre + "wk", [128, 8, HG * 64], BF16)
            wv = p.sb(pre + "wv", [128, 8, HG * 64], BF16)
            pT = [p.sb(pre + "pT%d" % j, [128, 512], BF16) for j in range(4)]
            rd1 = p.sb(pre + "rd", [64, 512], F32)
            rd = [rd1, rd1]
            ps_s = [p.ps(pre + "ps_s%d" % j, [128, 512]) for j in range(4)]
            ps_o = [p.ps(pre + "ps_o%d" % j, [128, 512]) for j in range(2)]
            ps_p = [p.ps(pre + "ps_p%d" % j, [128, 512]) for j in range(2)]
            for h in range(HG):
                p.op("dve", (lambda h: lambda e: e.memset(KTe[h][64:67, :], 1.0))(h), w=[K("KTones", h)])
            p.op("dve", lambda e: e.memset(Vext[:, :, :, 64:128], 1.0), w=["Vones"])
            def csrows(h, qb):
                base = csplit[h:h + 1, qb * 512:(qb + 1) * 512]
                return bass.AP(base.tensor, base.offset, [[32 * base.ap[0][0], 3]] + [list(x) for x in base.ap[1:]])
            nq = 0
            nsj = 0
            no = 0
            npp = 0
            for g in range(NG if CUT2 == 0 else 1):
                c0 = g * HG * 64
                p.op("pool", (lambda c0: lambda e: e.dma_start(out=wk[:], in_=win_d[:, D + c0:D + c0 + HG * 64].rearrange("(c p) f -> p c f", p=128)))(c0), w=["wk"], dma=pre + "wk")
                p.op("pool", (lambda c0: lambda e: e.dma_start(out=wq[:], in_=win_d[:, c0:c0 + HG * 64].rearrange("(c p) f -> p c f", p=128)))(c0), w=["wq"], dma=pre + "wq")
                p.op("pool", (lambda c0: lambda e: e.dma_start(out=wv[:], in_=win_d[:, 2 * D + c0:2 * D + c0 + HG * 64].rearrange("(c p) f -> p c f", p=128)))(c0), w=["wv"], dma=pre + "wv")
                if g == 0:
                    p.op("pool", lambda e: e.dma_start(out=wo[:], in_=wo_d.rearrange("(j p) f -> p j f", p=128)), w=["wo"], dma=pre + "wo")
                for a_ in range(HG // 2):
                    for tb in range(4):
                        pp = ps_p[npp % 2]
                        kp = K("ps_p", npp % 2)
                        npp += 1
                        for c8 in range(8):
                            p.mm(pp[:], wk[:, c8, a_ * 128:(a_ + 1) * 128], hFM[:, c8, tb * 512:(tb + 1) * 512], c8 == 0, c8 == 7, r=["wk"], w=[kp])
                        for hh_ in range(2):
                            hl = 2 * a_ + hh_
                            p.cp("dve", KTe[hl][0:64, tb * 512:(tb + 1) * 512], pp[hh_ * 64:(hh_ + 1) * 64, :], r=[kp], w=[K("KTe", hl, tb), kp])
                for i in range(NT):
                    pp = ps_p[npp % 2]
                    kp = K("ps_p", npp % 2)
                    npp += 1
                    for c8 in range(8):
                        p.op("pe", (lambda i, c8, pp: lambda e: e.matmul(pp[:, 0:HG * 64], hFM[:, c8, i * 128:(i + 1) * 128], wv[:, c8, :], start=(c8 == 0), stop=(c8 == 7)))(i, c8, pp),
                             r=["wv"], w=[kp])
                    p.op("dve", (lambda i, pp: lambda e: e.tensor_copy(Vext[:, i, :, 0:64], pp[:, 0:HG * 64].rearrange("p (h d) -> p h d", h=HG)))(i, pp),
                         r=[kp], w=[K("V", i), kp])
                for qb in range(4 if CUT2 == 0 else (0 if CUT2 == 1 else 1)):
                    qs = nq % 2
                    nq += 1
                    for hl in range(HG):
                        h = g * HG + hl
                        p.dma("sp", QTe[qs][hl][64:67, :], csplit[:, qb * 512:(qb + 1) * 512].rearrange("(j r) f -> j r f", r=32)[0:3, h, :] if False else csrows(h, qb),
                              pre + "qc%d_%d" % (qs, hl), r=["csplit"], w=[K("QTc", qs, hl)])
                    for a_ in range(HG // 2):
                        pp = ps_p[npp % 2]
                        kp = K("ps_p", npp % 2)
                        npp += 1
                        for c8 in range(8):
                            p.mm(pp[:], wq[:, c8, a_ * 128:(a_ + 1) * 128], hFM[:, c8, qb * 512:(qb + 1) * 512], c8 == 0, c8 == 7, r=["wq"], w=[kp])
                        for hh_ in range(2):
                            hl = 2 * a_ + hh_
                            p.cp("dve", QTe[qs][hl][0:64, :], pp[hh_ * 64:(hh_ + 1) * 64, :], r=[kp], w=[K("QTe", qs, hl), kp])
                    jobs = []
                    for hl in range(HG):
                        h = g * HG + hl
                        nkt = 4 * qb + 4
                        for kt in range(nkt):
                            jobs.append((hl, h, kt, nkt, no % 2))
                        no += 1
                    LA = 3
                    inflight = {}

                    def front(job, ji):
                        hl, h, kt, nkt, oj = job
                        sj = ji % 4
                        d_ = kt - 4 * qb
                        lo = 128 * d_ if d_ > 0 else 0
                        p.mm(ps_s[sj][:, lo:512], KTe[hl][:, kt * 128:(kt + 1) * 128], QTe[qs][hl][:, lo:512], True, True,
                             r=[K("KTe", hl, kt // 4), K("KTones", hl), K("QTe", qs, hl), K("QTc", qs, hl)], w=[K("ps_s", sj)])
                        p.act(pT[sj][:, lo:512], ps_s[sj][:, lo:512], AF.Exp, bias=ckTM[:, kt, h:h + 1], scale=0.125,
                              r=[K("ps_s", sj)], w=[K("pT", sj), K("ps_s", sj)])
                        if d_ >= 0:
                            p.op("pool", (lambda sj, lo, base: lambda e: e.affine_select(out=pT[sj][:, lo:512], in_=pT[sj][:, lo:512], pattern=[[1, 512 - lo]], compare_op=ALU.is_ge, fill=0.0, base=base, channel_multiplier=-1))(sj, lo, qb * 512 + lo - kt * 128),
                                 r=[K("pT", sj)], w=[K("pT", sj)])
                        inflight[ji] = (sj, lo)

                    def back(job, ji):
                        hl, h, kt, nkt, oj = job
                        sj, lo = inflight.pop(ji)
                        po = ps_o[oj]
                        ko = K("ps_o", oj)
                        p.mm(po[:, lo:512], Vext[:, kt, hl, :], pT[sj][:, lo:512], kt == 0, kt == nkt - 1,
                             r=[K("V", kt), "Vones", K("pT", sj)], w=[ko])
                        if kt == nkt - 1:
                            hh, hj = h % 2, h // 2
                            p.rcp(rd[oj][:], po[64:128, :], r=[ko], w=[K("rd", 0), ko])
                            p.tt("dve", OT[hh * 64:(hh + 1) * 64, hj, qb * 512:(qb + 1) * 512], po[0:64, :], rd[oj][:], ALU.mult,
                                 r=[ko, K("rd", 0)], w=[K("OT", qb), ko])
                    nj = len(jobs) if CUT2 not in (1, 2) else 0
                    for ji in range(nj + LA):
                        if ji < nj:
                            front(jobs[ji], nsj + ji)
                        if ji - LA >= 0:
                            back(jobs[ji - LA], nsj + ji - LA)
                    nsj += nj
            p.op("sp", None, r=[K("OT", qb) for qb in range(4)] + ["wo"])
            p.emit()
        nc.all_engine_barrier()
        with ExitStack() as es:
            p.begin(es)
            z4 = [p.sb(pre + "z4_%d" % j, [128, 4, D], F32) for j in range(2)]
            h2t = [p.sb(pre + "h2t%d" % j, [128, D], F32) for j in range(2)]
            sq = [p.sb(pre + "sq%d" % j, [128, D], F32) for j in range(2)]
            st4 = [p.sb(pre + "st4_%d" % j, [128, 8, 4], F32) for j in range(2)]
            gbc = p.sb(pre + "gbc", [128, D], F32)
            bbc = p.sb(pre + "bbc", [128, D], F32)
            ps_p = [p.ps(pre + "ps_p3_%d" % j, [128, 512]) for j in range(3)]
            p.op("sp", lambda e: e.dma_start(out=gbc[:], in_=lng_d.partition_broadcast(128)), w=[K(pre, "gbc")], dma=pre + "gbc")
            p.op("sp", lambda e: e.dma_start(out=bbc[:], in_=lnb_d.partition_broadcast(128)), w=[K(pre, "bbc")], dma=pre + "bbc")
            npp = 0
            outk = []
            pend = []
            for gi in range(NT // 4 if CUT2 in (0, 4) else 0):
                p.capture()
                zg = z4[gi % 2]
                zs, zks, outs = [], [], []
                for t in range(4):
                    i = gi * 4 + t
                    j = i % 2
                    p.dma("sp", h2t[j][:], h_in[i * 128:(i + 1) * 128, :], pre + "h2t%d" % j, w=[K("h2t", j)])
                    zk = K("z4", gi % 2, t)
                    for half in range(2):
                        pp = ps_p[npp % 3]
                        kp = K("ps_p", npp % 3)
                        npp += 1
                        for hj in range(8):
                            p.mm(pp[:], OT[:, hj, i * 128:(i + 1) * 128], wo[:, hj, half * 512:(half + 1) * 512], hj == 0, hj == 7, w=[kp])
                        sl = slice(half * 512, (half + 1) * 512)
                        p.stt("dve", zg[:, t, sl], h2t[j][:, sl], ALPHA, pp[:], ALU.mult, ALU.add, r=[kp, K("h2t", j)], w=[zk, kp])
                    zs.append(zg[:, t, :])
                    zks.append(zk)
                    outs.append(h_out[i * 128:(i + 1) * 128, :] if acc is None else ("sb", acc[:, i, :]))
                la = p.end_capture()
                p.replay_merged(la, pend)
                p.capture()
                outk += ln_group(p, pre, zs, zks, gbc, bbc, outs, gi, st4[gi % 2], sq)
                pend = p.end_capture()
            p.replay_merged(pend)
            p.op("sp", None, r=outk)
            p.emit()
        nc.all_engine_barrier()


def bc_load(p, name, src_1d, n=128):
    t = p.sb(name, [n, D], F32)
    p.dma("sp", t[:], src_1d.partition_broadcast(n), "ld_" + name, w=[name])
    return t


def stage_rwkv1(p, pre, xT_d, W, S):
    nc = p.nc
    with ExitStack() as es:
        p.begin(es)
        c = consts(p)
        wr = p.sb("wr", [128, 8, D], BF16)
        wk = p.sb("wk", [128, 8, D], BF16)
        wv = p.sb("wv", [128, 8, D], BF16)
        for t_, d_, n_ in ((wr, W["wr"], "wr"), (wk, W["wk"], "wk"), (wv, W["wv"], "wv")):
            p.dma("pool", t_[:], d_.rearrange("(c p) f -> p c f", p=128), "ld_" + n_, w=[n_])
        w1 = p.sb("w1", [128, 8, 64], BF16)
        a1 = p.sb("a1", [128, 8, 64], BF16)
        g1 = p.sb("g1", [128, 8, 128], BF16)
        w2 = p.sb("w2", [64, D], BF16)
        a2 = p.sb("a2", [64, D], BF16)
        g2 = p.sb("g2", [128, D], BF16)
        for t_, d_, n_ in ((w1, W["w1"], "w1"), (a1, W["a1"], "a1"), (g1, W["g1"], "g1")):
            p.dma("pool", t_[:], d_.rearrange("(c p) f -> p c f", p=128), "ld_" + n_, w=[n_])
        for t_, d_, n_ in ((w2, W["w2"], "w2"), (a2, W["a2"], "a2"), (g2, W["g2"], "g2")):
            p.dma("pool", t_[:], d_, "ld_" + n_, w=[n_])
        W0 = bc_load(p, "W0", W["w0"])
        A0 = bc_load(p, "A0", W["a0"])
        KKp = bc_load(p, "KKp", W["kk"])
        KAp = bc_load(p, "KAp", W["ka"])
        RKp = bc_load(p, "RKp", W["rk"].rearrange("h n -> (h n)"))
        mix = p.sb("mix", [128, 6, 8], F32)
        p.dma("sp", mix[:], W["mix"].rearrange("m (c p) -> p m c", p=128), "ld_mix", w=["mix"], slow=True)
        triu = p.sb("triu", [128, 128], F32)
        ones = p.sb("ones", [128, 128], F32)
        p.op("pool", lambda e: e.memset(triu[:], 1.0), w=["triu"])
        p.op("pool", lambda e: e.affine_select(out=triu[:], in_=triu[:], pattern=[[1, 128]], compare_op=ALU.is_ge, fill=0.0, base=0, channel_multiplier=-1), r=["triu"], w=["triu"])
        p.op("pool", lambda e: e.memset(ones[:], 1.0), w=["ones"])

        xt = [p.sb("xt%d" % j, [128, 8, 129], F32) for j in range(2)]
        xx = p.sb("xx", [128, 8, 128], F32)
        xm = [[p.sb("xm%d_%d" % (s_, j), [128, 8, 128], BF16) for j in range(6)] for s_ in range(2)]
        dbl = lambda n_: [p.sb("%s%d" % (n_, j), [128, D], F32) for j in range(2)]
        R, K0, V32, Aa, LDP = dbl("R"), dbl("K0"), dbl("V32"), dbl("Aa"), dbl("LDP")
        KKn = p.sb("KKn", [128, D], F32)
        KP = p.sb("KP", [128, D], F32)
        Bb = p.sb("Bb", [128, D], F32)
        C32 = p.sb("C32", [128, D], F32)
        T1 = p.sb("T1", [128, D], F32)
        T2 = p.sb("T2", [128, D], F32)
        T3 = p.sb("T3", [128, D], F32)
        Gg = p.sb("Gg", [128, D], F32)
        sm = p.sb("sm", [128, 4, 16], F32)
        ET = p.sb("ET", [64, 16], F32)
        hid = p.sb("hid", [128, 3, 128], F32)
        hidb = p.sb("hidb", [128, 3, 128], BF16)
        ob = {n_: p.sb("o_" + n_, [128, D], BF16) for n_ in ("RT", "KKT", "KH", "BH", "KG", "BG", "Vb")}
        psT = [[p.ps("psT%d_%d" % (j, h), [128, 512]) for h in range(2)] for j in range(3)]
        ps_l = p.ps("ps_l", [128, 512])
        ps_e = p.ps("ps_e", [128, 512])
        nps = [0]

        def tm_ps():
            j = nps[0] % 2
            nps[0] += 1
            return psT[j], [K("psT", j, 0), K("psT", j, 1)]

        def proj_tm(lhs_tile, lk, w_t, wkey, kc):
            pt, pk = tm_ps()
            for half in range(2):
                if kc is None:
                    p.mm(pt[half][:], lhs_tile, w_t[:, half * 512:(half + 1) * 512], True, True, r=[lk, wkey], w=[pk[half]])
                else:
                    for c8 in range(kc):
                        p.mm(pt[half][:], lhs_tile[:, c8, :], w_t[:, c8, half * 512:(half + 1) * 512], c8 == 0, c8 == kc - 1, r=[lk, wkey], w=[pk[half]])
            return pt, pk

        def halves(pt, pk, fn):
            for half in range(2):
                fn(slice(half * 512, (half + 1) * 512), pt[half], pk[half])

        xTv = xT_d.rearrange("(c p) t -> p c t", p=128)
        hn = lambda t_: t_[:].rearrange("p (h n) -> p h n", h=16)

        def load_xt(j):
            s_ = j % 2
            x_ = xt[s_]
            kx = K("xt", s_)
            if j == 0:
                p.op("pool", lambda e: e.memset(x_[:, :, 0:1], 0.0), w=[K("xt0c", 0)])
                p.dma("sp", x_[:, :, 1:129], xTv[:, :, 0:128], "ld_xt0", w=[kx])
            else:
                p.dma("sp", x_[:, :, :], xTv[:, :, j * 128 - 1:j * 128 + 128], "ld_xt%d" % s_, w=[kx, K("xt0c", 0)] if s_ == 0 else [kx])
        load_xt(0)
        load_xt(1)

        def P1(j):
            s_ = j % 2
            x_ = xt[s_]
            kx = K("xt", s_)
            kx_all = [kx, K("xt0c", 0)] if s_ == 0 else [kx]
            p.tt("dve", xx[:], x_[:, :, 0:128], x_[:, :, 1:129], ALU.subtract, r=kx_all, w=["xx"])
            xm_ = xm[s_]
            for m in (0, 2, 3, 1, 4, 5):
                for c8 in range(8):
                    p.stt("dve", xm_[m][:, c8, :], xx[:, c8, :], mix[:, m, c8:c8 + 1], x_[:, c8, 1:129], ALU.mult, ALU.add,
                          r=["xx", "mix"] + kx_all, w=[K("xm", s_, m)])
            if j + 2 < NT:
                load_xt(j + 2)
            pt, pk = proj_tm(xm_[0], K("xm", s_, 0), wr, "wr", 8)
            halves(pt, pk, lambda sl, ps_, k_: p.cp("act", R[s_][:, sl], ps_[:], r=[k_], w=[K("R", s_), k_]))
            pt, pk = proj_tm(xm_[2], K("xm", s_, 2), wk, "wk", 8)
            halves(pt, pk, lambda sl, ps_, k_: p.cp("act", K0[s_][:, sl], ps_[:], r=[k_], w=[K("K0", s_), k_]))
            pt, pk = proj_tm(xm_[3], K("xm", s_, 3), wv, "wv", 8)
            halves(pt, pk, lambda sl, ps_, k_: p.cp("act", V32[s_][:, sl], ps_[:], r=[k_], w=[K("V32", s_), k_]))
            halves(pt, pk, lambda sl, ps_, k_: p.cp("act", ob["Vb"][:, sl], ps_[:], r=[k_], w=["o_Vb", k_]))
            p.dma("sp", S["Vb"][j * 128:(j + 1) * 128, :], ob["Vb"][:], "st_Vb", r=["o_Vb"], w=[K("S_Vb", j)])
            for c8 in range(8):
                p.mm(ps_l[0:64, 0:128], w1[:, c8, :], xm_[1][:, c8, :], c8 == 0, c8 == 7, r=["w1", K("xm", s_, 1)], w=["ps_l"])
            for c8 in range(8):
                p.mm(ps_l[0:64, 128:256], a1[:, c8, :], xm_[4][:, c8, :], c8 == 0, c8 == 7, r=["a1", K("xm", s_, 4)], w=["ps_l"])
            for c8 in range(8):
                p.mm(ps_l[:, 256:384], g1[:, c8, :], xm_[5][:, c8, :], c8 == 0, c8 == 7, r=["g1", K("xm", s_, 5)], w=["ps_l"])
            p.act(hid[0:64, 0, :], ps_l[0:64, 0:128], AF.Exp, scale=2.0, r=["ps_l"], w=["hid0", "ps_l"])
            p.ts("dve", hid[0:64, 0, :], hid[0:64, 0, :], 1.0, None, ALU.add, r=["hid0"], w=["hid0"])
            p.rcp(hid[0:64, 0, :], hid[0:64, 0, :], r=["hid0"], w=["hid0"])
            p.ts("dve", hidb[0:64, 0, :], hid[0:64, 0, :], -2.0, 1.0, ALU.mult, ALU.add, r=["hid0"], w=["hidb0"])
            p.cp("act", hidb[0:64, 1, :], ps_l[0:64, 128:256], r=["ps_l"], w=["hidb1", "ps_l"])
            p.act(hid[:, 2, :], ps_l[:, 256:384], AF.Exp, scale=-1.0, r=["ps_l"], w=["hid2", "ps_l"])
            p.act(hid[:, 2, :], hid[:, 2, :], AF.Ln, bias=1.0, r=["hid2"], w=["hid2"])
            p.act(hidb[:, 2, :], hid[:, 2, :], AF.Exp, scale=-1.0, r=["hid2"], w=["hidb2"])
            L_ = LDP[s_]
            kL = K("LDP", s_)
            pt, pk = proj_tm(hidb[0:64, 0, :], "hidb0", w2, "w2", None)
            halves(pt, pk, lambda sl, ps_, k_: p.tt("dve", L_[:, sl], ps_[:], W0[:, sl], ALU.add, r=[k_, "W0"], w=[kL, k_]))
            p.act(L_[:], L_[:], AF.Exp, scale=-1.0, r=[kL], w=[kL])
            p.act(L_[:], L_[:], AF.Ln, bias=1.0, r=[kL], w=[kL])
            p.act(L_[:], L_[:], AF.Exp, scale=-1.0, bias=-0.5, r=[kL], w=[kL])
            A_ = Aa[s_]
            kA = K("Aa", s_)
            pt, pk = proj_tm(hidb[0:64, 1, :], "hidb1", a2, "a2", None)
            halves(pt, pk, lambda sl, ps_, k_: p.tt("dve", A_[:, sl], ps_[:], A0[:, sl], ALU.add, r=[k_, "A0"], w=[kA, k_]))
            p.act(A_[:], A_[:], AF.Exp, scale=-1.0, r=[kA], w=[kA])
            p.act(A_[:], A_[:], AF.Ln, bias=1.0, r=[kA], w=[kA])
            p.act(A_[:], A_[:], AF.Exp, scale=-1.0, r=[kA], w=[kA])
            pt, pk = proj_tm(hidb[:, 2, :], "hidb2", g2, "g2", None)
            halves(pt, pk, lambda sl, ps_, k_: p.cp("act", Gg[:, sl], ps_[:], r=[k_], w=["Gg", k_]))
            p.dma("sp", S["Gg"][j * 128:(j + 1) * 128, :], Gg[:], "st_Gg", r=["Gg"], w=[K("S_Gg", j)])

        def P2(j):
            s_ = j % 2
            R_, K_, V_, A_, L_ = R[s_], K0[s_], V32[s_], Aa[s_], LDP[s_]
            kR, kK, kV, kA, kL = K("R", s_), K("K0", s_), K("V32", s_), K("Aa", s_), K("LDP", s_)
            p.tt("dve", KKn[:], K_[:], KKp[:], ALU.mult, r=[kK, "KKp"], w=["KKn"])
            p.act(T3[:], KKn[:], AF.Square, r=["KKn"], w=["T3"])
            p.red(sm[:, 0, :], hn(T3), r=["T3"], w=["sm0"])
            p.ts("dve", sm[:, 0, :], sm[:, 0, :], 1e-24, None, ALU.max, r=["sm0"], w=["sm0"])
            p.act(sm[:, 0, :], sm[:, 0, :], AF.Ln, r=["sm0"], w=["sm0"])
            p.act(sm[:, 0, :], sm[:, 0, :], AF.Exp, scale=-0.5, r=["sm0"], w=["sm0"])
            p.tt("dve", hn(KKn), hn(KKn), sm[:, 0, :].unsqueeze(2).to_broadcast([128, 16, 64]), ALU.mult, r=["KKn", "sm0"], w=["KKn"])
            p.stt("dve", T3[:], A_[:], -1.0, KAp[:], ALU.add, ALU.mult, r=[kA, "KAp"], w=["T3"])
            p.stt("dve", KP[:], T3[:], 1.0, K_[:], ALU.add, ALU.mult, r=["T3", kK], w=["KP"])
            p.tt("pool", Bb[:], KKn[:], A_[:], ALU.mult, r=["KKn", kA], w=["Bb"])
            p.tt("pool", T3[:], R_[:], KP[:], ALU.mult, r=[kR, "KP"], w=["T3"])
            p.tt("pool", T3[:], T3[:], RKp[:], ALU.mult, r=["T3", "RKp"], w=["T3"])
            p.red(sm[:, 1, :], hn(T3), r=["T3"], w=["sm1"])
            p.tt("dve", hn(T3), hn(V_), sm[:, 1, :].unsqueeze(2).to_broadcast([128, 16, 64]), ALU.mult, r=[kV, "sm1"], w=["T3"])
            p.dma("sp", S["BV"][j * 128:(j + 1) * 128, :], T3[:], "st_BV", r=["T3"], w=[K("S_BV", j), "T3dma"])
            pc, pkc = psT[2], [K("psT", 2, 0), K("psT", 2, 1)]
            for half in range(2):
                sl = slice(half * 512, (half + 1) * 512)
                p.mm(pc[half][:], triu[:], L_[:, sl], True, True, r=["triu", kL], w=[pkc[half]])
            halves(pc, pkc, lambda sl, ps_, k_: p.cp("act", C32[:, sl], ps_[:], r=[k_], w=["C32", k_]))
            for half in range(2):
                sl = slice(half * 512, (half + 1) * 512)
                p.mm(pc[half][:], ones[:], L_[:, sl], True, True, r=["ones", kL], w=[pkc[half]])
            for h in range(16):
                p.mm(ps_e[0:64, h:h + 1], L_[:, h * 64:(h + 1) * 64], ones[:, 0:1], True, True, r=[kL, "ones"], w=["ps_e"])
            p.act(ET[:], ps_e[0:64, 0:16], AF.Exp, scale=-1.0, r=["ps_e"], w=["ET", "ps_e"])
            p.dma("sp", S["ET"][j], ET[:], "st_ET", r=["ET"], w=[K("S_ET", j)])
            halves(pc, pkc, lambda sl, ps_, k_: p.tt("dve", T2[:, sl], ps_[:], C32[:, sl], ALU.subtract, r=[k_, "C32", "T2"], w=["T2", k_]))
            p.act(T2[:], T2[:], AF.Exp, scale=-1.0, r=["T2"], w=["T2"])
            p.tt("dve", ob["KG"][:], KP[:], T2[:], ALU.mult, r=["KP", "T2"], w=["o_KG"])
            p.tt("pool", ob["BG"][:], Bb[:], T2[:], ALU.mult, r=["Bb", "T2"], w=["o_BG"])
            p.act(T1[:], C32[:], AF.Exp, scale=-1.0, r=["C32"], w=["T1"])
            p.tt("dve", ob["RT"][:], R_[:], T1[:], ALU.mult, r=[kR, "T1"], w=["o_RT"])
            p.act(T2[:], C32[:], AF.Exp, r=["C32", "T2"], w=["T2"])
            p.tt("dve", ob["KH"][:], KP[:], T2[:], ALU.mult, r=["KP", "T2"], w=["o_KH"])
            p.tt("pool", ob["BH"][:], Bb[:], T2[:], ALU.mult, r=["Bb", "T2"], w=["o_BH"])
            p.tt("dve", T1[:], C32[:], L_[:], ALU.subtract, r=["C32", kL, "T1"], w=["T1"])
            p.act(T1[:], T1[:], AF.Exp, scale=-1.0, r=["T1"], w=["T1"])
            p.tt("pool", ob["KKT"][:], KKn[:], T1[:], ALU.mult, r=["KKn", "T1"], w=["o_KKT"])
            for n_ in ("RT", "KKT", "KH", "BH", "KG", "BG"):
                p.dma("sp", S[n_][j * 128:(j + 1) * 128, :], ob[n_][:], "st_" + n_, r=["o_" + n_], w=[K("S_" + n_, j)])

        P1(0)
        for j in range(NT):
            p.capture()
            P2(j)
            la = p.end_capture()
            lb = []
            if j + 1 < NT:
                p.capture()
                P1(j + 1)
                lb = p.end_capture()
            p.replay_merged(la, lb)
        p.fence([K("S_" + n_, j) for n_ in ("RT", "KKT", "KH", "BH", "KG", "BG", "Vb", "Gg", "BV", "ET") for j in range(NT)])
        p.emit()
    nc.all_engine_barrier()


def stage_rwkv2(p, pre, W, S):
    nc = p.nc
    with ExitStack() as es:
        p.begin(es)
        c = consts(p)
        ident, identb = c["ident"], c["identb"]
        GNg = bc_load(p, "GNg", W["gn_g"])
        GNb = bc_load(p, "GNb", W["gn_b"])
        MASK4 = p.sb("MASK4", [128, 4, 128], F32)
        MASKL = p.sb("MASKL", [128, 4, 128], F32)
        p.op("pool", lambda e: e.memset(MASK4[:], 1.0), w=["MASK4"])
        p.op("pool", lambda e: e.memset(MASKL[:], 1.0), w=["MASKL"])
        for q4 in range(4):
            strict = q4 % 2 == 0
            p.op("pool", (lambda q4, strict: lambda e: e.affine_select(out=MASK4[:, q4, :], in_=MASK4[:, q4, :], pattern=[[1, 128]], compare_op=ALU.is_ge, fill=0.0, base=(-1 if strict else 0), channel_multiplier=-1))(q4, strict), r=["MASK4"], w=["MASK4"])
            p.op("pool", (lambda q4: lambda e: e.affine_select(out=MASKL[:, q4, :], in_=MASKL[:, q4, :], pattern=[[-1, 128]], compare_op=ALU.is_ge, fill=0.0, base=-1, channel_multiplier=1))(q4), r=["MASKL"], w=["MASKL"])
        names = ("RT", "KH", "BH", "KG", "BG", "Vb")
        inb = {n_: [p.sb("i_%s%d" % (n_, j), [128, D], BF16) for j in range(2)] for n_ in names}
        Rhs = [p.sb("Rhs%d" % j, [128, 16, 128], BF16) for j in range(2)]
        Gg = [p.sb("Gg%d" % j, [128, D], F32) for j in range(2)]
        BV = [p.sb("BV%d" % j, [128, D], F32) for j in range(2)]
        ET = [p.sb("ET%d" % j, [64, 16], F32) for j in range(2)]
        KHT = p.sb("KHT", [64, 16, 128], BF16)
        BHT = p.sb("BHT", [64, 16, 128], BF16)
        KR = p.sb("KR", [64, 16, 256], BF16)
        ATB = p.sb("ATB", [128, 16, 512], BF16)
        Lb = p.sb("Lb", [128, 16, 128], BF16)
        Nn = [p.sb("Nn%d" % j, [128, 16, 128], BF16) for j in range(2)]
        NTn = [p.sb("NTn%d" % j, [128, 16, 128], BF16) for j in range(2)]
        Yb = [p.sb("Yb%d" % j, [128, 16, 128], BF16) for j in range(2)]
        PUn = p.sb("PUn", [128, 16, 128], BF16)
        MTp = p.sb("MTp", [64, 16, 64], BF16)
        QT = p.sb("QT", [64, 16, 128], BF16)
        Gf = p.sb("Gf", [64, 16, 64], F32)
        H = p.sb("H", [64, 16, 64], F32)
        HG = p.sb("HG", [64, 16, 64], F32)
        Hb = p.sb("Hb", [64, 16, 64], BF16)
        Tn = [p.sb("Tn%d" % j, [128, D], F32) for j in range(2)]
        sm = p.sb("sm", [128, 6, 16], F32)
        YG = [p.sb("YG%d" % j, [128, D], BF16) for j in range(2)]
        TRb = [p.ps("TRb%d" % j, [128, 1024], BF16) for j in range(2)]
        PB = [p.ps("PB%d" % j, [128, 512]) for j in range(6)]
        nb = [0]

        def bank():
            j = nb[0] % 4
            nb[0] += 1
            return PB[j], K("PB", j)
        PY = [(PB[4], K("PB", 4)), (PB[5], K("PB", 5))]
        pendingB = []
        ntr = [0]
        nev = [0]

        def evq():
            nev[0] += 1
            return "dve" if nev[0] % 3 == 0 else "act"

        p.op("pool", lambda e: e.memset(H[:], 0.0), w=["H"])
        p.op("pool", lambda e: e.memset(Hb[:], 0.0), w=["Hb"])

        def loads(j):
            s = j % 2
            rows = slice(j * 128, (j + 1) * 128)
            for n_ in names:
                p.dma("sp", inb[n_][s][:], S[n_][rows, :], "ld_%s%d" % (n_, s), w=[K("i", n_, s)])
            p.dma("sp", Rhs[s][:, :, 0:64], S["KKT"][rows, :].rearrange("t (h n) -> t h n", h=16), "ld_KKT%d" % s, w=[K("RhsA", s)])
            p.dma("sp", ET[s][:], S["ET"][j], "ld_ET%d" % s, w=[K("ET", s)])
        loads(0)

        for j in range(NT):
            p.capture()
            s = j % 2
            rows = slice(j * 128, (j + 1) * 128)
            if j + 1 < NT:
                loads(j + 1)
            p.dma("sp", Gg[s][:], S["Gg"][rows, :], "ld_Gg%d" % s, w=[K("Gg", s)])
            p.dma("sp", BV[s][:], S["BV"][rows, :], "ld_BV%d" % s, w=[K("BV", s)])
            RT_, KH_, BH_, KG_, BG_, Vb_ = (inb[n_][s] for n_ in names)
            kRT, kKH, kBH, kKG, kBG, kVb = (K("i", n_, s) for n_ in names)
            def transp(src, rk, dst, wk_, pair_view=None):
                tb_ = TRb[ntr[0] % 2]
                tk = K("TRb", ntr[0] % 2)
                ntr[0] += 1
                for a_ in range(8):
                    p.tr(tb_[:, a_ * 128:(a_ + 1) * 128], src(a_), identb[:], r=[rk, "identb"], w=[tk])
                for par in range(2):
                    p.cp(evq(), dst(par), tb_[par * 64:(par + 1) * 64, :].rearrange("p (a t) -> p a t", a=8), r=[tk], w=[wk_, tk])
            ev = lambda t_, lo, hi: (lambda par: t_[:].rearrange("p (a two) t -> p a two t", two=2)[:, :, par, lo:hi])
            transp(lambda a_: KH_[:, a_ * 128:(a_ + 1) * 128], kKH, ev(KHT, 0, 128), "KHT")
            transp(lambda a_: BH_[:, a_ * 128:(a_ + 1) * 128], kBH, ev(BHT, 0, 128), "BHT")
            for hh in range(2):
                tb_ = TRb[ntr[0] % 2]
                tk = K("TRb", ntr[0] % 2)
                ntr[0] += 1
                for h8 in range(8):
                    p.tr(tb_[0:64, h8 * 128:(h8 + 1) * 128], Rhs[s][:, hh * 8 + h8, 0:64], identb[:], r=[K("RhsA", s), "identb"], w=[tk])
                p.cp(evq(), KR[:, hh * 8:(hh + 1) * 8, 0:128], tb_[0:64, :].rearrange("p (h t) -> p h t", h=8), r=[tk], w=["KR", tk])
            transp(lambda a_: RT_[:, a_ * 128:(a_ + 1) * 128], kRT, ev(KR, 128, 256), "KR")
            for h in range(16):
                b_, bk = bank()
                p.mm(b_[:, 0:256], KHT[:, h, :], KR[:, h, :], True, True, r=["KHT", "KR"], w=[bk])
                p.mm(b_[:, 256:512], BHT[:, h, :], KR[:, h, :], True, True, r=["BHT", "KR"], w=[bk])
                p.tt("dve", ATB[:, h, :], b_[:], MASK4[:].rearrange("p a t -> p (a t)"), ALU.mult, r=[bk, "MASK4"], w=[K("ATB", h // 4), bk])
            for g4 in range(4):
                b_, bk = bank()
                for hq in range(4):
                    h = g4 * 4 + hq
                    p.mm(b_[:, hq * 128:(hq + 1) * 128], KR[:, h, 0:128], BHT[:, h, :], True, True, r=["KR", "BHT"], w=[bk])
                p.tt("dve", Lb[:, g4 * 4:(g4 + 1) * 4, :], b_[:].rearrange("p (a t) -> p a t", a=4), MASKL[:], ALU.mult, r=[bk, "MASKL"], w=[K("Lb", g4), bk])
            ycur = 0
            for g4 in range(4):
                hs = slice(g4 * 4, (g4 + 1) * 4)
                p.tt("pool", Yb[0][:, hs, :], identb[:].unsqueeze(1).to_broadcast([128, 4, 128]), ATB[:, hs, 256:384], ALU.subtract,
                     r=["identb", K("ATB", g4)], w=[K("Yb", 0, g4)])
            Ncur = lambda h: Lb[:, h, :]
            NTcur = lambda h: ATB[:, h, 256:384]
            nkey = lambda g4: K("Lb", g4)
            ntkey = lambda g4: K("ATB", g4)
            for lvl in range(6):
                pp_ = lvl % 2
                last = lvl == 5
                for g4 in range(4):
                    hs = slice(g4 * 4, (g4 + 1) * 4)
                    bA, kA = bank()
                    for hq in range(4):
                        h = g4 * 4 + hq
                        p.mm(bA[:, hq * 128:(hq + 1) * 128], NTcur(h), Ncur(h), True, True, r=[nkey(g4), ntkey(g4)], w=[kA])
                    p.cp(evq(), Nn[pp_][:, hs, :], bA[:].rearrange("p (a t) -> p a t", a=4), r=[kA], w=[K("Nn", pp_, g4), kA])
                    if not last:
                        bB, kB = bank()
                        for hq in range(4):
                            h = g4 * 4 + hq
                            p.mm(bB[:, hq * 128:(hq + 1) * 128], Ncur(h), NTcur(h), True, True, r=[nkey(g4), ntkey(g4)], w=[kB])
                        p.cp(evq(), NTn[pp_][:, hs, :], bB[:].rearrange("p (a t) -> p a t", a=4), r=[kB], w=[K("NTn", pp_, g4), kB])
                for g4 in range(4):
                    hs = slice(g4 * 4, (g4 + 1) * 4)
                    bC, kC = bank()
                    p.mm(bC[:], identb[:], Yb[ycur][:, hs, :].rearrange("p a t -> p (a t)"), True, False, r=["identb", K("Yb", ycur, g4)], w=[kC])
                    for hq in range(4):
                        h = g4 * 4 + hq
                        osl = bC[:, hq * 128:(hq + 1) * 128]
                        p.mm(osl, Nn[pp_][:, h, :], Yb[ycur][:, h, :], False, hq == 3, r=[K("Nn", pp_, g4), K("Yb", ycur, g4)], w=[kC])
                    p.cp(evq(), Yb[1 - ycur][:, hs, :], bC[:].rearrange("p (a t) -> p a t", a=4), r=[kC], w=[K("Yb", 1 - ycur, g4), kC])
                ycur = 1 - ycur
                Ncur = (lambda pp_: lambda h: Nn[pp_][:, h, :])(pp_)
                NTcur = (lambda pp_: lambda h: NTn[pp_][:, h, :])(pp_)
                nkey = (lambda pp_: lambda g4: K("Nn", pp_, g4))(pp_)
                ntkey = (lambda pp_: lambda g4: K("NTn", pp_, g4))(pp_)
            Y = Yb[ycur]
            for g8 in range(2):
                b_, bk = bank()
                for h8 in range(8):
                    h = g8 * 8 + h8
                    p.mm(b_[:, h8 * 64:(h8 + 1) * 64], ATB[:, h, 0:128], Vb_[:, h * 64:(h + 1) * 64], True, True, r=[K("ATB", h // 4), kVb], w=[bk])
                p.cp(evq(), Rhs[s][:, g8 * 8:(g8 + 1) * 8, 64:128], b_[:].rearrange("p (h n) -> p h n", h=8), r=[bk], w=[K("RhsB", s, g8), bk])
            for g4 in range(4):
                b_, bk = bank()
                for hq in range(4):
                    h = g4 * 4 + hq
                    p.mm(b_[:, hq * 128:(hq + 1) * 128], Y[:, h, :], Rhs[s][:, h, :], True, True, r=[K("Yb", ycur, g4), K("RhsA", s), K("RhsB", s, h // 8)], w=[bk])
                p.op("act", (lambda b_, g4: lambda e: e.mul(PUn[:, g4 * 4:(g4 + 1) * 4, :], b_[:].rearrange("p (a t) -> p a t", a=4), -1.0))(b_, g4), r=[bk], w=[K("PUn", g4), bk])
            for g8 in range(2):
                b_, bk = bank()
                for h8 in range(8):
                    h = g8 * 8 + h8
                    p.mm(b_[0:64, h8 * 64:(h8 + 1) * 64], PUn[:, h, 0:64], BG_[:, h * 64:(h + 1) * 64], True, True, r=[K("PUn", h // 4), kBG], w=[bk])
                p.cp(evq(), MTp[:, g8 * 8:(g8 + 1) * 8, :], b_[0:64, :].rearrange("p (h n) -> p h n", h=8), r=[bk], w=[K("MTp", g8), bk])
            for g4 in range(4):
                b_, bk = bank()
                for hq in range(4):
                    h = g4 * 4 + hq
                    osl = b_[0:64, hq * 128:(hq + 1) * 128]
                    p.mm(osl, identb[0:64, 0:64], KR[:, h, 128:256], True, False, r=["identb", "KR"], w=[bk])
                    p.mm(osl, PUn[:, h, 0:64], ATB[:, h, 384:512], False, True, r=[K("PUn", g4), K("ATB", g4)], w=[bk])
                p.cp(evq(), QT[:, g4 * 4:(g4 + 1) * 4, :], b_[0:64, :].rearrange("p (a t) -> p a t", a=4), r=[bk], w=[K("QT", g4), bk])
            for g8 in range(2):
                b_, bk = bank()
                for h8 in range(8):
                    h = g8 * 8 + h8
                    osl = b_[0:64, h8 * 64:(h8 + 1) * 64]
                    p.mm(osl, KG_[:, h * 64:(h + 1) * 64], Vb_[:, h * 64:(h + 1) * 64], True, False, r=[kKG, kVb], w=[bk])
                    p.mm(osl, BG_[:, h * 64:(h + 1) * 64], PUn[:, h, 64:128], False, True, r=[kBG, K("PUn", h // 4)], w=[bk])
                p.cp(evq(), Gf[:, g8 * 8:(g8 + 1) * 8, :], b_[0:64, :].rearrange("p (h n) -> p h n", h=8), r=[bk], w=[K("Gf", g8), bk])
            ybanks = []
            for g8 in range(2):
                b_, bk = PY[g8]
                ybanks.append((b_, bk))
                for h8 in range(8):
                    h = g8 * 8 + h8
                    osl = b_[:, h8 * 64:(h8 + 1) * 64]
                    p.mm(osl, ATB[:, h, 128:256], Vb_[:, h * 64:(h + 1) * 64], True, False, r=[K("ATB", h // 4), kVb], w=[bk])
                    p.mm(osl, ATB[:, h, 384:512], PUn[:, h, 64:128], False, False, r=[K("ATB", h // 4), K("PUn", h // 4)], w=[bk])
                    p.mm(osl, QT[:, h, :], Hb[:, h, :], False, True, r=[K("QT", h // 4), "Hb"], w=[bk])
            hb_ = []
            for g8 in range(2):
                b_, bk = bank()
                hb_.append((b_, bk))
                for h8 in range(8):
                    h = g8 * 8 + h8
                    p.mm(b_[0:64, h8 * 64:(h8 + 1) * 64], MTp[:, h, :], Hb[:, h, :], True, True, r=[K("MTp", g8), "Hb"], w=[bk])
            p.tt("pool", HG[:], H[:], ET[s][:].unsqueeze(2).to_broadcast([64, 16, 64]), ALU.mult, r=["H", K("ET", s)], w=["HG"])
            p.tt("pool", HG[:], HG[:], Gf[:], ALU.add, r=["HG", K("Gf", 0), K("Gf", 1)], w=["HG"])
            for g8 in range(2):
                b_, bk = hb_[g8]
                p.tt("dve", H[:, g8 * 8:(g8 + 1) * 8, :], b_[0:64, :].rearrange("p (h n) -> p h n", h=8), HG[:, g8 * 8:(g8 + 1) * 8, :], ALU.add, r=[bk, "HG"], w=["H", bk])
            p.cp("act", Hb[:], H[:], r=["H"], w=["Hb"])
            la = p.end_capture()
            nf = min(len(la), 100000)
            p.replay_merged(la[:nf], pendingB)
            p.replay_merged(la[nf:])
            p.capture()
            T_ = Tn[s]
            kT = K("Tn", s)
            for g8 in range(2):
                b_, bk = ybanks[g8]
                sl = slice(g8 * 512, (g8 + 1) * 512)
                p.red(sm[:, 0, g8 * 8:(g8 + 1) * 8], b_[:].rearrange("p (h n) -> p h n", h=8), r=[bk], w=["sm0", bk])
                p.act(T_[:, sl], b_[:], AF.Square, r=[bk], w=[kT, bk])
            p.red(sm[:, 1, :], T_[:].rearrange("p (h n) -> p h n", h=16), r=[kT], w=["sm1"])
            p.ts("dve", sm[:, 0, :], sm[:, 0, :], 1.0 / 64, None, ALU.mult, r=["sm0"], w=["sm0"])
            p.tt("dve", sm[:, 2, :], sm[:, 0, :], sm[:, 0, :], ALU.mult, r=["sm0"], w=["sm2"])
            p.stt("dve", sm[:, 3, :], sm[:, 1, :], 1.0 / 64, sm[:, 2, :], ALU.mult, ALU.subtract, r=["sm1", "sm2"], w=["sm3"])
            p.ts("dve", sm[:, 3, :], sm[:, 3, :], GN_EPS, None, ALU.add, r=["sm3"], w=["sm3"])
            p.act(sm[:, 3, :], sm[:, 3, :], AF.Ln, r=["sm3"], w=["sm3"])
            p.act(sm[:, 3, :], sm[:, 3, :], AF.Exp, scale=-0.5, r=["sm3"], w=["sm3"])
            for g8 in range(2):
                b_, bk = ybanks[g8]
                sl = slice(g8 * 512, (g8 + 1) * 512)
                p.tt("dve", T_[:, sl].rearrange("p (h n) -> p h n", h=8), b_[:].rearrange("p (h n) -> p h n", h=8),
                     sm[:, 0, g8 * 8:(g8 + 1) * 8].unsqueeze(2).to_broadcast([128, 8, 64]), ALU.subtract, r=[bk, "sm0"], w=[kT, bk])
            p.tt("dve", T_[:].rearrange("p (h n) -> p h n", h=16), T_[:].rearrange("p (h n) -> p h n", h=16),
                 sm[:, 3, :].unsqueeze(2).to_broadcast([128, 16, 64]), ALU.mult, r=[kT, "sm3"], w=[kT])
            p.tt("pool", T_[:], T_[:], GNg[:], ALU.mult, r=[kT, "GNg"], w=[kT])
            p.tt("pool", T_[:], T_[:], GNb[:], ALU.add, r=[kT, "GNb"], w=[kT])
            p.tt("dve", T_[:], T_[:], BV[s][:], ALU.add, r=[kT, K("BV", s)], w=[kT])
            p.tt("pool", YG[s][:], T_[:], Gg[s][:], ALU.mult, r=[kT, K("Gg", s)], w=[K("YG", s)])
            p.dma("sp", S["YG"][rows, :], YG[s][:], "st_YG%d" % s, r=[K("YG", s)], w=[K("S_YG", j)])
            pendingB = p.end_capture()
        p.replay_merged(pendingB)
        p.fence([K("S_YG", j) for j in range(NT)])
        p.emit()
    nc.all_engine_barrier()


def stage_rwkv3(p, pre, x_d, W, S, lng_d, lnb_d, h_out, acc=None):
    nc = p.nc
    with ExitStack() as es:
        p.begin(es)
        c = consts(p)
        identb = c["identb"]
        wo = p.sb("wo3", [128, 8, D], BF16)
        p.dma("pool", wo[:], W["wo"].rearrange("(c p) f -> p c f", p=128), "ld_wo3", w=["wo"])
        gbc = p.sb(pre + "gbc", [128, D], F32)
        bbc = p.sb(pre + "bbc", [128, D], F32)
        p.dma("sp", gbc[:], lng_d.partition_broadcast(128), pre + "gbc", w=[K(pre, "gbc")])
        p.dma("sp", bbc[:], lnb_d.partition_broadcast(128), pre + "bbc", w=[K(pre, "bbc")])
        yg = [p.sb("yg%d" % j, [128, D], BF16) for j in range(2)]
        ygT = [p.sb("ygT%d" % j, [128, 8, 128], BF16) for j in range(2)]
        xt = [p.sb("x3_%d" % j, [128, D], F32) for j in range(2)]
        z4 = [p.sb("z43_%d" % j, [128, 4, D], F32) for j in range(2)]
        sq = [p.sb("sq3_%d" % j, [128, D], F32) for j in range(2)]
        st4 = [p.sb("st43_%d" % j, [128, 8, 4], F32) for j in range(2)]
        TRb = [p.ps("TR3_%d" % j, [128, 1024], BF16) for j in range(2)]
        PZ = [p.ps("PZ%d" % j, [128, 512]) for j in range(4)]
        outk = []
        pend = []
        for gi in range(NT // 4):
            p.capture()
            zg = z4[gi % 2]
            zs, zks, outs = [], [], []
            for t in range(4):
                i = gi * 4 + t
                s = i % 2
                rows = slice(i * 128, (i + 1) * 128)
                p.dma("sp", yg[s][:], S["YG"][rows, :], "ld_yg%d" % s, w=[K("yg", s)])
                p.dma("sp", xt[s][:], x_d[rows, :], "ld_x3%d" % s, w=[K("x3", s)])
                for c8 in range(8):
                    p.tr(TRb[s][:, c8 * 128:(c8 + 1) * 128], yg[s][:, c8 * 128:(c8 + 1) * 128], identb[:], r=[K("yg", s), "identb"], w=[K("TR3", s)])
                p.cp("act", ygT[s][:], TRb[s][:].rearrange("p (c t) -> p c t", c=8), r=[K("TR3", s)], w=[K("ygT", s), K("TR3", s)])
                zk = K("z43", gi % 2, t)
                for half in range(2):
                    pz = PZ[(i * 2 + half) % 4]
                    kz = K("PZ", (i * 2 + half) % 4)
                    for c8 in range(8):
                        p.mm(pz[:], ygT[s][:, c8, :], wo[:, c8, half * 512:(half + 1) * 512], c8 == 0, c8 == 7, r=[K("ygT", s), "wo"], w=[kz])
                    sl = slice(half * 512, (half + 1) * 512)
                    p.stt("dve", zg[:, t, sl], xt[s][:, sl], ALPHA, pz[:], ALU.mult, ALU.add, r=[kz, K("x3", s)], w=[zk, kz])
                zs.append(zg[:, t, :])
                zks.append(zk)
                outs.append(h_out[rows, :] if acc is None else ("sb", acc[:, i, :]))
            la = p.end_capture()
            p.replay_merged(la, pend)
            p.capture()
            outk += ln_group(p, pre, zs, zks, gbc, bbc, outs, gi, st4[gi % 2], sq)
            pend = p.end_capture()
        p.replay_merged(pend)
        p.fence(outk)
        p.emit()
    nc.all_engine_barrier()


RW_SHAPES = (("mix", [6, D]), ("wr", [D, D]), ("wk", [D, D]), ("wv", [D, D]), ("wo", [D, D]), ("w0", [D]), ("w1", [D, 64]), ("w2", [64, D]),
             ("a0", [D]), ("a1", [D, 64]), ("a2", [64, D]), ("g1", [D, 128]), ("g2", [128, D]), ("kk", [D]), ("ka", [D]), ("rk", [16, 64]),
             ("gn_g", [D]), ("gn_b", [D]))
BNAMES = ("RT", "KKT", "KH", "BH", "KG", "BG", "Vb", "YG")


def build_full():
    nc = bass.Bass("TRN2", target_bir_lowering=False)

    def dt(n, s, kind="ExternalInput", t=F32):
        if kind is None:
            return nc.dram_tensor(n, s, t).ap()
        return nc.dram_tensor(n, s, t, kind=kind).ap()
    x_d = dt("x", [T, D])
    xT_d = dt("xT", [D, T])
    W = {n_: dt("rw_" + n_, shp) for n_, shp in RW_SHAPES}
    win = dt("fx_w_in", [D, 3088])
    bf = dt("fx_b_f", [16])
    fwo = dt("fx_wo", [D, D])
    rw = dt("router_w", [D, 16])
    rb = dt("router_bias", [16])
    wg = dt("moe_w_gate", [2, 16, D, 512])
    wu = dt("moe_w_up", [2, 16, D, 512])
    wd = dt("moe_w_down", [2, 16, 512, D])
    lng = dt("ln_g", [2, 2, D])
    lnb = dt("ln_b", [2, 2, D])
    out = dt("out", [T, D], "ExternalOutput")
    S = {n_: dt("S_" + n_, [T, D], None, BF16) for n_ in BNAMES}
    S["Gg"] = dt("S_Gg", [T, D], None)
    S["BV"] = dt("S_BV", [T, D], None)
    S["ET"] = dt("S_ET", [NT, 64, 16], None)
    h1 = dt("S_h1", [T, D], None)
    h2 = dt("S_h2", [T, D], None)
    h3 = dt("S_h3", [T, D], None)
    p = Prog(nc)
    stage_rwkv1(p, "r1", xT_d, W, S)
    stage_rwkv2(p, "r2", W, S)
    with ExitStack() as ea:
        p.es = ea
        acc0 = p.sb("acc_r", [128, NT, D], F32)
        stage_rwkv3(p, "r3", x_d, W, S, lng[0, 0], lnb[0, 0], h1, acc=acc0)
        stage_moe(p, "m0", h1, h2, rw, rb, wg[0], wu[0], wd[0], lng[0, 1], lnb[0, 1], acc=acc0)
    with ExitStack() as eb:
        p.es = eb
        acc1 = p.sb("acc_f", [128, NT, D], F32)
        stage_fox(p, "fx", h2, h3, win, bf, fwo, lng[1, 0], lnb[1, 0], acc=acc1)
        stage_moe(p, "m1", h3, out, rw, rb, wg[1], wu[1], wd[1], lng[1, 1], lnb[1, 1], acc=acc1)
    return nc


def kernel(**inputs):
    x = np.asarray(inputs["x"], dtype=np.float32)
    B = x.shape[0]
    shared = {}
    for n_, _ in RW_SHAPES:
        shared["rw_" + n_] = np.ascontiguousarray(np.asarray(inputs["rw_" + n_], dtype=np.float32)[0])
    shared["fx_w_in"] = np.ascontiguousarray(np.asarray(inputs["fx_w_in"], dtype=np.float32)[0])
    shared["fx_b_f"] = np.ascontiguousarray(np.asarray(inputs["fx_b_f"], dtype=np.float32)[0])
    shared["fx_wo"] = np.ascontiguousarray(np.asarray(inputs["fx_wo"], dtype=np.float32)[0])
    for n_ in ("router_w", "router_bias", "moe_w_gate", "moe_w_up", "moe_w_down", "ln_g", "ln_b"):
        shared[n_] = np.ascontiguousarray(np.asarray(inputs[n_], dtype=np.float32))
    nc = build_full()
    in_maps = []
    for b in range(B):
        m = dict(shared)
        m["x"] = np.ascontiguousarray(x[b])
        m["xT"] = np.ascontiguousarray(x[b].T)
        in_maps.append(m)
    res = run_bass_kernel_spmd(nc, in_maps, core_ids=list(range(B)))
    return np.stack([np.asarray(r["out"], dtype=np.float32) for r in res.results], axis=0)
```

```python
import numpy as np
from contextlib import ExitStack
import concourse.bass as bass
import concourse.mybir as mybir
from concourse.bass_utils import run_bass_kernel_spmd

F32 = mybir.dt.float32
BF16 = mybir.dt.bfloat16
AF = mybir.ActivationFunctionType
ALU = mybir.AluOpType
AX = mybir.AxisListType


STRICT = False


class Ins:
    __slots__ = ("q", "fn", "deps", "sem", "val", "dkey", "need", "n")

    def __init__(self, q, fn, dkey):
        self.q = q
        self.fn = fn
        self.dkey = dkey
        self.deps = {}
        self.need = False
        self.sem = None
        self.val = 0


class Prog:
    ENG = ("pe", "act", "dve", "pool", "sp")

    def __init__(self, nc):
        self.nc = nc
        self.begin(None)
        self.pool_es = ExitStack()
        self.esems = None
        self.dpool = {}
        self.base = {}

    def begin(self, es):
        self.es = es
        self.streams = {e: [] for e in self.ENG}
        self.lastw = {}
        self.readers = {}
        self.n = 0

    _uid = [0]

    def sb(self, name, shape, dt):
        Prog._uid[0] += 1
        return self.es.enter_context(self.nc.sbuf_tensor("%s_%d" % (name, Prog._uid[0]), list(shape), dt))

    def ps(self, name, shape, dt=F32):
        Prog._uid[0] += 1
        return self.es.enter_context(self.nc.psum_tensor("%s_%d" % (name, Prog._uid[0]), list(shape), dt))

    def capture(self):
        self._cap = []

    def end_capture(self):
        lst, self._cap = self._cap, None
        return lst

    def replay_merged(self, *lists):
        pos = [0] * len(lists)
        total = sum(len(l) for l in lists)
        for _ in range(total):
            best, bf = None, 2.0
            for k, l in enumerate(lists):
                if pos[k] < len(l):
                    f = pos[k] / len(l)
                    if f < bf:
                        best, bf = k, f
            a = lists[best][pos[best]]
            pos[best] += 1
            self.op(*a[0], **a[1])

    def op(self, q, fn, r=(), w=(), dma=None):
        if getattr(self, "_cap", None) is not None:
            self._cap.append(((q, fn), dict(r=list(r), w=list(w), dma=dma)))
            return None
        ins = Ins(q, fn, dma)
        ins.n = self.n
        self.n += 1
        for k in r:
            d = self.lastw.get(k)
            if d is not None:
                ins.deps[d] = "raw"
        for k in w:
            d = self.lastw.get(k)
            if d is not None and d not in ins.deps:
                ins.deps[d] = "waw"
            for d in self.readers.get(k, ()):
                if d not in ins.deps and d is not ins:
                    ins.deps[d] = "war"
        for k in w:
            self.lastw[k] = ins
            self.readers[k] = []
        for k in r:
            self.readers.setdefault(k, []).append(ins)
        self.streams[q].append(ins)
        return ins

    @staticmethod
    def _skip(i, d, kind):
        if d.dkey is not None:
            return False
        if i.dkey is None and d.q == i.q:
            if i.q == "pe":
                return True
            return False if STRICT else kind != "raw"
        return False


    def mm(self, out, lhsT, rhs, start=True, stop=True, r=(), w=()):
        return self.op("pe", lambda e: e.matmul(out, lhsT, rhs, start=start, stop=stop), r=r, w=w)

    def tr(self, out, in_, ident, r=(), w=()):
        return self.op("pe", lambda e: e.transpose(out, in_, ident), r=r, w=w)

    def act(self, out, in_, func, r=(), w=(), bias=None, scale=None):
        kw = {}
        if bias is not None:
            kw["bias"] = bias
        if scale is not None:
            kw["scale"] = scale
        return self.op("act", lambda e: e.activation(out, in_, func, **kw), r=r, w=w)

    def tt(self, q, out, in0, in1, op, r=(), w=()):
        return self.op(q, lambda e: e.tensor_tensor(out, in0, in1, op), r=r, w=w)

    def ts(self, q, out, in0, s1, s2, op0, op1=None, r=(), w=()):
        if op1 is None:
            return self.op(q, lambda e: e.tensor_scalar(out, in0, s1, None, op0), r=r, w=w)
        return self.op(q, lambda e: e.tensor_scalar(out, in0, s1, s2, op0, op1), r=r, w=w)

    def stt(self, q, out, in0, scalar, in1, op0, op1, r=(), w=()):
        return self.op(q, lambda e: e.scalar_tensor_tensor(out, in0, scalar, in1, op0, op1), r=r, w=w)

    def cp(self, q, out, in_, r=(), w=()):
        if q == "act":
            return self.op(q, lambda e: e.copy(out, in_), r=r, w=w)
        return self.op(q, lambda e: e.tensor_copy(out, in_), r=r, w=w)

    def red(self, out, in_, op=None, r=(), w=()):
        op = op or ALU.add
        return self.op("dve", lambda e: e.tensor_reduce(out, in_, AX.X, op), r=r, w=w)

    def rcp(self, out, in_, r=(), w=()):
        return self.op("dve", lambda e: e.reciprocal(out, in_), r=r, w=w)

    def dma(self, q, out, in_, dkey, r=(), w=(), slow=False):
        if slow:
            return self.op(q, lambda e: e.dma_start(out=out, in_=in_, allow_slow_non_contiguous=True), r=r, w=w, dma=dkey)
        return self.op(q, lambda e: e.dma_start(out=out, in_=in_), r=r, w=w, dma=dkey)

    def fence(self, keys):
        return self.op("sp", None, r=keys)

    def sem(self, name):
        Prog._uid[0] += 1
        return self.es.enter_context(self.nc.semaphore("%s_%d" % (name, Prog._uid[0])))

    def emit(self):
        nc = self.nc
        es = self.es
        for q in self.ENG:
            for i in self.streams[q]:
                for d, kind in i.deps.items():
                    if not self._skip(i, d, kind):
                        d.need = True
        if self.esems is None:
            self.esems = {}
            for q in ("pe", "act", "dve", "pool"):
                Prog._uid[0] += 1
                self.esems[q] = self.pool_es.enter_context(nc.semaphore("s_%s_%d" % (q, Prog._uid[0])))
                self.base[id(self.esems[q])] = 0
        sems = self.esems
        dsem = {}
        dcnt = {}
        nused = {}
        for q in self.ENG:
            cnt = self.base[id(sems[q])] if q in sems else 0
            for i in self.streams[q]:
                if i.dkey is not None:
                    if i.dkey not in dsem:
                        dp = self.dpool.setdefault(q, [])
                        used = nused.setdefault(q, 0)
                        nused[q] = used + 1
                        if used >= len(dp):
                            Prog._uid[0] += 1
                            sm_ = self.pool_es.enter_context(nc.semaphore("d%s_%d" % (q, Prog._uid[0])))
                            dp.append(sm_)
                            self.base[id(sm_)] = 0
                        dsem[i.dkey] = dp[used]
                        dcnt[i.dkey] = self.base[id(dp[used])]
                    dcnt[i.dkey] += 16
                    i.sem = dsem[i.dkey]
                    i.val = dcnt[i.dkey]
                elif i.need:
                    cnt += 1
                    i.sem = sems[q]
                    i.val = cnt
            if q in sems:
                self.base[id(sems[q])] = cnt
        for k_, sm_ in dsem.items():
            self.base[id(sm_)] = dcnt[k_]
        block = es.enter_context(nc.Block())

        def run(q, eng):
            waited = {}
            for i in self.streams[q]:
                for d, kind in i.deps.items():
                    if self._skip(i, d, kind):
                        continue
                    sid = id(d.sem)
                    if waited.get(sid, 0) >= d.val:
                        continue
                    eng.wait_ge(d.sem, d.val)
                    waited[sid] = d.val
                if i.fn is None:
                    continue
                if i.dkey is not None and i.val - 16 > 0:
                    sid = id(i.sem)
                    if waited.get(sid, 0) < i.val - 16:
                        eng.wait_ge(i.sem, i.val - 16)
                        waited[sid] = i.val - 16
                bi = i.fn(eng)
                if i.dkey is not None:
                    bi.then_inc(i.sem, 16)
                elif i.need:
                    bi.then_inc(i.sem, 1)

        @block.tensor
        def _(e):
            run("pe", e)

        @block.scalar
        def _(e):
            run("act", e)

        @block.vector
        def _(e):
            run("dve", e)

        @block.gpsimd
        def _(e):
            run("pool", e)

        @block.sync
        def _(e):
            run("sp", e)


T = 2048
D = 1024
NT = 16
ALPHA = 4.0 ** 0.25
LN_EPS = 1e-5
GN_EPS = 64e-5


def K(name, *idx):
    return (name,) + idx


def consts(p):
    c = {}
    ident = p.sb("ident", [128, 128], F32)
    identb = p.sb("identb", [128, 128], BF16)
    p.op("pool", lambda e: e.memset(ident[:], 0.0), w=["ident"])
    p.op("pool", lambda e: e.affine_select(out=ident[:], in_=ident[:], pattern=[[-1, 128]],
                                           compare_op=ALU.not_equal, fill=1.0, base=0, channel_multiplier=1),
         r=["ident"], w=["ident"])
    p.op("pool", lambda e: e.tensor_copy(identb[:], ident[:]), r=["ident"], w=["identb"])
    c["ident"] = ident
    c["identb"] = identb
    return c


def layer_norm_tiles(p, pre, acc, g_d, b_d, out_d, tmp):
    nc = p.nc
    gbc = p.sb(pre + "gbc", [128, D], F32)
    bbc = p.sb(pre + "bbc", [128, D], F32)
    p.op("sp", lambda e: e.dma_start(out=gbc[:], in_=g_d.partition_broadcast(128)), w=[K(pre, "gbc")], dma=pre + "gbc")
    p.op("sp", lambda e: e.dma_start(out=bbc[:], in_=b_d.partition_broadcast(128)), w=[K(pre, "bbc")], dma=pre + "bbc")
    s1 = p.sb(pre + "s1", [128, NT], F32)
    s2 = p.sb(pre + "s2", [128, NT], F32)
    st = p.sb(pre + "st", [128, 4, NT], F32)
    for i in range(NT):
        p.op("dve", (lambda i: lambda e: e.reduce_sum(s1[:, i:i + 1], acc[:, i, :], axis=AX.X))(i),
             r=[K("acc", i)], w=[K(pre, "s1", i)])
        sl = tmp[i % 2]
        p.op("act", (lambda i, sl: lambda e: e.activation(sl[:], acc[:, i, :], AF.Square))(i, sl),
             r=[K("acc", i)], w=[K(pre, "sq", i % 2)])
        p.op("dve", (lambda i, sl: lambda e: e.reduce_sum(s2[:, i:i + 1], sl[:], axis=AX.X))(i, sl),
             r=[K(pre, "sq", i % 2)], w=[K(pre, "s2", i)])
    allk1 = [K(pre, "s1", i) for i in range(NT)]
    allk2 = [K(pre, "s2", i) for i in range(NT)]
    mean, m2, var, rstd = st[:, 0, :], st[:, 1, :], st[:, 2, :], st[:, 3, :]
    p.op("dve", lambda e: e.tensor_scalar(mean, s1[:], 1.0 / D, None, ALU.mult), r=allk1, w=[K(pre, "mean")])
    p.op("dve", lambda e: e.tensor_tensor(m2, mean, mean, ALU.mult), r=[K(pre, "mean")], w=[K(pre, "m2")])
    p.op("dve", lambda e: e.scalar_tensor_tensor(var, s2[:], 1.0 / D, m2, ALU.mult, ALU.subtract),
         r=allk2 + [K(pre, "m2")], w=[K(pre, "var")])
    p.op("dve", lambda e: e.tensor_scalar(var, var, LN_EPS, None, ALU.add), r=[K(pre, "var")], w=[K(pre, "var")])
    p.op("act", lambda e: e.activation(var, var, AF.Sqrt), r=[K(pre, "var")], w=[K(pre, "var")])
    p.op("dve", lambda e: e.reciprocal(rstd, var), r=[K(pre, "var")], w=[K(pre, "rstd")])
    p.op("dve", lambda e: e.scalar_tensor_tensor(m2, mean, -1.0, rstd, ALU.mult, ALU.mult),
         r=[K(pre, "mean"), K(pre, "rstd")], w=[K(pre, "m2")])
    for i in range(NT):
        p.op("act", (lambda i: lambda e: e.activation(acc[:, i, :], acc[:, i, :], AF.Identity,
                                                      bias=m2[:, i:i + 1], scale=rstd[:, i:i + 1]))(i),
             r=[K("acc", i), K(pre, "m2"), K(pre, "rstd")], w=[K("acc", i)])
        p.op("dve", (lambda i: lambda e: e.tensor_tensor(acc[:, i, :], acc[:, i, :], gbc[:], ALU.mult))(i),
             r=[K("acc", i), K(pre, "gbc")], w=[K("acc", i)])
        p.op("dve" if i % 2 == 0 else "pool", (lambda i: lambda e: e.tensor_tensor(acc[:, i, :], acc[:, i, :], bbc[:], ALU.add))(i),
             r=[K("acc", i), K(pre, "bbc")], w=[K("acc", i)])
        p.op("sp", (lambda i: lambda e: e.dma_start(out=out_d[i * 128:(i + 1) * 128, :], in_=acc[:, i, :]))(i),
             r=[K("acc", i)], w=[K(pre, "out", i)], dma=pre + "st%d" % (i % 4))
    return [K(pre, "out", i) for i in range(NT)]


def stage_moe(p, pre, h_in, h_out, rw_d, rb_d, wg_d, wu_d, wd_d, lng_d, lnb_d, acc=None):
    es = ExitStack()
    with es:
        p.begin(es)
        c = consts(p)
        preloaded = acc is not None
        if not preloaded:
            acc = p.sb(pre + "acc", [128, NT, D], F32)
        hFM = p.sb(pre + "hFM", [128, 8, T], BF16)
        hF32 = [p.sb(pre + "hF32_%d" % j, [128, 8, 128], F32) for j in range(2)]
        rw = p.sb(pre + "rw", [128, 8, 16], F32)
        rbb = p.sb(pre + "rbb", [128, 16], F32)
        lg = p.sb(pre + "lg", [128, NT, 16], F32)
        tmp = [p.sb(pre + "tmp%d" % j, [128, D], F32) for j in range(2)]
        ps_tr = [p.ps(pre + "ps_tr%d" % j, [128, 512]) for j in range(2)]
        ps_g = [p.ps(pre + "ps_g%d" % j, [128, 512]) for j in range(2)]
        ps_u = [p.ps(pre + "ps_u%d" % j, [128, 512]) for j in range(2)]
        ps_d = [p.ps(pre + "ps_d%d" % j, [128, 512]) for j in range(2)]
        wg = [p.sb(pre + "wg%d" % j, [128, 8, 512], BF16) for j in range(2)]
        wu = [p.sb(pre + "wu%d" % j, [128, 8, 512], BF16) for j in range(2)]
        wd = [p.sb(pre + "wd%d" % j, [128, 4, 1024], BF16) for j in range(2)]
        hid = [p.sb(pre + "hid%d" % j, [128, 4, 512], BF16) for j in range(2)]
        sg = [p.sb(pre + "sg%d" % j, [128, 512], F32) for j in range(2)]

        for i in range(NT if not preloaded else 0):
            p.op("sp", (lambda i: lambda e: e.dma_start(out=acc[:, i, :], in_=h_in[i * 128:(i + 1) * 128, :]))(i),
                 w=[K("acc", i)], dma=pre + "ld%d" % (i % 4))
        p.op("sp", lambda e: e.dma_start(out=rw[:], in_=rw_d.rearrange("(c p) e -> p c e", p=128)), w=[K(pre, "rw")], dma=pre + "rw")
        p.op("sp", lambda e: e.dma_start(out=rbb[:], in_=rb_d.partition_broadcast(128)), w=[K(pre, "rbb")], dma=pre + "rbb")

        def load_w(e_, after=()):
            s = e_ % 2
            p.op("pool", lambda e: e.dma_start(out=wg[s][:], in_=wg_d[e_].rearrange("(c p) f -> p c f", p=128)),
                 r=list(after), w=[K(pre, "wg", s)], dma=pre + "wg%d" % s)
            p.op("pool", lambda e: e.dma_start(out=wu[s][:], in_=wu_d[e_].rearrange("(c p) f -> p c f", p=128)),
                 r=list(after), w=[K(pre, "wu", s)], dma=pre + "wu%d" % s)
            p.op("pool", lambda e: e.dma_start(out=wd[s][:], in_=wd_d[e_].rearrange("(c p) f -> p c f", p=128)),
                 r=list(after), w=[K(pre, "wd", s)], dma=pre + "wd%d" % s)
        load_w(0)
        load_w(1)

        for i in range(NT):
            for half in range(2):
                pt = ps_tr[half]
                for cc in range(4):
                    c8 = half * 4 + cc
                    p.op("pe", (lambda i, c8, cc, pt: lambda e: e.transpose(pt[:, cc * 128:(cc + 1) * 128], acc[:, i, c8 * 128:(c8 + 1) * 128], c["ident"][:]))(i, c8, cc, pt),
                         r=[K("acc", i), "ident"], w=[K(pre, "ps_tr", half)])
                f32t = hF32[i % 2]
                p.op("act", (lambda i, half, pt, f32t: lambda e: e.copy(f32t[:, half * 4:(half + 1) * 4, :], pt[:].rearrange("p (c t) -> p c t", c=4)))(i, half, pt, f32t),
                     r=[K(pre, "ps_tr", half)], w=[K(pre, "hF32", i % 2, half), K(pre, "ps_tr", half)])
                p.op("dve", (lambda i, half, pt: lambda e: e.tensor_copy(hFM[:, half * 4:(half + 1) * 4, i * 128:(i + 1) * 128], pt[:].rearrange("p (c t) -> p c t", c=4)))(i, half, pt),
                     r=[K(pre, "ps_tr", half)], w=[K(pre, "hFM", i // 4), K(pre, "ps_tr", half)])
            pl = ps_d[i % 2]
            for c8 in range(8):
                p.op("pe", (lambda i, c8, pl: lambda e: e.matmul(pl[:, 0:16], hF32[i % 2][:, c8, :], rw[:, c8, :], start=(c8 == 0), stop=(c8 == 7)))(i, c8, pl),
                     r=[K(pre, "hF32", i % 2, c8 // 4), K(pre, "rw")], w=[K(pre, "ps_d", i % 2)])
            p.op("act", (lambda i, pl: lambda e: e.copy(lg[:, i, :], pl[:, 0:16]))(i, pl), r=[K(pre, "ps_d", i % 2)], w=[K(pre, "lg")])
            p.op("act", (lambda i: lambda e: e.mul(acc[:, i, :], acc[:, i, :], ALPHA))(i), r=[K("acc", i)], w=[K("acc", i)])

        S = p.sb(pre + "S", [128, NT, 16], F32)
        SS = p.sb(pre + "SS", [128, NT, 16], F32)
        CMP = p.sb(pre + "CMP", [128, NT * 4, 4, 4], F32)
        RK = p.sb(pre + "RK", [128, NT * 4, 4], F32)
        GS = p.sb(pre + "GS", [128, NT, 4], F32)
        GM = p.sb(pre + "GM", [128, NT], F32)
        G = p.sb(pre + "G", [128, NT, 16], F32)
        kr = K(pre, "router")
        p.op("act", lambda e: e.activation(S[:], lg[:], AF.Sigmoid), r=[K(pre, "lg")], w=[K(pre, "S")])
        p.op("dve", lambda e: e.tensor_tensor(SS[:], S[:], rbb[:].unsqueeze(1).to_broadcast([128, NT, 16]), ALU.add),
             r=[K(pre, "S"), K(pre, "rbb")], w=[kr])
        ssv = SS[:].rearrange("p i (g a) -> p (i g) a", g=4)
        in_b = ssv.unsqueeze(2).to_broadcast([128, NT * 4, 4, 4])
        in_a = ssv.unsqueeze(3).to_broadcast([128, NT * 4, 4, 4])
        p.op("dve", lambda e: e.tensor_tensor(CMP[:], in_b, in_a, ALU.is_gt), r=[kr], w=[kr])
        p.op("dve", lambda e: e.tensor_reduce(RK[:], CMP[:], AX.X, ALU.add), r=[kr], w=[kr])
        p.op("dve", lambda e: e.tensor_scalar(RK[:], RK[:], 1.5, None, ALU.is_lt), r=[kr], w=[kr])
        p.op("dve", lambda e: e.tensor_tensor(CMP[:, :, 0, :], ssv, RK[:], ALU.mult), r=[kr], w=[kr])
        p.op("dve", lambda e: e.tensor_reduce(GS[:].rearrange("p i g -> p (i g)"), CMP[:, :, 0, :], AX.X, ALU.add), r=[kr], w=[kr])
        p.op("dve", lambda e: e.tensor_reduce(GM[:], GS[:], AX.X, ALU.max), r=[kr], w=[kr])
        p.op("dve", lambda e: e.tensor_tensor(GS[:], GS[:], GM[:].unsqueeze(2).to_broadcast([128, NT, 4]), ALU.is_ge), r=[kr], w=[kr])
        p.op("dve", lambda e: e.tensor_tensor(G[:].rearrange("p i (g a) -> p i g a", g=4), RK[:].rearrange("p (i g) a -> p i g a", g=4),
                                              GS[:].unsqueeze(3).to_broadcast([128, NT, 4, 4]), ALU.mult), r=[kr], w=[kr])
        p.op("dve", lambda e: e.tensor_tensor(G[:], G[:], S[:], ALU.mult), r=[kr, K(pre, "S")], w=[kr])
        p.op("dve", lambda e: e.tensor_reduce(GM[:], G[:], AX.X, ALU.add), r=[kr], w=[kr])
        p.op("dve", lambda e: e.reciprocal(GM[:], GM[:]), r=[kr], w=[kr])
        p.op("dve", lambda e: e.tensor_tensor(G[:], G[:], GM[:].unsqueeze(2).to_broadcast([128, NT, 16]), ALU.mult), r=[kr], w=[kr])

        gbc = p.sb(pre + "gbc", [128, D], F32)
        bbc = p.sb(pre + "bbc", [128, D], F32)
        st4 = [p.sb(pre + "st4_%d" % j, [128, 8, 4], F32) for j in range(2)]
        p.dma("sp", gbc[:], lng_d.partition_broadcast(128), pre + "gbc", w=[K(pre + "ln", "gbc")])
        p.dma("sp", bbc[:], lnb_d.partition_broadcast(128), pre + "bbc", w=[K(pre + "ln", "bbc")])
        outk = []
        pendln = []
        pd4 = [(ps_d[0], K(pre, "ps_d", 0)), (ps_d[1], K(pre, "ps_d", 1)), (ps_tr[0], K(pre, "ps_tr", 0)), (ps_tr[1], K(pre, "ps_tr", 1))]
        n = 0
        for e_ in range(16):
            s = e_ % 2
            for tb in range(4):
                if e_ == 15:
                    p.capture()
                hb = hid[tb % 2]
                for hc in range(4):
                    j = n % 2
                    n += 1
                    for c8 in range(8):
                        p.op("pe", (lambda s, hc, c8, tb, j: lambda e: e.matmul(ps_g[j][:], wg[s][:, c8, hc * 128:(hc + 1) * 128], hFM[:, c8, tb * 512:(tb + 1) * 512], start=(c8 == 0), stop=(c8 == 7)))(s, hc, c8, tb, j),
                             r=[K(pre, "wg", s), K(pre, "hFM", tb)], w=[K(pre, "ps_g", j)])
                    for c8 in range(8):
                        p.op("pe", (lambda s, hc, c8, tb, j: lambda e: e.matmul(ps_u[j][:], wu[s][:, c8, hc * 128:(hc + 1) * 128], hFM[:, c8, tb * 512:(tb + 1) * 512], start=(c8 == 0), stop=(c8 == 7)))(s, hc, c8, tb, j),
                             r=[K(pre, "wu", s), K(pre, "hFM", tb)], w=[K(pre, "ps_u", j)])
                    p.op("act", (lambda j: lambda e: e.activation(sg[j][:], ps_g[j][:], AF.Silu))(j),
                         r=[K(pre, "ps_g", j)], w=[K(pre, "sg", j)])
                    p.op("dve", (lambda j, hb, hc: lambda e: e.tensor_tensor(hb[:, hc, :], sg[j][:], ps_u[j][:], ALU.mult))(j, hb, hc),
                         r=[K(pre, "sg", j), K(pre, "ps_u", j)], w=[K(pre, "hid", tb % 2, hc)])
                for tt in range(4):
                    i = tb * 4 + tt
                    for half in range(2):
                        j = (tt * 2 + half) % 4
                        pdj, kdj = pd4[j]
                        for hc in range(4):
                            p.op("pe", (lambda s, hc, tt, half, pdj, hb: lambda e: e.matmul(pdj[:], hb[:, hc, tt * 128:(tt + 1) * 128], wd[s][:, hc, half * 512:(half + 1) * 512], start=(hc == 0), stop=(hc == 3)))(s, hc, tt, half, pdj, hb),
                                 r=[K(pre, "hid", tb % 2, hc), K(pre, "wd", s)], w=[kdj])
                        p.op("dve", (lambda i, half, pdj, e_: lambda e: e.scalar_tensor_tensor(acc[:, i, half * 512:(half + 1) * 512], pdj[:], G[:, i, e_:e_ + 1], acc[:, i, half * 512:(half + 1) * 512], ALU.mult, ALU.add))(i, half, pdj, e_),
                             r=[kdj, kr, K("acc", i)], w=[K("acc", i), kdj])
                if e_ == 15:
                    la = p.end_capture()
                    p.replay_merged(la, pendln)
                    zs = [acc[:, tb * 4 + t, :] for t in range(4)]
                    zks = [K("acc", tb * 4 + t) for t in range(4)]
                    outs = [h_out[(tb * 4 + t) * 128:(tb * 4 + t + 1) * 128, :] for t in range(4)]
                    p.capture()
                    outk += ln_group(p, pre + "ln", zs, zks, gbc, bbc, outs, tb, st4[tb % 2], tmp)
                    pendln = p.end_capture()
                    if tb == 3:
                        p.replay_merged(pendln)
            if e_ + 2 < 16:
                load_w(e_ + 2)
        p.op("sp", None, r=outk)
        p.emit()
    p.nc.all_engine_barrier()


def ln_tile(p, pre, z, gbc, bbc, out_rows, i, zk, st, sq):
    j = i % 2
    ks = K(pre, "lnst", j)
    p.op("dve", lambda e: e.reduce_sum(st[:, 0:1], z, axis=AX.X), r=[zk], w=[ks])
    p.op("act", lambda e: e.activation(sq[:], z, AF.Square), r=[zk], w=[K(pre, "lnsq", j)])
    p.op("dve", lambda e: e.reduce_sum(st[:, 1:2], sq[:], axis=AX.X), r=[K(pre, "lnsq", j), ks], w=[ks])
    p.op("dve", lambda e: e.tensor_scalar(st[:, 0:2], st[:, 0:2], 1.0 / D, None, ALU.mult), r=[ks], w=[ks])
    p.op("dve", lambda e: e.tensor_tensor(st[:, 2:3], st[:, 0:1], st[:, 0:1], ALU.mult), r=[ks], w=[ks])
    p.op("dve", lambda e: e.tensor_tensor(st[:, 3:4], st[:, 1:2], st[:, 2:3], ALU.subtract), r=[ks], w=[ks])
    p.op("dve", lambda e: e.tensor_scalar(st[:, 3:4], st[:, 3:4], LN_EPS, None, ALU.add), r=[ks], w=[ks])
    p.op("act", lambda e: e.activation(st[:, 4:5], st[:, 3:4], AF.Sqrt), r=[ks], w=[ks])
    p.op("dve", lambda e: e.reciprocal(st[:, 5:6], st[:, 4:5]), r=[ks], w=[ks])
    p.op("dve", lambda e: e.scalar_tensor_tensor(st[:, 6:7], st[:, 0:1], -1.0, st[:, 5:6], ALU.mult, ALU.mult), r=[ks], w=[ks])
    p.op("act", lambda e: e.activation(z, z, AF.Identity, bias=st[:, 6:7], scale=st[:, 5:6]), r=[zk, ks], w=[zk])
    p.op("dve", lambda e: e.tensor_tensor(z, z, gbc[:], ALU.mult), r=[zk, K(pre, "gbc")], w=[zk])
    p.op("pool", lambda e: e.tensor_tensor(z, z, bbc[:], ALU.add), r=[zk, K(pre, "bbc")], w=[zk])
    ok = K(pre, "out", i)
    p.op("sp", lambda e: e.dma_start(out=out_rows, in_=z), r=[zk], w=[ok], dma=pre + "st%d" % j)
    return ok


import os
CUT = int(os.environ.get('FOX_CUT', '0'))
CUT2 = int(os.environ.get('FOX_CUT2', '0'))


def ln_group(p, pre, zs, zks, gbc, bbc, outs, gi, st, sq):
    n = len(zs)
    j = gi % 2
    ks = K(pre, "lnst", j)
    for t in range(n):
        sqt = sq[t % len(sq)]
        kq = K(pre, "lnsq", t % len(sq))
        p.op("act", (lambda sqt, z, o_: lambda e: e.activation(sqt[:], z, AF.Identity, accum_out=o_))(sqt, zs[t], st[:, 0, t:t + 1]), r=[zks[t], ks], w=[kq, ks])
        p.op("act", (lambda sqt, z, o_: lambda e: e.activation(sqt[:], z, AF.Square, accum_out=o_))(sqt, zs[t], st[:, 1, t:t + 1]), r=[zks[t], ks], w=[kq, ks])
    p.ts("dve", st[:, 0:2, 0:n], st[:, 0:2, 0:n], 1.0 / D, None, ALU.mult, r=[ks], w=[ks])
    p.tt("dve", st[:, 2, 0:n], st[:, 0, 0:n], st[:, 0, 0:n], ALU.mult, r=[ks], w=[ks])
    p.tt("dve", st[:, 3, 0:n], st[:, 1, 0:n], st[:, 2, 0:n], ALU.subtract, r=[ks], w=[ks])
    p.ts("dve", st[:, 3, 0:n], st[:, 3, 0:n], LN_EPS, None, ALU.add, r=[ks], w=[ks])
    p.act(st[:, 4, 0:n], st[:, 3, 0:n], AF.Ln, r=[ks], w=[ks])
    p.act(st[:, 5, 0:n], st[:, 4, 0:n], AF.Exp, scale=-0.5, r=[ks], w=[ks])
    p.stt("dve", st[:, 6, 0:n], st[:, 0, 0:n], -1.0, st[:, 5, 0:n], ALU.mult, ALU.mult, r=[ks], w=[ks])
    oks = []
    for t in range(n):
        z = zs[t]
        p.act(z, z, AF.Identity, bias=st[:, 6, t:t + 1], scale=st[:, 5, t:t + 1], r=[zks[t], ks], w=[zks[t]])
        p.tt("dve", z, z, gbc[:], ALU.mult, r=[zks[t], K(pre, "gbc")], w=[zks[t]])
        ok = K(pre, "out", gi, t)
        if isinstance(outs[t], tuple):
            p.tt("pool", outs[t][1], z, bbc[:], ALU.add, r=[zks[t], K(pre, "bbc")], w=[ok])
        else:
            p.tt("pool", z, z, bbc[:], ALU.add, r=[zks[t], K(pre, "bbc")], w=[zks[t]])
            p.dma("sp", outs[t], z, pre + "st%d_%d" % (j, t), r=[zks[t]], w=[ok])
        oks.append(ok)
    return oks


def stage_fox(p, pre, h_in, h_out, win_d, bf_d, wo_d, lng_d, lnb_d, dbg=None, acc=None):
    nc = p.nc
    HG = 4
    NG = 16 // HG
    outer = ExitStack()
    with outer:
        p.es = outer
        hFM = p.sb(pre + "hFM", [128, 8, T], BF16)
        csplit = p.sb(pre + "csplit", [80, T], BF16)
        ckTM = p.sb(pre + "ckTM", [128, NT, 16], F32)
        with ExitStack() as es:
            p.begin(es)
            c = consts(p)
            ht = [p.sb(pre + "ht%d" % j, [128, D], F32) for j in range(2)]
            hF32 = [p.sb(pre + "hF32_%d" % j, [128, 8, 128], F32) for j in range(2)]
            wf = p.sb(pre + "wf", [128, 8, 16], F32)
            nbf = p.sb(pre + "nbf", [16, 1], F32)
            fT = p.sb(pre + "fT", [16, T], F32)
            A = p.sb(pre + "A", [16, T], F32)
            B = p.sb(pre + "B", [16, T], F32)
            hi = p.sb(pre + "hi", [16, T], BF16)
            ps_tr = [p.ps(pre + "ps_tr%d" % j, [128, 512]) for j in range(2)]
            ps_f = [p.ps(pre + "ps_f%d" % j, [128, 512]) for j in range(2)]
            p.op("sp", lambda e: e.dma_start(out=wf[:], in_=win_d[:, 3072:3088].rearrange("(c p) e -> p c e", p=128)), w=["wf"], dma=pre + "wf")
            p.op("sp", lambda e: e.dma_start(out=nbf[:], in_=bf_d.rearrange("(h o) -> h o", o=1)), w=["nbf"], dma=pre + "nbf")
            p.op("dve", lambda e: e.tensor_scalar(nbf[:], nbf[:], -1.0, None, ALU.mult), r=["nbf"], w=["nbf"])
            p.op("pool", lambda e: e.memset(csplit[:], 0.0), w=["csplit"])
            for i in range(NT):
                hti = ht[i % 2]
                p.op("sp", (lambda i, hti: lambda e: e.dma_start(out=hti[:], in_=h_in[i * 128:(i + 1) * 128, :]))(i, hti),
                     w=[K("ht", i % 2)], dma=pre + "ht%d" % (i % 2))
                for half in range(2):
                    pt = ps_tr[half]
                    for cc in range(4):
                        c8 = half * 4 + cc
                        p.op("pe", (lambda hti, c8, cc, pt: lambda e: e.transpose(pt[:, cc * 128:(cc + 1) * 128], hti[:, c8 * 128:(c8 + 1) * 128], c["ident"][:]))(hti, c8, cc, pt),
                             r=[K("ht", i % 2), "ident"], w=[K("ps_tr", half)])
                    f32t = hF32[i % 2]
                    p.op("act", (lambda half, pt, f32t: lambda e: e.copy(f32t[:, half * 4:(half + 1) * 4, :], pt[:].rearrange("p (c t) -> p c t", c=4)))(half, pt, f32t),
                         r=[K("ps_tr", half)], w=[K("hF32", i % 2, half), K("ps_tr", half)])
                    p.op("dve", (lambda i, half, pt: lambda e: e.tensor_copy(hFM[:, half * 4:(half + 1) * 4, i * 128:(i + 1) * 128], pt[:].rearrange("p (c t) -> p c t", c=4)))(i, half, pt),
                         r=[K("ps_tr", half)], w=[K("hFM", i), K("ps_tr", half)])
                if CUT == 1:
                    continue
                pf = ps_f[(i // 4) % 2]
                for c8 in range(8):
                    p.op("pe", (lambda i, c8, pf: lambda e: e.matmul(pf[0:16, (i % 4) * 128:(i % 4 + 1) * 128], wf[:, c8, :], hF32[i % 2][:, c8, :], start=(c8 == 0), stop=(c8 == 7)))(i, c8, pf),
                         r=[K("hF32", i % 2, c8 // 4), "wf"], w=[K("ps_f", (i // 4) % 2)])
                if i % 4 == 3:
                    tb = i // 4
                    p.op("act", (lambda tb, pf: lambda e: e.activation(fT[:, tb * 512:(tb + 1) * 512], pf[0:16, :], AF.Exp, bias=nbf[:], scale=-1.0))(tb, pf),
                         r=[K("ps_f", tb % 2), "nbf"], w=[K("fT", tb), K("ps_f", tb % 2)])
                    p.op("act", (lambda tb: lambda e: e.activation(A[:, tb * 512:(tb + 1) * 512], fT[:, tb * 512:(tb + 1) * 512], AF.Ln, bias=1.0))(tb),
                         r=[K("fT", tb)], w=[id(A)])
            cur, oth = A, B
            s = 1
            while s < T and CUT not in (1, 2):
                p.op("dve", (lambda cur, oth, s: lambda e: e.tensor_tensor(oth[:, s:T], cur[:, s:T], cur[:, 0:T - s], ALU.add))(cur, oth, s),
                     r=[id(cur)], w=[id(oth)])
                p.op("act", (lambda cur, oth, s: lambda e: e.copy(oth[:, 0:s], cur[:, 0:s]))(cur, oth, s),
                     r=[id(cur)], w=[id(oth)])
                cur, oth = oth, cur
                s *= 2
            csum = cur
            for i in range(NT if CUT not in (1, 2, 3) else 0):
                p.op("pe", (lambda i: lambda e: e.transpose(ps_tr[0][:, i * 16:(i + 1) * 16], csum[:, i * 128:(i + 1) * 128], c["ident"][0:16, 0:16]))(i),
                     r=[id(csum), "ident"], w=[K("ps_tr", 0)])
            p.op("act", lambda e: e.copy(ckTM[:].rearrange("p i h -> p (i h)"), ps_tr[0][:, 0:256]), r=[K("ps_tr", 0)], w=["ckTM", K("ps_tr", 0)])
            m8 = oth
            p.op("dve", lambda e: e.tensor_scalar(m8[:], csum[:], -8.0, None, ALU.mult), r=[id(csum)], w=[id(m8)])
            for j in range(3 if CUT not in (1, 2, 3, 4) else 0):
                dst = csplit[32 * j:32 * j + 16, :]
                p.op("act", (lambda dst: lambda e: e.copy(dst, m8[:]))(dst), r=[id(m8)], w=["csplit"])
                if j < 2:
                    p.op("act", lambda e: e.copy(hi[:], m8[:]), r=[id(m8)], w=["hi"])
                    p.op("act", lambda e: e.copy(fT[:], hi[:]), r=["hi"], w=["hif"])
                    p.op("dve", lambda e: e.tensor_tensor(m8[:], m8[:], fT[:], ALU.subtract), r=[id(m8), "hif"], w=[id(m8)])
            if dbg is not None:
                p.op("sp", lambda e: e.dma_start(out=dbg[0:128, 0:256], in_=ckTM[:].rearrange("p i h -> p (i h)")), r=["ckTM"], w=["dbg"], dma="dbg")
                p.op("sp", None, r=["dbg"])
            p.op("sp", None, r=["csplit", "ckTM"] + [K("hFM", i) for i in range(NT)])
            p.emit()
        nc.all_engine_barrier()
        if dbg is not None:
            return
        p.es = outer
        OT = p.sb(pre + "OT", [128, 8, T], BF16)
        wo = p.sb(pre + "wo", [128, 8, D], BF16)
        with ExitStack() as es:
            p.begin(es)
            KTe = [p.sb(pre + "KTe%d" % h, [67, T], BF16) for h in range(HG)]
            QTe = [[p.sb(pre + "QTe%d_%d" % (j, h), [67, 512], BF16) for h in range(HG)] for j in range(2)]
            Vext = p.sb(pre + "Vext", [128, NT, HG, 128], BF16)
            wq = p.sb(pre + "wq", [128, 8, HG * 64], BF16)
            wk = p.sb(pre + "wk", [128, 8, HG * 64], BF16)
            wv = p.sb(pre + "wv", [128, 8, HG * 64], BF16)
            pT = [p.sb(pre + "pT%d" % j, [128, 512], BF16) for j in range(4)]
            rd1 = p.sb(pre + "rd", [64, 512], F32)
            rd = [rd1, rd1]
            ps_s = [p.ps(pre + "ps_s%d" % j, [128, 512]) for j in range(4)]
            ps_o = [p.ps(pre + "ps_o%d" % j, [128, 512]) for j in range(2)]
            ps_p = [p.ps(pre + "ps_p%d" % j, [128, 512]) for j in range(2)]
            for h in range(HG):
                p.op("dve", (lambda h: lambda e: e.memset(KTe[h][64:67, :], 1.0))(h), w=[K("KTones", h)])
            p.op("dve", lambda e: e.memset(Vext[:, :, :, 64:128], 1.0), w=["Vones"])
            def csrows(h, qb):
                base = csplit[h:h + 1, qb * 512:(qb + 1) * 512]
                return bass.AP(base.tensor, base.offset, [[32 * base.ap[0][0], 3]] + [list(x) for x in base.ap[1:]])
            nq = 0
            nsj = 0
            no = 0
            npp = 0
            for g in range(NG if CUT2 == 0 else 1):
                c0 = g * HG * 64
                p.op("pool", (lambda c0: lambda e: e.dma_start(out=wk[:], in_=win_d[:, D + c0:D + c0 + HG * 64].rearrange("(c p) f -> p c f", p=128)))(c0), w=["wk"], dma=pre + "wk")
                p.op("pool", (lambda c0: lambda e: e.dma_start(out=wq[:], in_=win_d[:, c0:c0 + HG * 64].rearrange("(c p) f -> p c f", p=128)))(c0), w=["wq"], dma=pre + "wq")
                p.op("pool", (lambda c0: lambda e: e.dma_start(out=wv[:], in_=win_d[:, 2 * D + c0:2 * D + c0 + HG * 64].rearrange("(c p) f -> p c f", p=128)))(c0), w=["wv"], dma=pre + "wv")
                if g == 0:
                    p.op("pool", lambda e: e.dma_start(out=wo[:], in_=wo_d.rearrange("(j p) f -> p j f", p=128)), w=["wo"], dma=pre + "wo")
                for a_ in range(HG // 2):
                    for tb in range(4):
                        pp = ps_p[npp % 2]
                        kp = K("ps_p", npp % 2)
                        npp += 1
                        for c8 in range(8):
                            p.mm(pp[:], wk[:, c8, a_ * 128:(a_ + 1) * 128], hFM[:, c8, tb * 512:(tb + 1) * 512], c8 == 0, c8 == 7, r=["wk"], w=[kp])
                        for hh_ in range(2):
                            hl = 2 * a_ + hh_
                            p.cp("dve", KTe[hl][0:64, tb * 512:(tb + 1) * 512], pp[hh_ * 64:(hh_ + 1) * 64, :], r=[kp], w=[K("KTe", hl, tb), kp])
                for i in range(NT):
                    pp = ps_p[npp % 2]
                    kp = K("ps_p", npp % 2)
                    npp += 1
                    for c8 in range(8):
                        p.op("pe", (lambda i, c8, pp: lambda e: e.matmul(pp[:, 0:HG * 64], hFM[:, c8, i * 128:(i + 1) * 128], wv[:, c8, :], start=(c8 == 0), stop=(c8 == 7)))(i, c8, pp),
                             r=["wv"], w=[kp])
                    p.op("dve", (lambda i, pp: lambda e: e.tensor_copy(Vext[:, i, :, 0:64], pp[:, 0:HG * 64].rearrange("p (h d) -> p h d", h=HG)))(i, pp),
                         r=[kp], w=[K("V", i), kp])
                for qb in range(4 if CUT2 == 0 else (0 if CUT2 == 1 else 1)):
                    qs = nq % 2
                    nq += 1
                    for hl in range(HG):
                        h = g * HG + hl
                        p.dma("sp", QTe[qs][hl][64:67, :], csplit[:, qb * 512:(qb + 1) * 512].rearrange("(j r) f -> j r f", r=32)[0:3, h, :] if False else csrows(h, qb),
                              pre + "qc%d_%d" % (qs, hl), r=["csplit"], w=[K("QTc", qs, hl)])
                    for a_ in range(HG // 2):
                        pp = ps_p[npp % 2]
                        kp = K("ps_p", npp % 2)
                        npp += 1
                        for c8 in range(8):
                            p.mm(pp[:], wq[:, c8, a_ * 128:(a_ + 1) * 128], hFM[:, c8, qb * 512:(qb + 1) * 512], c8 == 0, c8 == 7, r=["wq"], w=[kp])
                        for hh_ in range(2):
                            hl = 2 * a_ + hh_
                            p.cp("dve", QTe[qs][hl][0:64, :], pp[hh_ * 64:(hh_ + 1) * 64, :], r=[kp], w=[K("QTe", qs, hl), kp])
                    jobs = []
                    for hl in range(HG):
                        h = g * HG + hl
                        nkt = 4 * qb + 4
                        for kt in range(nkt):
                            jobs.append((hl, h, kt, nkt, no % 2))
                        no += 1
                    LA = 3
                    inflight = {}

                    def front(job, ji):
                        hl, h, kt, nkt, oj = job
                        sj = ji % 4
                        d_ = kt - 4 * qb
                        lo = 128 * d_ if d_ > 0 else 0
                        p.mm(ps_s[sj][:, lo:512], KTe[hl][:, kt * 128:(kt + 1) * 128], QTe[qs][hl][:, lo:512], True, True,
                             r=[K("KTe", hl, kt // 4), K("KTones", hl), K("QTe", qs, hl), K("QTc", qs, hl)], w=[K("ps_s", sj)])
                        p.act(pT[sj][:, lo:512], ps_s[sj][:, lo:512], AF.Exp, bias=ckTM[:, kt, h:h + 1], scale=0.125,
                              r=[K("ps_s", sj)], w=[K("pT", sj), K("ps_s", sj)])
                        if d_ >= 0:
                            p.op("pool", (lambda sj, lo, base: lambda e: e.affine_select(out=pT[sj][:, lo:512], in_=pT[sj][:, lo:512], pattern=[[1, 512 - lo]], compare_op=ALU.is_ge, fill=0.0, base=base, channel_multiplier=-1))(sj, lo, qb * 512 + lo - kt * 128),
                                 r=[K("pT", sj)], w=[K("pT", sj)])
                        inflight[ji] = (sj, lo)

                    def back(job, ji):
                        hl, h, kt, nkt, oj = job
                        sj, lo = inflight.pop(ji)
                        po = ps_o[oj]
                        ko = K("ps_o", oj)
                        p.mm(po[:, lo:512], Vext[:, kt, hl, :], pT[sj][:, lo:512], kt == 0, kt == nkt - 1,
                             r=[K("V", kt), "Vones", K("pT", sj)], w=[ko])
                        if kt == nkt - 1:
                            hh, hj = h % 2, h // 2
                            p.rcp(rd[oj][:], po[64:128, :], r=[ko], w=[K("rd", 0), ko])
                            p.tt("dve", OT[hh * 64:(hh + 1) * 64, hj, qb * 512:(qb + 1) * 512], po[0:64, :], rd[oj][:], ALU.mult,
                                 r=[ko, K("rd", 0)], w=[K("OT", qb), ko])
                    nj = len(jobs) if CUT2 not in (1, 2) else 0
                    for ji in range(nj + LA):
                        if ji < nj:
                            front(jobs[ji], nsj + ji)
                        if ji - LA >= 0:
                            back(jobs[ji - LA], nsj + ji - LA)
                    nsj += nj
            p.op("sp", None, r=[K("OT", qb) for qb in range(4)] + ["wo"])
            p.emit()
        nc.all_engine_barrier()
        with ExitStack() as es:
            p.begin(es)
            z4 = [p.sb(pre + "z4_%d" % j, [128, 4, D], F32) for j in range(2)]
            h2t = [p.sb(pre + "h2t%d" % j, [128, D], F32) for j in range(2)]
            sq = [p.sb(pre + "sq%d" % j, [128, D], F32) for j in range(2)]
            st4 = [p.sb(pre + "st4_%d" % j, [128, 8, 4], F32) for j in range(2)]
            gbc = p.sb(pre + "gbc", [128, D], F32)
            bbc = p.sb(pre + "bbc", [128, D], F32)
            ps_p = [p.ps(pre + "ps_p3_%d" % j, [128, 512]) for j in range(3)]
            p.op("sp", lambda e: e.dma_start(out=gbc[:], in_=lng_d.partition_broadcast(128)), w=[K(pre, "gbc")], dma=pre + "gbc")
            p.op("sp", lambda e: e.dma_start(out=bbc[:], in_=lnb_d.partition_broadcast(128)), w=[K(pre, "bbc")], dma=pre + "bbc")
            npp = 0
            outk = []
            pend = []
            for gi in range(NT // 4 if CUT2 in (0, 4) else 0):
                p.capture()
                zg = z4[gi % 2]
                zs, zks, outs = [], [], []
                for t in range(4):
                    i = gi * 4 + t
                    j = i % 2
                    p.dma("sp", h2t[j][:], h_in[i * 128:(i + 1) * 128, :], pre + "h2t%d" % j, w=[K("h2t", j)])
                    zk = K("z4", gi % 2, t)
                    for half in range(2):
                        pp = ps_p[npp % 3]
                        kp = K("ps_p", npp % 3)
                        npp += 1
                        for hj in range(8):
                            p.mm(pp[:], OT[:, hj, i * 128:(i + 1) * 128], wo[:, hj, half * 512:(half + 1) * 512], hj == 0, hj == 7, w=[kp])
                        sl = slice(half * 512, (half + 1) * 512)
                        p.stt("dve", zg[:, t, sl], h2t[j][:, sl], ALPHA, pp[:], ALU.mult, ALU.add, r=[kp, K("h2t", j)], w=[zk, kp])
                    zs.append(zg[:, t, :])
                    zks.append(zk)
                    outs.append(h_out[i * 128:(i + 1) * 128, :] if acc is None else ("sb", acc[:, i, :]))
                la = p.end_capture()
                p.replay_merged(la, pend)
                p.capture()
                outk += ln_group(p, pre, zs, zks, gbc, bbc, outs, gi, st4[gi % 2], sq)
                pend = p.end_capture()
            p.replay_merged(pend)
            p.op("sp", None, r=outk)
            p.emit()
        nc.all_engine_barrier()


def bc_load(p, name, src_1d, n=128):
    t = p.sb(name, [n, D], F32)
    p.dma("sp", t[:], src_1d.partition_broadcast(n), "ld_" + name, w=[name])
    return t


def stage_rwkv1(p, pre, xT_d, W, S):
    nc = p.nc
    with ExitStack() as es:
        p.begin(es)
        c = consts(p)
        wr = p.sb("wr", [128, 8, D], BF16)
        wk = p.sb("wk", [128, 8, D], BF16)
        wv = p.sb("wv", [128, 8, D], BF16)
        for t_, d_, n_ in ((wr, W["wr"], "wr"), (wk, W["wk"], "wk"), (wv, W["wv"], "wv")):
            p.dma("pool", t_[:], d_.rearrange("(c p) f -> p c f", p=128), "ld_" + n_, w=[n_])
        w1 = p.sb("w1", [128, 8, 64], BF16)
        a1 = p.sb("a1", [128, 8, 64], BF16)
        g1 = p.sb("g1", [128, 8, 128], BF16)
        w2 = p.sb("w2", [64, D], BF16)
        a2 = p.sb("a2", [64, D], BF16)
        g2 = p.sb("g2", [128, D], BF16)
        for t_, d_, n_ in ((w1, W["w1"], "w1"), (a1, W["a1"], "a1"), (g1, W["g1"], "g1")):
            p.dma("pool", t_[:], d_.rearrange("(c p) f -> p c f", p=128), "ld_" + n_, w=[n_])
        for t_, d_, n_ in ((w2, W["w2"], "w2"), (a2, W["a2"], "a2"), (g2, W["g2"], "g2")):
            p.dma("pool", t_[:], d_, "ld_" + n_, w=[n_])
        W0 = bc_load(p, "W0", W["w0"])
        A0 = bc_load(p, "A0", W["a0"])
        KKp = bc_load(p, "KKp", W["kk"])
        KAp = bc_load(p, "KAp", W["ka"])
        RKp = bc_load(p, "RKp", W["rk"].rearrange("h n -> (h n)"))
        mix = p.sb("mix", [128, 6, 8], F32)
        p.dma("sp", mix[:], W["mix"].rearrange("m (c p) -> p m c", p=128), "ld_mix", w=["mix"], slow=True)
        triu = p.sb("triu", [128, 128], F32)
        ones = p.sb("ones", [128, 128], F32)
        p.op("pool", lambda e: e.memset(triu[:], 1.0), w=["triu"])
        p.op("pool", lambda e: e.affine_select(out=triu[:], in_=triu[:], pattern=[[1, 128]], compare_op=ALU.is_ge, fill=0.0, base=0, channel_multiplier=-1), r=["triu"], w=["triu"])
        p.op("pool", lambda e: e.memset(ones[:], 1.0), w=["ones"])

        xt = [p.sb("xt%d" % j, [128, 8, 129], F32) for j in range(2)]
        xx = p.sb("xx", [128, 8, 128], F32)
        xm = [[p.sb("xm%d_%d" % (s_, j), [128, 8, 128], BF16) for j in range(6)] for s_ in range(2)]
        dbl = lambda n_: [p.sb("%s%d" % (n_, j), [128, D], F32) for j in range(2)]
        R, K0, V32, Aa, LDP = dbl("R"), dbl("K0"), dbl("V32"), dbl("Aa"), dbl("LDP")
        KKn = p.sb("KKn", [128, D], F32)
        KP = p.sb("KP", [128, D], F32)
        Bb = p.sb("Bb", [128, D], F32)
        C32 = p.sb("C32", [128, D], F32)
        T1 = p.sb("T1", [128, D], F32)
        T2 = p.sb("T2", [128, D], F32)
        T3 = p.sb("T3", [128, D], F32)
        Gg = p.sb("Gg", [128, D], F32)
        sm = p.sb("sm", [128, 4, 16], F32)
        ET = p.sb("ET", [64, 16], F32)
        hid = p.sb("hid", [128, 3, 128], F32)
        hidb = p.sb("hidb", [128, 3, 128], BF16)
        ob = {n_: p.sb("o_" + n_, [128, D], BF16) for n_ in ("RT", "KKT", "KH", "BH", "KG", "BG", "Vb")}
        psT = [[p.ps("psT%d_%d" % (j, h), [128, 512]) for h in range(2)] for j in range(3)]
        ps_l = p.ps("ps_l", [128, 512])
        ps_e = p.ps("ps_e", [128, 512])
        nps = [0]

        def tm_ps():
            j = nps[0] % 2
            nps[0] += 1
            return psT[j], [K("psT", j, 0), K("psT", j, 1)]

        def proj_tm(lhs_tile, lk, w_t, wkey, kc):
            pt, pk = tm_ps()
            for half in range(2):
                if kc is None:
                    p.mm(pt[half][:], lhs_tile, w_t[:, half * 512:(half + 1) * 512], True, True, r=[lk, wkey], w=[pk[half]])
                else:
                    for c8 in range(kc):
                        p.mm(pt[half][:], lhs_tile[:, c8, :], w_t[:, c8, half * 512:(half + 1) * 512], c8 == 0, c8 == kc - 1, r=[lk, wkey], w=[pk[half]])
            return pt, pk

        def halves(pt, pk, fn):
            for half in range(2):
                fn(slice(half * 512, (half + 1) * 512), pt[half], pk[half])

        xTv = xT_d.rearrange("(c p) t -> p c t", p=128)
        hn = lambda t_: t_[:].rearrange("p (h n) -> p h n", h=16)

        def load_xt(j):
            s_ = j % 2
            x_ = xt[s_]
            kx = K("xt", s_)
            if j == 0:
                p.op("pool", lambda e: e.memset(x_[:, :, 0:1], 0.0), w=[K("xt0c", 0)])
                p.dma("sp", x_[:, :, 1:129], xTv[:, :, 0:128], "ld_xt0", w=[kx])
            else:
                p.dma("sp", x_[:, :, :], xTv[:, :, j * 128 - 1:j * 128 + 128], "ld_xt%d" % s_, w=[kx, K("xt0c", 0)] if s_ == 0 else [kx])
        load_xt(0)
        load_xt(1)

        def P1(j):
            s_ = j % 2
            x_ = xt[s_]
            kx = K("xt", s_)
            kx_all = [kx, K("xt0c", 0)] if s_ == 0 else [kx]
            p.tt("dve", xx[:], x_[:, :, 0:128], x_[:, :, 1:129], ALU.subtract, r=kx_all, w=["xx"])
            xm_ = xm[s_]
            for m in (0, 2, 3, 1, 4, 5):
                for c8 in range(8):
                    p.stt("dve", xm_[m][:, c8, :], xx[:, c8, :], mix[:, m, c8:c8 + 1], x_[:, c8, 1:129], ALU.mult, ALU.add,
                          r=["xx", "mix"] + kx_all, w=[K("xm", s_, m)])
            if j + 2 < NT:
                load_xt(j + 2)
            pt, pk = proj_tm(xm_[0], K("xm", s_, 0), wr, "wr", 8)
            halves(pt, pk, lambda sl, ps_, k_: p.cp("act", R[s_][:, sl], ps_[:], r=[k_], w=[K("R", s_), k_]))
            pt, pk = proj_tm(xm_[2], K("xm", s_, 2), wk, "wk", 8)
            halves(pt, pk, lambda sl, ps_, k_: p.cp("act", K0[s_][:, sl], ps_[:], r=[k_], w=[K("K0", s_), k_]))
            pt, pk = proj_tm(xm_[3], K("xm", s_, 3), wv, "wv", 8)
            halves(pt, pk, lambda sl, ps_, k_: p.cp("act", V32[s_][:, sl], ps_[:], r=[k_], w=[K("V32", s_), k_]))
            halves(pt, pk, lambda sl, ps_, k_: p.cp("act", ob["Vb"][:, sl], ps_[:], r=[k_], w=["o_Vb", k_]))
            p.dma("sp", S["Vb"][j * 128:(j + 1) * 128, :], ob["Vb"][:], "st_Vb", r=["o_Vb"], w=[K("S_Vb", j)])
            for c8 in range(8):
                p.mm(ps_l[0:64, 0:128], w1[:, c8, :], xm_[1][:, c8, :], c8 == 0, c8 == 7, r=["w1", K("xm", s_, 1)], w=["ps_l"])
            for c8 in range(8):
                p.mm(ps_l[0:64, 128:256], a1[:, c8, :], xm_[4][:, c8, :], c8 == 0, c8 == 7, r=["a1", K("xm", s_, 4)], w=["ps_l"])
            for c8 in range(8):
                p.mm(ps_l[:, 256:384], g1[:, c8, :], xm_[5][:, c8, :], c8 == 0, c8 == 7, r=["g1", K("xm", s_, 5)], w=["ps_l"])
            p.act(hid[0:64, 0, :], ps_l[0:64, 0:128], AF.Exp, scale=2.0, r=["ps_l"], w=["hid0", "ps_l"])
            p.ts("dve", hid[0:64, 0, :], hid[0:64, 0, :], 1.0, None, ALU.add, r=["hid0"], w=["hid0"])
            p.rcp(hid[0:64, 0, :], hid[0:64, 0, :], r=["hid0"], w=["hid0"])
            p.ts("dve", hidb[0:64, 0, :], hid[0:64, 0, :], -2.0, 1.0, ALU.mult, ALU.add, r=["hid0"], w=["hidb0"])
            p.cp("act", hidb[0:64, 1, :], ps_l[0:64, 128:256], r=["ps_l"], w=["hidb1", "ps_l"])
            p.act(hid[:, 2, :], ps_l[:, 256:384], AF.Exp, scale=-1.0, r=["ps_l"], w=["hid2", "ps_l"])
            p.act(hid[:, 2, :], hid[:, 2, :], AF.Ln, bias=1.0, r=["hid2"], w=["hid2"])
            p.act(hidb[:, 2, :], hid[:, 2, :], AF.Exp, scale=-1.0, r=["hid2"], w=["hidb2"])
            L_ = LDP[s_]
            kL = K("LDP", s_)
            pt, pk = proj_tm(hidb[0:64, 0, :], "hidb0", w2, "w2", None)
            halves(pt, pk, lambda sl, ps_, k_: p.tt("dve", L_[:, sl], ps_[:], W0[:, sl], ALU.add, r=[k_, "W0"], w=[kL, k_]))
            p.act(L_[:], L_[:], AF.Exp, scale=-1.0, r=[kL], w=[kL])
            p.act(L_[:], L_[:], AF.Ln, bias=1.0, r=[kL], w=[kL])
            p.act(L_[:], L_[:], AF.Exp, scale=-1.0, bias=-0.5, r=[kL], w=[kL])
            A_ = Aa[s_]
            kA = K("Aa", s_)
            pt, pk = proj_tm(hidb[0:64, 1, :], "hidb1", a2, "a2", None)
            halves(pt, pk, lambda sl, ps_, k_: p.tt("dve", A_[:, sl], ps_[:], A0[:, sl], ALU.add, r=[k_, "A0"], w=[kA, k_]))
            p.act(A_[:], A_[:], AF.Exp, scale=-1.0, r=[kA], w=[kA])
            p.act(A_[:], A_[:], AF.Ln, bias=1.0, r=[kA], w=[kA])
            p.act(A_[:], A_[:], AF.Exp, scale=-1.0, r=[kA], w=[kA])
            pt, pk = proj_tm(hidb[:, 2, :], "hidb2", g2, "g2", None)
            halves(pt, pk, lambda sl, ps_, k_: p.cp("act", Gg[:, sl], ps_[:], r=[k_], w=["Gg", k_]))
            p.dma("sp", S["Gg"][j * 128:(j + 1) * 128, :], Gg[:], "st_Gg", r=["Gg"], w=[K("S_Gg", j)])

        def P2(j):
            s_ = j % 2
            R_, K_, V_, A_, L_ = R[s_], K0[s_], V32[s_], Aa[s_], LDP[s_]
            kR, kK, kV, kA, kL = K("R", s_), K("K0", s_), K("V32", s_), K("Aa", s_), K("LDP", s_)
            p.tt("dve", KKn[:], K_[:], KKp[:], ALU.mult, r=[kK, "KKp"], w=["KKn"])
            p.act(T3[:], KKn[:], AF.Square, r=["KKn"], w=["T3"])
            p.red(sm[:, 0, :], hn(T3), r=["T3"], w=["sm0"])
            p.ts("dve", sm[:, 0, :], sm[:, 0, :], 1e-24, None, ALU.max, r=["sm0"], w=["sm0"])
            p.act(sm[:, 0, :], sm[:, 0, :], AF.Ln, r=["sm0"], w=["sm0"])
            p.act(sm[:, 0, :], sm[:, 0, :], AF.Exp, scale=-0.5, r=["sm0"], w=["sm0"])
            p.tt("dve", hn(KKn), hn(KKn), sm[:, 0, :].unsqueeze(2).to_broadcast([128, 16, 64]), ALU.mult, r=["KKn", "sm0"], w=["KKn"])
            p.stt("dve", T3[:], A_[:], -1.0, KAp[:], ALU.add, ALU.mult, r=[kA, "KAp"], w=["T3"])
            p.stt("dve", KP[:], T3[:], 1.0, K_[:], ALU.add, ALU.mult, r=["T3", kK], w=["KP"])
            p.tt("pool", Bb[:], KKn[:], A_[:], ALU.mult, r=["KKn", kA], w=["Bb"])
            p.tt("pool", T3[:], R_[:], KP[:], ALU.mult, r=[kR, "KP"], w=["T3"])
            p.tt("pool", T3[:], T3[:], RKp[:], ALU.mult, r=["T3", "RKp"], w=["T3"])
            p.red(sm[:, 1, :], hn(T3), r=["T3"], w=["sm1"])
            p.tt("dve", hn(T3), hn(V_), sm[:, 1, :].unsqueeze(2).to_broadcast([128, 16, 64]), ALU.mult, r=[kV, "sm1"], w=["T3"])
            p.dma("sp", S["BV"][j * 128:(j + 1) * 128, :], T3[:], "st_BV", r=["T3"], w=[K("S_BV", j), "T3dma"])
            pc, pkc = psT[2], [K("psT", 2, 0), K("psT", 2, 1)]
            for half in range(2):
                sl = slice(half * 512, (half + 1) * 512)
                p.mm(pc[half][:], triu[:], L_[:, sl], True, True, r=["triu", kL], w=[pkc[half]])
            halves(pc, pkc, lambda sl, ps_, k_: p.cp("act", C32[:, sl], ps_[:], r=[k_], w=["C32", k_]))
            for half in range(2):
                sl = slice(half * 512, (half + 1) * 512)
                p.mm(pc[half][:], ones[:], L_[:, sl], True, True, r=["ones", kL], w=[pkc[half]])
            for h in range(16):
                p.mm(ps_e[0:64, h:h + 1], L_[:, h * 64:(h + 1) * 64], ones[:, 0:1], True, True, r=[kL, "ones"], w=["ps_e"])
            p.act(ET[:], ps_e[0:64, 0:16], AF.Exp, scale=-1.0, r=["ps_e"], w=["ET", "ps_e"])
            p.dma("sp", S["ET"][j], ET[:], "st_ET", r=["ET"], w=[K("S_ET", j)])
            halves(pc, pkc, lambda sl, ps_, k_: p.tt("dve", T2[:, sl], ps_[:], C32[:, sl], ALU.subtract, r=[k_, "C32", "T2"], w=["T2", k_]))
            p.act(T2[:], T2[:], AF.Exp, scale=-1.0, r=["T2"], w=["T2"])
            p.tt("dve", ob["KG"][:], KP[:], T2[:], ALU.mult, r=["KP", "T2"], w=["o_KG"])
            p.tt("pool", ob["BG"][:], Bb[:], T2[:], ALU.mult, r=["Bb", "T2"], w=["o_BG"])
            p.act(T1[:], C32[:], AF.Exp, scale=-1.0, r=["C32"], w=["T1"])
            p.tt("dve", ob["RT"][:], R_[:], T1[:], ALU.mult, r=[kR, "T1"], w=["o_RT"])
            p.act(T2[:], C32[:], AF.Exp, r=["C32", "T2"], w=["T2"])
            p.tt("dve", ob["KH"][:], KP[:], T2[:], ALU.mult, r=["KP", "T2"], w=["o_KH"])
            p.tt("pool", ob["BH"][:], Bb[:], T2[:], ALU.mult, r=["Bb", "T2"], w=["o_BH"])
            p.tt("dve", T1[:], C32[:], L_[:], ALU.subtract, r=["C32", kL, "T1"], w=["T1"])
            p.act(T1[:], T1[:], AF.Exp, scale=-1.0, r=["T1"], w=["T1"])
            p.tt("pool", ob["KKT"][:], KKn[:], T1[:], ALU.mult, r=["KKn", "T1"], w=["o_KKT"])
            for n_ in ("RT", "KKT", "KH", "BH", "KG", "BG"):
                p.dma("sp", S[n_][j * 128:(j + 1) * 128, :], ob[n_][:], "st_" + n_, r=["o_" + n_], w=[K("S_" + n_, j)])

        P1(0)
        for j in range(NT):
            p.capture()
            P2(j)
            la = p.end_capture()
            lb = []
            if j + 1 < NT:
                p.capture()
                P1(j + 1)
                lb = p.end_capture()
            p.replay_merged(la, lb)
        p.fence([K("S_" + n_, j) for n_ in ("RT", "KKT", "KH", "BH", "KG", "BG", "Vb", "Gg", "BV", "ET") for j in range(NT)])
        p.emit()
    nc.all_engine_barrier()


def stage_rwkv2(p, pre, W, S):
    nc = p.nc
    with ExitStack() as es:
        p.begin(es)
        c = consts(p)
        ident, identb = c["ident"], c["identb"]
        GNg = bc_load(p, "GNg", W["gn_g"])
        GNb = bc_load(p, "GNb", W["gn_b"])
        MASK4 = p.sb("MASK4", [128, 4, 128], F32)
        MASKL = p.sb("MASKL", [128, 4, 128], F32)
        p.op("pool", lambda e: e.memset(MASK4[:], 1.0), w=["MASK4"])
        p.op("pool", lambda e: e.memset(MASKL[:], 1.0), w=["MASKL"])
        for q4 in range(4):
            strict = q4 % 2 == 0
            p.op("pool", (lambda q4, strict: lambda e: e.affine_select(out=MASK4[:, q4, :], in_=MASK4[:, q4, :], pattern=[[1, 128]], compare_op=ALU.is_ge, fill=0.0, base=(-1 if strict else 0), channel_multiplier=-1))(q4, strict), r=["MASK4"], w=["MASK4"])
            p.op("pool", (lambda q4: lambda e: e.affine_select(out=MASKL[:, q4, :], in_=MASKL[:, q4, :], pattern=[[-1, 128]], compare_op=ALU.is_ge, fill=0.0, base=-1, channel_multiplier=1))(q4), r=["MASKL"], w=["MASKL"])
        names = ("RT", "KH", "BH", "KG", "BG", "Vb")
        inb = {n_: [p.sb("i_%s%d" % (n_, j), [128, D], BF16) for j in range(2)] for n_ in names}
        Rhs = [p.sb("Rhs%d" % j, [128, 16, 128], BF16) for j in range(2)]
        Gg = [p.sb("Gg%d" % j, [128, D], F32) for j in range(2)]
        BV = [p.sb("BV%d" % j, [128, D], F32) for j in range(2)]
        ET = [p.sb("ET%d" % j, [64, 16], F32) for j in range(2)]
        KHT = p.sb("KHT", [64, 16, 128], BF16)
        BHT = p.sb("BHT", [64, 16, 128], BF16)
        KR = p.sb("KR", [64, 16, 256], BF16)
        ATB = p.sb("ATB", [128, 16, 512], BF16)
        Lb = p.sb("Lb", [128, 16, 128], BF16)
        Nn = [p.sb("Nn%d" % j, [128, 16, 128], BF16) for j in range(2)]
        NTn = [p.sb("NTn%d" % j, [128, 16, 128], BF16) for j in range(2)]
        Yb = [p.sb("Yb%d" % j, [128, 16, 128], BF16) for j in range(2)]
        PUn = p.sb("PUn", [128, 16, 128], BF16)
        MTp = p.sb("MTp", [64, 16, 64], BF16)
        QT = p.sb("QT", [64, 16, 128], BF16)
        Gf = p.sb("Gf", [64, 16, 64], F32)
        H = p.sb("H", [64, 16, 64], F32)
        HG = p.sb("HG", [64, 16, 64], F32)
        Hb = p.sb("Hb", [64, 16, 64], BF16)
        Tn = [p.sb("Tn%d" % j, [128, D], F32) for j in range(2)]
        sm = p.sb("sm", [128, 6, 16], F32)
        YG = [p.sb("YG%d" % j, [128, D], BF16) for j in range(2)]
        TRb = [p.ps("TRb%d" % j, [128, 1024], BF16) for j in range(2)]
        PB = [p.ps("PB%d" % j, [128, 512]) for j in range(6)]
        nb = [0]

        def bank():
            j = nb[0] % 4
            nb[0] += 1
            return PB[j], K("PB", j)
        PY = [(PB[4], K("PB", 4)), (PB[5], K("PB", 5))]
        pendingB = []
        ntr = [0]
        nev = [0]

        def evq():
            nev[0] += 1
            return "dve" if nev[0] % 3 == 0 else "act"

        p.op("pool", lambda e: e.memset(H[:], 0.0), w=["H"])
        p.op("pool", lambda e: e.memset(Hb[:], 0.0), w=["Hb"])

        def loads(j):
            s = j % 2
            rows = slice(j * 128, (j + 1) * 128)
            for n_ in names:
                p.dma("sp", inb[n_][s][:], S[n_][rows, :], "ld_%s%d" % (n_, s), w=[K("i", n_, s)])
            p.dma("sp", Rhs[s][:, :, 0:64], S["KKT"][rows, :].rearrange("t (h n) -> t h n", h=16), "ld_KKT%d" % s, w=[K("RhsA", s)])
            p.dma("sp", ET[s][:], S["ET"][j], "ld_ET%d" % s, w=[K("ET", s)])
        loads(0)

        for j in range(NT):
            p.capture()
            s = j % 2
            rows = slice(j * 128, (j + 1) * 128)
            if j + 1 < NT:
                loads(j + 1)
            p.dma("sp", Gg[s][:], S["Gg"][rows, :], "ld_Gg%d" % s, w=[K("Gg", s)])
            p.dma("sp", BV[s][:], S["BV"][rows, :], "ld_BV%d" % s, w=[K("BV", s)])
            RT_, KH_, BH_, KG_, BG_, Vb_ = (inb[n_][s] for n_ in names)
            kRT, kKH, kBH, kKG, kBG, kVb = (K("i", n_, s) for n_ in names)
            def transp(src, rk, dst, wk_, pair_view=None):
                tb_ = TRb[ntr[0] % 2]
                tk = K("TRb", ntr[0] % 2)
                ntr[0] += 1
                for a_ in range(8):
                    p.tr(tb_[:, a_ * 128:(a_ + 1) * 128], src(a_), identb[:], r=[rk, "identb"], w=[tk])
                for par in range(2):
                    p.cp(evq(), dst(par), tb_[par * 64:(par + 1) * 64, :].rearrange("p (a t) -> p a t", a=8), r=[tk], w=[wk_, tk])
            ev = lambda t_, lo, hi: (lambda par: t_[:].rearrange("p (a two) t -> p a two t", two=2)[:, :, par, lo:hi])
            transp(lambda a_: KH_[:, a_ * 128:(a_ + 1) * 128], kKH, ev(KHT, 0, 128), "KHT")
            transp(lambda a_: BH_[:, a_ * 128:(a_ + 1) * 128], kBH, ev(BHT, 0, 128), "BHT")
            for hh in range(2):
                tb_ = TRb[ntr[0] % 2]
                tk = K("TRb", ntr[0] % 2)
                ntr[0] += 1
                for h8 in range(8):
                    p.tr(tb_[0:64, h8 * 128:(h8 + 1) * 128], Rhs[s][:, hh * 8 + h8, 0:64], identb[:], r=[K("RhsA", s), "identb"], w=[tk])
                p.cp(evq(), KR[:, hh * 8:(hh + 1) * 8, 0:128], tb_[0:64, :].rearrange("p (h t) -> p h t", h=8), r=[tk], w=["KR", tk])
            transp(lambda a_: RT_[:, a_ * 128:(a_ + 1) * 128], kRT, ev(KR, 128, 256), "KR")
            for h in range(16):
                b_, bk = bank()
                p.mm(b_[:, 0:256], KHT[:, h, :], KR[:, h, :], True, True, r=["KHT", "KR"], w=[bk])
                p.mm(b_[:, 256:512], BHT[:, h, :], KR[:, h, :], True, True, r=["BHT", "KR"], w=[bk])
                p.tt("dve", ATB[:, h, :], b_[:], MASK4[:].rearrange("p a t -> p (a t)"), ALU.mult, r=[bk, "MASK4"], w=[K("ATB", h // 4), bk])
            for g4 in range(4):
                b_, bk = bank()
                for hq in range(4):
                    h = g4 * 4 + hq
                    p.mm(b_[:, hq * 128:(hq + 1) * 128], KR[:, h, 0:128], BHT[:, h, :], True, True, r=["KR", "BHT"], w=[bk])
                p.tt("dve", Lb[:, g4 * 4:(g4 + 1) * 4, :], b_[:].rearrange("p (a t) -> p a t", a=4), MASKL[:], ALU.mult, r=[bk, "MASKL"], w=[K("Lb", g4), bk])
            ycur = 0
            for g4 in range(4):
                hs = slice(g4 * 4, (g4 + 1) * 4)
                p.tt("pool", Yb[0][:, hs, :], identb[:].unsqueeze(1).to_broadcast([128, 4, 128]), ATB[:, hs, 256:384], ALU.subtract,
                     r=["identb", K("ATB", g4)], w=[K("Yb", 0, g4)])
            Ncur = lambda h: Lb[:, h, :]
            NTcur = lambda h: ATB[:, h, 256:384]
            nkey = lambda g4: K("Lb", g4)
            ntkey = lambda g4: K("ATB", g4)
            for lvl in range(6):
                pp_ = lvl % 2
                last = lvl == 5
                for g4 in range(4):
                    hs = slice(g4 * 4, (g4 + 1) * 4)
                    bA, kA = bank()
                    for hq in range(4):
                        h = g4 * 4 + hq
                        p.mm(bA[:, hq * 128:(hq + 1) * 128], NTcur(h), Ncur(h), True, True, r=[nkey(g4), ntkey(g4)], w=[kA])
                    p.cp(evq(), Nn[pp_][:, hs, :], bA[:].rearrange("p (a t) -> p a t", a=4), r=[kA], w=[K("Nn", pp_, g4), kA])
                    if not last:
                        bB, kB = bank()
                        for hq in range(4):
                            h = g4 * 4 + hq
                            p.mm(bB[:, hq * 128:(hq + 1) * 128], Ncur(h), NTcur(h), True, True, r=[nkey(g4), ntkey(g4)], w=[kB])
                        p.cp(evq(), NTn[pp_][:, hs, :], bB[:].rearrange("p (a t) -> p a t", a=4), r=[kB], w=[K("NTn", pp_, g4), kB])
                for g4 in range(4):
                    hs = slice(g4 * 4, (g4 + 1) * 4)
                    bC, kC = bank()
                    p.mm(bC[:], identb[:], Yb[ycur][:, hs, :].rearrange("p a t -> p (a t)"), True, False, r=["identb", K("Yb", ycur, g4)], w=[kC])
                    for hq in range(4):
                        h = g4 * 4 + hq
                        osl = bC[:, hq * 128:(hq + 1) * 128]
                        p.mm(osl, Nn[pp_][:, h, :], Yb[ycur][:, h, :], False, hq == 3, r=[K("Nn", pp_, g4), K("Yb", ycur, g4)], w=[kC])
                    p.cp(evq(), Yb[1 - ycur][:, hs, :], bC[:].rearrange("p (a t) -> p a t", a=4), r=[kC], w=[K("Yb", 1 - ycur, g4), kC])
                ycur = 1 - ycur
                Ncur = (lambda pp_: lambda h: Nn[pp_][:, h, :])(pp_)
                NTcur = (lambda pp_: lambda h: NTn[pp_][:, h, :])(pp_)
                nkey = (lambda pp_: lambda g4: K("Nn", pp_, g4))(pp_)
                ntkey = (lambda pp_: lambda g4: K("NTn", pp_, g4))(pp_)
            Y = Yb[ycur]
            for g8 in range(2):
                b_, bk = bank()
                for h8 in range(8):
                    h = g8 * 8 + h8
                    p.mm(b_[:, h8 * 64:(h8 + 1) * 64], ATB[:, h, 0:128], Vb_[:, h * 64:(h + 1) * 64], True, True, r=[K("ATB", h // 4), kVb], w=[bk])
                p.cp(evq(), Rhs[s][:, g8 * 8:(g8 + 1) * 8, 64:128], b_[:].rearrange("p (h n) -> p h n", h=8), r=[bk], w=[K("RhsB", s, g8), bk])
            for g4 in range(4):
                b_, bk = bank()
                for hq in range(4):
                    h = g4 * 4 + hq
                    p.mm(b_[:, hq * 128:(hq + 1) * 128], Y[:, h, :], Rhs[s][:, h, :], True, True, r=[K("Yb", ycur, g4), K("RhsA", s), K("RhsB", s, h // 8)], w=[bk])
                p.op("act", (lambda b_, g4: lambda e: e.mul(PUn[:, g4 * 4:(g4 + 1) * 4, :], b_[:].rearrange("p (a t) -> p a t", a=4), -1.0))(b_, g4), r=[bk], w=[K("PUn", g4), bk])
            for g8 in range(2):
                b_, bk = bank()
                for h8 in range(8):
                    h = g8 * 8 + h8
                    p.mm(b_[0:64, h8 * 64:(h8 + 1) * 64], PUn[:, h, 0:64], BG_[:, h * 64:(h + 1) * 64], True, True, r=[K("PUn", h // 4), kBG], w=[bk])
                p.cp(evq(), MTp[:, g8 * 8:(g8 + 1) * 8, :], b_[0:64, :].rearrange("p (h n) -> p h n", h=8), r=[bk], w=[K("MTp", g8), bk])
            for g4 in range(4):
                b_, bk = bank()
                for hq in range(4):
                    h = g4 * 4 + hq
                    osl = b_[0:64, hq * 128:(hq + 1) * 128]
                    p.mm(osl, identb[0:64, 0:64], KR[:, h, 128:256], True, False, r=["identb", "KR"], w=[bk])
                    p.mm(osl, PUn[:, h, 0:64], ATB[:, h, 384:512], False, True, r=[K("PUn", g4), K("ATB", g4)], w=[bk])
                p.cp(evq(), QT[:, g4 * 4:(g4 + 1) * 4, :], b_[0:64, :].rearrange("p (a t) -> p a t", a=4), r=[bk], w=[K("QT", g4), bk])
            for g8 in range(2):
                b_, bk = bank()
                for h8 in range(8):
                    h = g8 * 8 + h8
                    osl = b_[0:64, h8 * 64:(h8 + 1) * 64]
                    p.mm(osl, KG_[:, h * 64:(h + 1) * 64], Vb_[:, h * 64:(h + 1) * 64], True, False, r=[kKG, kVb], w=[bk])
                    p.mm(osl, BG_[:, h * 64:(h + 1) * 64], PUn[:, h, 64:128], False, True, r=[kBG, K("PUn", h // 4)], w=[bk])
                p.cp(evq(), Gf[:, g8 * 8:(g8 + 1) * 8, :], b_[0:64, :].rearrange("p (h n) -> p h n", h=8), r=[bk], w=[K("Gf", g8), bk])
            ybanks = []
            for g8 in range(2):
                b_, bk = PY[g8]
                ybanks.append((b_, bk))
                for h8 in range(8):
                    h = g8 * 8 + h8
                    osl = b_[:, h8 * 64:(h8 + 1) * 64]
                    p.mm(osl, ATB[:, h, 128:256], Vb_[:, h * 64:(h + 1) * 64], True, False, r=[K("ATB", h // 4), kVb], w=[bk])
                    p.mm(osl, ATB[:, h, 384:512], PUn[:, h, 64:128], False, False, r=[K("ATB", h // 4), K("PUn", h // 4)], w=[bk])
                    p.mm(osl, QT[:, h, :], Hb[:, h, :], False, True, r=[K("QT", h // 4), "Hb"], w=[bk])
            hb_ = []
            for g8 in range(2):
                b_, bk = bank()
                hb_.append((b_, bk))
                for h8 in range(8):
                    h = g8 * 8 + h8
                    p.mm(b_[0:64, h8 * 64:(h8 + 1) * 64], MTp[:, h, :], Hb[:, h, :], True, True, r=[K("MTp", g8), "Hb"], w=[bk])
            p.tt("pool", HG[:], H[:], ET[s][:].unsqueeze(2).to_broadcast([64, 16, 64]), ALU.mult, r=["H", K("ET", s)], w=["HG"])
            p.tt("pool", HG[:], HG[:], Gf[:], ALU.add, r=["HG", K("Gf", 0), K("Gf", 1)], w=["HG"])
            for g8 in range(2):
                b_, bk = hb_[g8]
                p.tt("dve", H[:, g8 * 8:(g8 + 1) * 8, :], b_[0:64, :].rearrange("p (h n) -> p h n", h=8), HG[:, g8 * 8:(g8 + 1) * 8, :], ALU.add, r=[bk, "HG"], w=["H", bk])
            p.cp("act", Hb[:], H[:], r=["H"], w=["Hb"])
            la = p.end_capture()
            nf = min(len(la), 100000)
            p.replay_merged(la[:nf], pendingB)
            p.replay_merged(la[nf:])
            p.capture()
            T_ = Tn[s]
            kT = K("Tn", s)
            for g8 in range(2):
                b_, bk = ybanks[g8]
                sl = slice(g8 * 512, (g8 + 1) * 512)
                p.red(sm[:, 0, g8 * 8:(g8 + 1) * 8], b_[:].rearrange("p (h n) -> p h n", h=8), r=[bk], w=["sm0", bk])
                p.act(T_[:, sl], b_[:], AF.Square, r=[bk], w=[kT, bk])
            p.red(sm[:, 1, :], T_[:].rearrange("p (h n) -> p h n", h=16), r=[kT], w=["sm1"])
            p.ts("dve", sm[:, 0, :], sm[:, 0, :], 1.0 / 64, None, ALU.mult, r=["sm0"], w=["sm0"])
            p.tt("dve", sm[:, 2, :], sm[:, 0, :], sm[:, 0, :], ALU.mult, r=["sm0"], w=["sm2"])
            p.stt("dve", sm[:, 3, :], sm[:, 1, :], 1.0 / 64, sm[:, 2, :], ALU.mult, ALU.subtract, r=["sm1", "sm2"], w=["sm3"])
            p.ts("dve", sm[:, 3, :], sm[:, 3, :], GN_EPS, None, ALU.add, r=["sm3"], w=["sm3"])
            p.act(sm[:, 3, :], sm[:, 3, :], AF.Ln, r=["sm3"], w=["sm3"])
            p.act(sm[:, 3, :], sm[:, 3, :], AF.Exp, scale=-0.5, r=["sm3"], w=["sm3"])
            for g8 in range(2):
                b_, bk = ybanks[g8]
                sl = slice(g8 * 512, (g8 + 1) * 512)
                p.tt("dve", T_[:, sl].rearrange("p (h n) -> p h n", h=8), b_[:].rearrange("p (h n) -> p h n", h=8),
                     sm[:, 0, g8 * 8:(g8 + 1) * 8].unsqueeze(2).to_broadcast([128, 8, 64]), ALU.subtract, r=[bk, "sm0"], w=[kT, bk])
            p.tt("dve", T_[:].rearrange("p (h n) -> p h n", h=16), T_[:].rearrange("p (h n) -> p h n", h=16),
                 sm[:, 3, :].unsqueeze(2).to_broadcast([128, 16, 64]), ALU.mult, r=[kT, "sm3"], w=[kT])
            p.tt("pool", T_[:], T_[:], GNg[:], ALU.mult, r=[kT, "GNg"], w=[kT])
            p.tt("pool", T_[:], T_[:], GNb[:], ALU.add, r=[kT, "GNb"], w=[kT])
            p.tt("dve", T_[:], T_[:], BV[s][:], ALU.add, r=[kT, K("BV", s)], w=[kT])
            p.tt("pool", YG[s][:], T_[:], Gg[s][:], ALU.mult, r=[kT, K("Gg", s)], w=[K("YG", s)])
            p.dma("sp", S["YG"][rows, :], YG[s][:], "st_YG%d" % s, r=[K("YG", s)], w=[K("S_YG", j)])
            pendingB = p.end_capture()
        p.replay_merged(pendingB)
        p.fence([K("S_YG", j) for j in range(NT)])
        p.emit()
    nc.all_engine_barrier()


def stage_rwkv3(p, pre, x_d, W, S, lng_d, lnb_d, h_out, acc=None):
    nc = p.nc
    with ExitStack() as es:
        p.begin(es)
        c = consts(p)
        identb = c["identb"]
        wo = p.sb("wo3", [128, 8, D], BF16)
        p.dma("pool", wo[:], W["wo"].rearrange("(c p) f -> p c f", p=128), "ld_wo3", w=["wo"])
        gbc = p.sb(pre + "gbc", [128, D], F32)
        bbc = p.sb(pre + "bbc", [128, D], F32)
        p.dma("sp", gbc[:], lng_d.partition_broadcast(128), pre + "gbc", w=[K(pre, "gbc")])
        p.dma("sp", bbc[:], lnb_d.partition_broadcast(128), pre + "bbc", w=[K(pre, "bbc")])
        yg = [p.sb("yg%d" % j, [128, D], BF16) for j in range(2)]
        ygT = [p.sb("ygT%d" % j, [128, 8, 128], BF16) for j in range(2)]
        xt = [p.sb("x3_%d" % j, [128, D], F32) for j in range(2)]
        z4 = [p.sb("z43_%d" % j, [128, 4, D], F32) for j in range(2)]
        sq = [p.sb("sq3_%d" % j, [128, D], F32) for j in range(2)]
        st4 = [p.sb("st43_%d" % j, [128, 8, 4], F32) for j in range(2)]
        TRb = [p.ps("TR3_%d" % j, [128, 1024], BF16) for j in range(2)]
        PZ = [p.ps("PZ%d" % j, [128, 512]) for j in range(6)]
        outk = []
        pend = []
        for gi in range(NT // 4):
            p.capture()
            zg = z4[gi % 2]
            zs, zks, outs = [], [], []
            for t in range(4):
                i = gi * 4 + t
                s = i % 2
                rows = slice(i * 128, (i + 1) * 128)
                p.dma("sp", yg[s][:], S["YG"][rows, :], "ld_yg%d" % s, w=[K("yg", s)])
                p.dma("sp", xt[s][:], x_d[rows, :], "ld_x3%d" % s, w=[K("x3", s)])
                for c8 in range(8):
                    p.tr(TRb[s][:, c8 * 128:(c8 + 1) * 128], yg[s][:, c8 * 128:(c8 + 1) * 128], identb[:], r=[K("yg", s), "identb"], w=[K("TR3", s)])
                p.cp("act", ygT[s][:], TRb[s][:].rearrange("p (c t) -> p c t", c=8), r=[K("TR3", s)], w=[K("ygT", s), K("TR3", s)])
                zk = K("z43", gi % 2, t)
                for half in range(2):
                    pz = PZ[(i * 2 + half) % 6]
                    kz = K("PZ", (i * 2 + half) % 6)
                    for c8 in range(8):
                        p.mm(pz[:], ygT[s][:, c8, :], wo[:, c8, half * 512:(half + 1) * 512], c8 == 0, c8 == 7, r=[K("ygT", s), "wo"], w=[kz])
                    sl = slice(half * 512, (half + 1) * 512)
                    p.stt("dve", zg[:, t, sl], xt[s][:, sl], ALPHA, pz[:], ALU.mult, ALU.add, r=[kz, K("x3", s)], w=[zk, kz])
                zs.append(zg[:, t, :])
                zks.append(zk)
                outs.append(h_out[rows, :] if acc is None else ("sb", acc[:, i, :]))
            la = p.end_capture()
            p.replay_merged(la, pend)
            p.capture()
            outk += ln_group(p, pre, zs, zks, gbc, bbc, outs, gi, st4[gi % 2], sq)
            pend = p.end_capture()
        p.replay_merged(pend)
        p.fence(outk)
        p.emit()
    nc.all_engine_barrier()


RW_SHAPES = (("mix", [6, D]), ("wr", [D, D]), ("wk", [D, D]), ("wv", [D, D]), ("wo", [D, D]), ("w0", [D]), ("w1", [D, 64]), ("w2", [64, D]),
             ("a0", [D]), ("a1", [D, 64]), ("a2", [64, D]), ("g1", [D, 128]), ("g2", [128, D]), ("kk", [D]), ("ka", [D]), ("rk", [16, 64]),
             ("gn_g", [D]), ("gn_b", [D]))
BNAMES = ("RT", "KKT", "KH", "BH", "KG", "BG", "Vb", "YG")


def build_full():
    nc = bass.Bass("TRN2", target_bir_lowering=False)

    def dt(n, s, kind="ExternalInput", t=F32):
        if kind is None:
            return nc.dram_tensor(n, s, t).ap()
        return nc.dram_tensor(n, s, t, kind=kind).ap()
    x_d = dt("x", [T, D])
    xT_d = dt("xT", [D, T])
    W = {n_: dt("rw_" + n_, shp) for n_, shp in RW_SHAPES}
    win = dt("fx_w_in", [D, 3088])
    bf = dt("fx_b_f", [16])
    fwo = dt("fx_wo", [D, D])
    rw = dt("router_w", [D, 16])
    rb = dt("router_bias", [16])
    wg = dt("moe_w_gate", [2, 16, D, 512])
    wu = dt("moe_w_up", [2, 16, D, 512])
    wd = dt("moe_w_down", [2, 16, 512, D])
    lng = dt("ln_g", [2, 2, D])
    lnb = dt("ln_b", [2, 2, D])
    out = dt("out", [T, D], "ExternalOutput")
    S = {n_: dt("S_" + n_, [T, D], None, BF16) for n_ in BNAMES}
    S["Gg"] = dt("S_Gg", [T, D], None)
    S["BV"] = dt("S_BV", [T, D], None)
    S["ET"] = dt("S_ET", [NT, 64, 16], None)
    h1 = dt("S_h1", [T, D], None)
    h2 = dt("S_h2", [T, D], None)
    h3 = dt("S_h3", [T, D], None)
    p = Prog(nc)
    stage_rwkv1(p, "r1", xT_d, W, S)
    stage_rwkv2(p, "r2", W, S)
    with ExitStack() as ea:
        p.es = ea
        acc0 = p.sb("acc_r", [128, NT, D], F32)
        stage_rwkv3(p, "r3", x_d, W, S, lng[0, 0], lnb[0, 0], h1, acc=acc0)
        stage_moe(p, "m0", h1, h2, rw, rb, wg[0], wu[0], wd[0], lng[0, 1], lnb[0, 1], acc=acc0)
    with ExitStack() as eb:
        p.es = eb
        acc1 = p.sb("acc_f", [128, NT, D], F32)
        stage_fox(p, "fx", h2, h3, win, bf, fwo, lng[1, 0], lnb[1, 0], acc=acc1)
        stage_moe(p, "m1", h3, out, rw, rb, wg[1], wu[1], wd[1], lng[1, 1], lnb[1, 1], acc=acc1)
    return nc


def kernel(**inputs):
    x = np.asarray(inputs["x"], dtype=np.float32)
    B = x.shape[0]
    shared = {}
    for n_, _ in RW_SHAPES:
        shared["rw_" + n_] = np.ascontiguousarray(np.asarray(inputs["rw_" + n_], dtype=np.float32)[0])
    shared["fx_w_in"] = np.ascontiguousarray(np.asarray(inputs["fx_w_in"], dtype=np.float32)[0])
    shared["fx_b_f"] = np.ascontiguousarray(np.asarray(inputs["fx_b_f"], dtype=np.float32)[0])
    shared["fx_wo"] = np.ascontiguousarray(np.asarray(inputs["fx_wo"], dtype=np.float32)[0])
    for n_ in ("router_w", "router_bias", "moe_w_gate", "moe_w_up", "moe_w_down", "ln_g", "ln_b"):
        shared[n_] = np.ascontiguousarray(np.asarray(inputs[n_], dtype=np.float32))
    nc = build_full()
    in_maps = []
    for b in range(B):
        m = dict(shared)
        m["x"] = np.ascontiguousarray(x[b])
        m["xT"] = np.ascontiguousarray(x[b].T)
        in_maps.append(m)
    res = run_bass_kernel_spmd(nc, in_maps, core_ids=list(range(B)))
    return np.stack([np.asarray(r["out"], dtype=np.float32) for r in res.results], axis=0)
```

```python
import numpy as np
from contextlib import ExitStack
import concourse.bass as bass
import concourse.mybir as mybir
from concourse.bass_utils import run_bass_kernel_spmd

F32 = mybir.dt.float32
BF16 = mybir.dt.bfloat16
AF = mybir.ActivationFunctionType
ALU = mybir.AluOpType
AX = mybir.AxisListType


STRICT = False


class Ins:
    __slots__ = ("q", "fn", "deps", "sem", "val", "dkey", "need", "n")

    def __init__(self, q, fn, dkey):
        self.q = q
        self.fn = fn
        self.dkey = dkey
        self.deps = {}
        self.need = False
        self.sem = None
        self.val = 0


class Prog:
    ENG = ("pe", "act", "dve", "pool", "sp")

    def __init__(self, nc):
        self.nc = nc
        self.begin(None)
        self.pool_es = ExitStack()
        self.esems = None
        self.dpool = {}
        self.base = {}

    def begin(self, es):
        self.es = es
        self.streams = {e: [] for e in self.ENG}
        self.lastw = {}
        self.readers = {}
        self.n = 0

    _uid = [0]

    def sb(self, name, shape, dt):
        Prog._uid[0] += 1
        return self.es.enter_context(self.nc.sbuf_tensor("%s_%d" % (name, Prog._uid[0]), list(shape), dt))

    def ps(self, name, shape, dt=F32):
        Prog._uid[0] += 1
        return self.es.enter_context(self.nc.psum_tensor("%s_%d" % (name, Prog._uid[0]), list(shape), dt))

    def capture(self):
        self._cap = []

    def end_capture(self):
        lst, self._cap = self._cap, None
        return lst

    def replay_merged(self, *lists):
        pos = [0] * len(lists)
        total = sum(len(l) for l in lists)
        for _ in range(total):
            best, bf = None, 2.0
            for k, l in enumerate(lists):
                if pos[k] < len(l):
                    f = pos[k] / len(l)
                    if f < bf:
                        best, bf = k, f
            a = lists[best][pos[best]]
            pos[best] += 1
            self.op(*a[0], **a[1])

    def op(self, q, fn, r=(), w=(), dma=None):
        if getattr(self, "_cap", None) is not None:
            self._cap.append(((q, fn), dict(r=list(r), w=list(w), dma=dma)))
            return None
        ins = Ins(q, fn, dma)
        ins.n = self.n
        self.n += 1
        for k in r:
            d = self.lastw.get(k)
            if d is not None:
                ins.deps[d] = "raw"
        for k in w:
            d = self.lastw.get(k)
            if d is not None and d not in ins.deps:
                ins.deps[d] = "waw"
            for d in self.readers.get(k, ()):
                if d not in ins.deps and d is not ins:
                    ins.deps[d] = "war"
        for k in w:
            self.lastw[k] = ins
            self.readers[k] = []
        for k in r:
            self.readers.setdefault(k, []).append(ins)
        self.streams[q].append(ins)
        return ins

    @staticmethod
    def _skip(i, d, kind):
        if d.dkey is not None:
            return False
        if i.dkey is None and d.q == i.q:
            if i.q == "pe":
                return True
            return False if STRICT else kind != "raw"
        return False


    def mm(self, out, lhsT, rhs, start=True, stop=True, r=(), w=()):
        return self.op("pe", lambda e: e.matmul(out, lhsT, rhs, start=start, stop=stop), r=r, w=w)

    def tr(self, out, in_, ident, r=(), w=()):
        return self.op("pe", lambda e: e.transpose(out, in_, ident), r=r, w=w)

    def act(self, out, in_, func, r=(), w=(), bias=None, scale=None):
        kw = {}
        if bias is not None:
            kw["bias"] = bias
        if scale is not None:
            kw["scale"] = scale
        return self.op("act", lambda e: e.activation(out, in_, func, **kw), r=r, w=w)

    def tt(self, q, out, in0, in1, op, r=(), w=()):
        return self.op(q, lambda e: e.tensor_tensor(out, in0, in1, op), r=r, w=w)

    def ts(self, q, out, in0, s1, s2, op0, op1=None, r=(), w=()):
        if op1 is None:
            return self.op(q, lambda e: e.tensor_scalar(out, in0, s1, None, op0), r=r, w=w)
        return self.op(q, lambda e: e.tensor_scalar(out, in0, s1, s2, op0, op1), r=r, w=w)

    def stt(self, q, out, in0, scalar, in1, op0, op1, r=(), w=()):
        return self.op(q, lambda e: e.scalar_tensor_tensor(out, in0, scalar, in1, op0, op1), r=r, w=w)

    def cp(self, q, out, in_, r=(), w=()):
        if q == "act":
            return self.op(q, lambda e: e.copy(out, in_), r=r, w=w)
        return self.op(q, lambda e: e.tensor_copy(out, in_), r=r, w=w)

    def red(self, out, in_, op=None, r=(), w=()):
        op = op or ALU.add
        return self.op("dve", lambda e: e.tensor_reduce(out, in_, AX.X, op), r=r, w=w)

    def rcp(self, out, in_, r=(), w=()):
        return self.op("dve", lambda e: e.reciprocal(out, in_), r=r, w=w)

    def dma(self, q, out, in_, dkey, r=(), w=(), slow=False):
        if slow:
            return self.op(q, lambda e: e.dma_start(out=out, in_=in_, allow_slow_non_contiguous=True), r=r, w=w, dma=dkey)
        return self.op(q, lambda e: e.dma_start(out=out, in_=in_), r=r, w=w, dma=dkey)

    def fence(self, keys):
        return self.op("sp", None, r=keys)

    def sem(self, name):
        Prog._uid[0] += 1
        return self.es.enter_context(self.nc.semaphore("%s_%d" % (name, Prog._uid[0])))

    def emit(self):
        nc = self.nc
        es = self.es
        for q in self.ENG:
            for i in self.streams[q]:
                for d, kind in i.deps.items():
                    if not self._skip(i, d, kind):
                        d.need = True
        if self.esems is None:
            self.esems = {}
            for q in ("pe", "act", "dve", "pool"):
                Prog._uid[0] += 1
                self.esems[q] = self.pool_es.enter_context(nc.semaphore("s_%s_%d" % (q, Prog._uid[0])))
                self.base[id(self.esems[q])] = 0
        sems = self.esems
        dsem = {}
        dcnt = {}
        nused = {}
        for q in self.ENG:
            cnt = self.base[id(sems[q])] if q in sems else 0
            for i in self.streams[q]:
                if i.dkey is not None:
                    if i.dkey not in dsem:
                        dp = self.dpool.setdefault(q, [])
                        used = nused.setdefault(q, 0)
                        nused[q] = used + 1
                        if used >= len(dp):
                            Prog._uid[0] += 1
                            sm_ = self.pool_es.enter_context(nc.semaphore("d%s_%d" % (q, Prog._uid[0])))
                            dp.append(sm_)
                            self.base[id(sm_)] = 0
                        dsem[i.dkey] = dp[used]
                        dcnt[i.dkey] = self.base[id(dp[used])]
                    dcnt[i.dkey] += 16
                    i.sem = dsem[i.dkey]
                    i.val = dcnt[i.dkey]
                elif i.need:
                    cnt += 1
                    i.sem = sems[q]
                    i.val = cnt
            if q in sems:
                self.base[id(sems[q])] = cnt
        for k_, sm_ in dsem.items():
            self.base[id(sm_)] = dcnt[k_]
        block = es.enter_context(nc.Block())

        def run(q, eng):
            waited = {}
            for i in self.streams[q]:
                for d, kind in i.deps.items():
                    if self._skip(i, d, kind):
                        continue
                    sid = id(d.sem)
                    if waited.get(sid, 0) >= d.val:
                        continue
                    eng.wait_ge(d.sem, d.val)
                    waited[sid] = d.val
                if i.fn is None:
                    continue
                if i.dkey is not None and i.val - 16 > 0:
                    sid = id(i.sem)
                    if waited.get(sid, 0) < i.val - 16:
                        eng.wait_ge(i.sem, i.val - 16)
                        waited[sid] = i.val - 16
                bi = i.fn(eng)
                if i.dkey is not None:
                    bi.then_inc(i.sem, 16)
                elif i.need:
                    bi.then_inc(i.sem, 1)

        @block.tensor
        def _(e):
            run("pe", e)

        @block.scalar
        def _(e):
            run("act", e)

        @block.vector
        def _(e):
            run("dve", e)

        @block.gpsimd
        def _(e):
            run("pool", e)

        @block.sync
        def _(e):
            run("sp", e)


T = 2048
D = 1024
NT = 16
ALPHA = 4.0 ** 0.25
LN_EPS = 1e-5
GN_EPS = 64e-5


def K(name, *idx):
    return (name,) + idx


def consts(p):
    c = {}
    ident = p.sb("ident", [128, 128], F32)
    identb = p.sb("identb", [128, 128], BF16)
    p.op("pool", lambda e: e.memset(ident[:], 0.0), w=["ident"])
    p.op("pool", lambda e: e.affine_select(out=ident[:], in_=ident[:], pattern=[[-1, 128]],
                                           compare_op=ALU.not_equal, fill=1.0, base=0, channel_multiplier=1),
         r=["ident"], w=["ident"])
    p.op("pool", lambda e: e.tensor_copy(identb[:], ident[:]), r=["ident"], w=["identb"])
    c["ident"] = ident
    c["identb"] = identb
    return c


def layer_norm_tiles(p, pre, acc, g_d, b_d, out_d, tmp):
    nc = p.nc
    gbc = p.sb(pre + "gbc", [128, D], F32)
    bbc = p.sb(pre + "bbc", [128, D], F32)
    p.op("sp", lambda e: e.dma_start(out=gbc[:], in_=g_d.partition_broadcast(128)), w=[K(pre, "gbc")], dma=pre + "gbc")
    p.op("sp", lambda e: e.dma_start(out=bbc[:], in_=b_d.partition_broadcast(128)), w=[K(pre, "bbc")], dma=pre + "bbc")
    s1 = p.sb(pre + "s1", [128, NT], F32)
    s2 = p.sb(pre + "s2", [128, NT], F32)
    st = p.sb(pre + "st", [128, 4, NT], F32)
    for i in range(NT):
        p.op("dve", (lambda i: lambda e: e.reduce_sum(s1[:, i:i + 1], acc[:, i, :], axis=AX.X))(i),
             r=[K("acc", i)], w=[K(pre, "s1", i)])
        sl = tmp[i % 2]
        p.op("act", (lambda i, sl: lambda e: e.activation(sl[:], acc[:, i, :], AF.Square))(i, sl),
             r=[K("acc", i)], w=[K(pre, "sq", i % 2)])
        p.op("dve", (lambda i, sl: lambda e: e.reduce_sum(s2[:, i:i + 1], sl[:], axis=AX.X))(i, sl),
             r=[K(pre, "sq", i % 2)], w=[K(pre, "s2", i)])
    allk1 = [K(pre, "s1", i) for i in range(NT)]
    allk2 = [K(pre, "s2", i) for i in range(NT)]
    mean, m2, var, rstd = st[:, 0, :], st[:, 1, :], st[:, 2, :], st[:, 3, :]
    p.op("dve", lambda e: e.tensor_scalar(mean, s1[:], 1.0 / D, None, ALU.mult), r=allk1, w=[K(pre, "mean")])
    p.op("dve", lambda e: e.tensor_tensor(m2, mean, mean, ALU.mult), r=[K(pre, "mean")], w=[K(pre, "m2")])
    p.op("dve", lambda e: e.scalar_tensor_tensor(var, s2[:], 1.0 / D, m2, ALU.mult, ALU.subtract),
         r=allk2 + [K(pre, "m2")], w=[K(pre, "var")])
    p.op("dve", lambda e: e.tensor_scalar(var, var, LN_EPS, None, ALU.add), r=[K(pre, "var")], w=[K(pre, "var")])
    p.op("act", lambda e: e.activation(var, var, AF.Sqrt), r=[K(pre, "var")], w=[K(pre, "var")])
    p.op("dve", lambda e: e.reciprocal(rstd, var), r=[K(pre, "var")], w=[K(pre, "rstd")])
    p.op("dve", lambda e: e.scalar_tensor_tensor(m2, mean, -1.0, rstd, ALU.mult, ALU.mult),
         r=[K(pre, "mean"), K(pre, "rstd")], w=[K(pre, "m2")])
    for i in range(NT):
        p.op("act", (lambda i: lambda e: e.activation(acc[:, i, :], acc[:, i, :], AF.Identity,
                                                      bias=m2[:, i:i + 1], scale=rstd[:, i:i + 1]))(i),
             r=[K("acc", i), K(pre, "m2"), K(pre, "rstd")], w=[K("acc", i)])
        p.op("dve", (lambda i: lambda e: e.tensor_tensor(acc[:, i, :], acc[:, i, :], gbc[:], ALU.mult))(i),
             r=[K("acc", i), K(pre, "gbc")], w=[K("acc", i)])
        p.op("dve" if i % 2 == 0 else "pool", (lambda i: lambda e: e.tensor_tensor(acc[:, i, :], acc[:, i, :], bbc[:], ALU.add))(i),
             r=[K("acc", i), K(pre, "bbc")], w=[K("acc", i)])
        p.op("sp", (lambda i: lambda e: e.dma_start(out=out_d[i * 128:(i + 1) * 128, :], in_=acc[:, i, :]))(i),
             r=[K("acc", i)], w=[K(pre, "out", i)], dma=pre + "st%d" % (i % 4))
    return [K(pre, "out", i) for i in range(NT)]


def stage_moe(p, pre, h_in, h_out, rw_d, rb_d, wg_d, wu_d, wd_d, lng_d, lnb_d, acc=None):
    es = ExitStack()
    with es:
        p.begin(es)
        c = consts(p)
        preloaded = acc is not None
        if not preloaded:
            acc = p.sb(pre + "acc", [128, NT, D], F32)
        hFM = p.sb(pre + "hFM", [128, 8, T], BF16)
        hF32 = [p.sb(pre + "hF32_%d" % j, [128, 8, 128], F32) for j in range(2)]
        rw = p.sb(pre + "rw", [128, 8, 16], F32)
        rbb = p.sb(pre + "rbb", [128, 16], F32)
        lg = p.sb(pre + "lg", [128, NT, 16], F32)
        tmp = [p.sb(pre + "tmp%d" % j, [128, D], F32) for j in range(2)]
        ps_tr = [p.ps(pre + "ps_tr%d" % j, [128, 512]) for j in range(2)]
        ps_g = [p.ps(pre + "ps_g%d" % j, [128, 512]) for j in range(2)]
        ps_u = [p.ps(pre + "ps_u%d" % j, [128, 512]) for j in range(2)]
        ps_d = [p.ps(pre + "ps_d%d" % j, [128, 512]) for j in range(2)]
        wg = [p.sb(pre + "wg%d" % j, [128, 8, 512], BF16) for j in range(2)]
        wu = [p.sb(pre + "wu%d" % j, [128, 8, 512], BF16) for j in range(2)]
        wd = [p.sb(pre + "wd%d" % j, [128, 4, 1024], BF16) for j in range(2)]
        hid = [p.sb(pre + "hid%d" % j, [128, 4, 512], BF16) for j in range(2)]
        sg = [p.sb(pre + "sg%d" % j, [128, 512], F32) for j in range(2)]

        for i in range(NT if not preloaded else 0):
            p.op("sp", (lambda i: lambda e: e.dma_start(out=acc[:, i, :], in_=h_in[i * 128:(i + 1) * 128, :]))(i),
                 w=[K("acc", i)], dma=pre + "ld%d" % (i % 4))
        p.op("sp", lambda e: e.dma_start(out=rw[:], in_=rw_d.rearrange("(c p) e -> p c e", p=128)), w=[K(pre, "rw")], dma=pre + "rw")
        p.op("sp", lambda e: e.dma_start(out=rbb[:], in_=rb_d.partition_broadcast(128)), w=[K(pre, "rbb")], dma=pre + "rbb")

        def load_w(e_, after=()):
            s = e_ % 2
            p.op("pool", lambda e: e.dma_start(out=wg[s][:], in_=wg_d[e_].rearrange("(c p) f -> p c f", p=128)),
                 r=list(after), w=[K(pre, "wg", s)], dma=pre + "wg%d" % s)
            p.op("pool", lambda e: e.dma_start(out=wu[s][:], in_=wu_d[e_].rearrange("(c p) f -> p c f", p=128)),
                 r=list(after), w=[K(pre, "wu", s)], dma=pre + "wu%d" % s)
            p.op("pool", lambda e: e.dma_start(out=wd[s][:], in_=wd_d[e_].rearrange("(c p) f -> p c f", p=128)),
                 r=list(after), w=[K(pre, "wd", s)], dma=pre + "wd%d" % s)
        load_w(0)
        load_w(1)

        for i in range(NT):
            for half in range(2):
                pt = ps_tr[half]
                for cc in range(4):
                    c8 = half * 4 + cc
                    p.op("pe", (lambda i, c8, cc, pt: lambda e: e.transpose(pt[:, cc * 128:(cc + 1) * 128], acc[:, i, c8 * 128:(c8 + 1) * 128], c["ident"][:]))(i, c8, cc, pt),
                         r=[K("acc", i), "ident"], w=[K(pre, "ps_tr", half)])
                f32t = hF32[i % 2]
                p.op("act", (lambda i, half, pt, f32t: lambda e: e.copy(f32t[:, half * 4:(half + 1) * 4, :], pt[:].rearrange("p (c t) -> p c t", c=4)))(i, half, pt, f32t),
                     r=[K(pre, "ps_tr", half)], w=[K(pre, "hF32", i % 2, half), K(pre, "ps_tr", half)])
                p.op("dve", (lambda i, half, pt: lambda e: e.tensor_copy(hFM[:, half * 4:(half + 1) * 4, i * 128:(i + 1) * 128], pt[:].rearrange("p (c t) -> p c t", c=4)))(i, half, pt),
                     r=[K(pre, "ps_tr", half)], w=[K(pre, "hFM", i // 4), K(pre, "ps_tr", half)])
            pl = ps_d[i % 2]
            for c8 in range(8):
                p.op("pe", (lambda i, c8, pl: lambda e: e.matmul(pl[:, 0:16], hF32[i % 2][:, c8, :], rw[:, c8, :], start=(c8 == 0), stop=(c8 == 7)))(i, c8, pl),
                     r=[K(pre, "hF32", i % 2, c8 // 4), K(pre, "rw")], w=[K(pre, "ps_d", i % 2)])
            p.op("act", (lambda i, pl: lambda e: e.copy(lg[:, i, :], pl[:, 0:16]))(i, pl), r=[K(pre, "ps_d", i % 2)], w=[K(pre, "lg")])
            p.op("act", (lambda i: lambda e: e.mul(acc[:, i, :], acc[:, i, :], ALPHA))(i), r=[K("acc", i)], w=[K("acc", i)])

        S = p.sb(pre + "S", [128, NT, 16], F32)
        SS = p.sb(pre + "SS", [128, NT, 16], F32)
        CMP = p.sb(pre + "CMP", [128, NT * 4, 4, 4], F32)
        RK = p.sb(pre + "RK", [128, NT * 4, 4], F32)
        GS = p.sb(pre + "GS", [128, NT, 4], F32)
        GM = p.sb(pre + "GM", [128, NT], F32)
        G = p.sb(pre + "G", [128, NT, 16], F32)
        kr = K(pre, "router")
        p.op("act", lambda e: e.activation(S[:], lg[:], AF.Sigmoid), r=[K(pre, "lg")], w=[K(pre, "S")])
        p.op("dve", lambda e: e.tensor_tensor(SS[:], S[:], rbb[:].unsqueeze(1).to_broadcast([128, NT, 16]), ALU.add),
             r=[K(pre, "S"), K(pre, "rbb")], w=[kr])
        ssv = SS[:].rearrange("p i (g a) -> p (i g) a", g=4)
        in_b = ssv.unsqueeze(2).to_broadcast([128, NT * 4, 4, 4])
        in_a = ssv.unsqueeze(3).to_broadcast([128, NT * 4, 4, 4])
        p.op("dve", lambda e: e.tensor_tensor(CMP[:], in_b, in_a, ALU.is_gt), r=[kr], w=[kr])
        p.op("dve", lambda e: e.tensor_reduce(RK[:], CMP[:], AX.X, ALU.add), r=[kr], w=[kr])
        p.op("dve", lambda e: e.tensor_scalar(RK[:], RK[:], 1.5, None, ALU.is_lt), r=[kr], w=[kr])
        p.op("dve", lambda e: e.tensor_tensor(CMP[:, :, 0, :], ssv, RK[:], ALU.mult), r=[kr], w=[kr])
        p.op("dve", lambda e: e.tensor_reduce(GS[:].rearrange("p i g -> p (i g)"), CMP[:, :, 0, :], AX.X, ALU.add), r=[kr], w=[kr])
        p.op("dve", lambda e: e.tensor_reduce(GM[:], GS[:], AX.X, ALU.max), r=[kr], w=[kr])
        p.op("dve", lambda e: e.tensor_tensor(GS[:], GS[:], GM[:].unsqueeze(2).to_broadcast([128, NT, 4]), ALU.is_ge), r=[kr], w=[kr])
        p.op("dve", lambda e: e.tensor_tensor(G[:].rearrange("p i (g a) -> p i g a", g=4), RK[:].rearrange("p (i g) a -> p i g a", g=4),
                                              GS[:].unsqueeze(3).to_broadcast([128, NT, 4, 4]), ALU.mult), r=[kr], w=[kr])
        p.op("dve", lambda e: e.tensor_tensor(G[:], G[:], S[:], ALU.mult), r=[kr, K(pre, "S")], w=[kr])
        p.op("dve", lambda e: e.tensor_reduce(GM[:], G[:], AX.X, ALU.add), r=[kr], w=[kr])
        p.op("dve", lambda e: e.reciprocal(GM[:], GM[:]), r=[kr], w=[kr])
        p.op("dve", lambda e: e.tensor_tensor(G[:], G[:], GM[:].unsqueeze(2).to_broadcast([128, NT, 16]), ALU.mult), r=[kr], w=[kr])

        gbc = p.sb(pre + "gbc", [128, D], F32)
        bbc = p.sb(pre + "bbc", [128, D], F32)
        st4 = [p.sb(pre + "st4_%d" % j, [128, 8, 4], F32) for j in range(2)]
        p.dma("sp", gbc[:], lng_d.partition_broadcast(128), pre + "gbc", w=[K(pre + "ln", "gbc")])
        p.dma("sp", bbc[:], lnb_d.partition_broadcast(128), pre + "bbc", w=[K(pre + "ln", "bbc")])
        outk = []
        pendln = []
        pd4 = [(ps_d[0], K(pre, "ps_d", 0)), (ps_d[1], K(pre, "ps_d", 1)), (ps_tr[0], K(pre, "ps_tr", 0)), (ps_tr[1], K(pre, "ps_tr", 1))]
        n = 0
        for e_ in range(16):
            s = e_ % 2
            for tb in range(4):
                if e_ == 15:
                    p.capture()
                hb = hid[tb % 2]
                for hc in range(4):
                    j = n % 2
                    n += 1
                    for c8 in range(8):
                        p.op("pe", (lambda s, hc, c8, tb, j: lambda e: e.matmul(ps_g[j][:], wg[s][:, c8, hc * 128:(hc + 1) * 128], hFM[:, c8, tb * 512:(tb + 1) * 512], start=(c8 == 0), stop=(c8 == 7)))(s, hc, c8, tb, j),
                             r=[K(pre, "wg", s), K(pre, "hFM", tb)], w=[K(pre, "ps_g", j)])
                    for c8 in range(8):
                        p.op("pe", (lambda s, hc, c8, tb, j: lambda e: e.matmul(ps_u[j][:], wu[s][:, c8, hc * 128:(hc + 1) * 128], hFM[:, c8, tb * 512:(tb + 1) * 512], start=(c8 == 0), stop=(c8 == 7)))(s, hc, c8, tb, j),
                             r=[K(pre, "wu", s), K(pre, "hFM", tb)], w=[K(pre, "ps_u", j)])
                    p.op("act", (lambda j: lambda e: e.activation(sg[j][:], ps_g[j][:], AF.Silu))(j),
                         r=[K(pre, "ps_g", j)], w=[K(pre, "sg", j)])
                    p.op("dve", (lambda j, hb, hc: lambda e: e.tensor_tensor(hb[:, hc, :], sg[j][:], ps_u[j][:], ALU.mult))(j, hb, hc),
                         r=[K(pre, "sg", j), K(pre, "ps_u", j)], w=[K(pre, "hid", tb % 2, hc)])
                for tt in range(4):
                    i = tb * 4 + tt
                    for half in range(2):
                        j = (tt * 2 + half) % 4
                        pdj, kdj = pd4[j]
                        for hc in range(4):
                            p.op("pe", (lambda s, hc, tt, half, pdj, hb: lambda e: e.matmul(pdj[:], hb[:, hc, tt * 128:(tt + 1) * 128], wd[s][:, hc, half * 512:(half + 1) * 512], start=(hc == 0), stop=(hc == 3)))(s, hc, tt, half, pdj, hb),
                                 r=[K(pre, "hid", tb % 2, hc), K(pre, "wd", s)], w=[kdj])
                        p.op("dve", (lambda i, half, pdj, e_: lambda e: e.scalar_tensor_tensor(acc[:, i, half * 512:(half + 1) * 512], pdj[:], G[:, i, e_:e_ + 1], acc[:, i, half * 512:(half + 1) * 512], ALU.mult, ALU.add))(i, half, pdj, e_),
                             r=[kdj, kr, K("acc", i)], w=[K("acc", i), kdj])
                if e_ == 15:
                    la = p.end_capture()
                    p.replay_merged(la, pendln)
                    zs = [acc[:, tb * 4 + t, :] for t in range(4)]
                    zks = [K("acc", tb * 4 + t) for t in range(4)]
                    outs = [h_out[(tb * 4 + t) * 128:(tb * 4 + t + 1) * 128, :] for t in range(4)]
                    p.capture()
                    outk += ln_group(p, pre + "ln", zs, zks, gbc, bbc, outs, tb, st4[tb % 2], tmp)
                    pendln = p.end_capture()
                    if tb == 3:
                        p.replay_merged(pendln)
            if e_ + 2 < 16:
                load_w(e_ + 2)
        p.op("sp", None, r=outk)
        p.emit()
    p.nc.all_engine_barrier()


def ln_tile(p, pre, z, gbc, bbc, out_rows, i, zk, st, sq):
    j = i % 2
    ks = K(pre, "lnst", j)
    p.op("dve", lambda e: e.reduce_sum(st[:, 0:1], z, axis=AX.X), r=[zk], w=[ks])
    p.op("act", lambda e: e.activation(sq[:], z, AF.Square), r=[zk], w=[K(pre, "lnsq", j)])
    p.op("dve", lambda e: e.reduce_sum(st[:, 1:2], sq[:], axis=AX.X), r=[K(pre, "lnsq", j), ks], w=[ks])
    p.op("dve", lambda e: e.tensor_scalar(st[:, 0:2], st[:, 0:2], 1.0 / D, None, ALU.mult), r=[ks], w=[ks])
    p.op("dve", lambda e: e.tensor_tensor(st[:, 2:3], st[:, 0:1], st[:, 0:1], ALU.mult), r=[ks], w=[ks])
    p.op("dve", lambda e: e.tensor_tensor(st[:, 3:4], st[:, 1:2], st[:, 2:3], ALU.subtract), r=[ks], w=[ks])
    p.op("dve", lambda e: e.tensor_scalar(st[:, 3:4], st[:, 3:4], LN_EPS, None, ALU.add), r=[ks], w=[ks])
    p.op("act", lambda e: e.activation(st[:, 4:5], st[:, 3:4], AF.Sqrt), r=[ks], w=[ks])
    p.op("dve", lambda e: e.reciprocal(st[:, 5:6], st[:, 4:5]), r=[ks], w=[ks])
    p.op("dve", lambda e: e.scalar_tensor_tensor(st[:, 6:7], st[:, 0:1], -1.0, st[:, 5:6], ALU.mult, ALU.mult), r=[ks], w=[ks])
    p.op("act", lambda e: e.activation(z, z, AF.Identity, bias=st[:, 6:7], scale=st[:, 5:6]), r=[zk, ks], w=[zk])
    p.op("dve", lambda e: e.tensor_tensor(z, z, gbc[:], ALU.mult), r=[zk, K(pre, "gbc")], w=[zk])
    p.op("pool", lambda e: e.tensor_tensor(z, z, bbc[:], ALU.add), r=[zk, K(pre, "bbc")], w=[zk])
    ok = K(pre, "out", i)
    p.op("sp", lambda e: e.dma_start(out=out_rows, in_=z), r=[zk], w=[ok], dma=pre + "st%d" % j)
    return ok


import os
CUT = int(os.environ.get('FOX_CUT', '0'))
CUT2 = int(os.environ.get('FOX_CUT2', '0'))


def ln_group(p, pre, zs, zks, gbc, bbc, outs, gi, st, sq):
    n = len(zs)
    j = gi % 2
    ks = K(pre, "lnst", j)
    for t in range(n):
        sqt = sq[t % len(sq)]
        kq = K(pre, "lnsq", t % len(sq))
        p.op("act", (lambda sqt, z, o_: lambda e: e.activation(sqt[:], z, AF.Identity, accum_out=o_))(sqt, zs[t], st[:, 0, t:t + 1]), r=[zks[t], ks], w=[kq, ks])
        p.op("act", (lambda sqt, z, o_: lambda e: e.activation(sqt[:], z, AF.Square, accum_out=o_))(sqt, zs[t], st[:, 1, t:t + 1]), r=[zks[t], ks], w=[kq, ks])
    p.ts("dve", st[:, 0:2, 0:n], st[:, 0:2, 0:n], 1.0 / D, None, ALU.mult, r=[ks], w=[ks])
    p.tt("dve", st[:, 2, 0:n], st[:, 0, 0:n], st[:, 0, 0:n], ALU.mult, r=[ks], w=[ks])
    p.tt("dve", st[:, 3, 0:n], st[:, 1, 0:n], st[:, 2, 0:n], ALU.subtract, r=[ks], w=[ks])
    p.ts("dve", st[:, 3, 0:n], st[:, 3, 0:n], LN_EPS, None, ALU.add, r=[ks], w=[ks])
    p.act(st[:, 4, 0:n], st[:, 3, 0:n], AF.Ln, r=[ks], w=[ks])
    p.act(st[:, 5, 0:n], st[:, 4, 0:n], AF.Exp, scale=-0.5, r=[ks], w=[ks])
    p.stt("dve", st[:, 6, 0:n], st[:, 0, 0:n], -1.0, st[:, 5, 0:n], ALU.mult, ALU.mult, r=[ks], w=[ks])
    oks = []
    for t in range(n):
        z = zs[t]
        p.act(z, z, AF.Identity, bias=st[:, 6, t:t + 1], scale=st[:, 5, t:t + 1], r=[zks[t], ks], w=[zks[t]])
        p.tt("dve", z, z, gbc[:], ALU.mult, r=[zks[t], K(pre, "gbc")], w=[zks[t]])
        ok = K(pre, "out", gi, t)
        if isinstance(outs[t], tuple):
            p.tt("pool", outs[t][1], z, bbc[:], ALU.add, r=[zks[t], K(pre, "bbc")], w=[ok])
        else:
            p.tt("pool", z, z, bbc[:], ALU.add, r=[zks[t], K(pre, "bbc")], w=[zks[t]])
            p.dma("sp", outs[t], z, pre + "st%d_%d" % (j, t), r=[zks[t]], w=[ok])
        oks.append(ok)
    return oks


def stage_fox(p, pre, h_in, h_out, win_d, bf_d, wo_d, lng_d, lnb_d, dbg=None, acc=None):
    nc = p.nc
    HG = 4
    NG = 16 // HG
    outer = ExitStack()
    with outer:
        p.es = outer
        hFM = p.sb(pre + "hFM", [128, 8, T], BF16)
        csplit = p.sb(pre + "csplit", [80, T], BF16)
        ckTM = p.sb(pre + "ckTM", [128, NT, 16], F32)
        with ExitStack() as es:
            p.begin(es)
            c = consts(p)
            ht = [p.sb(pre + "ht%d" % j, [128, D], F32) for j in range(2)]
            hF32 = [p.sb(pre + "hF32_%d" % j, [128, 8, 128], F32) for j in range(2)]
            wf = p.sb(pre + "wf", [128, 8, 16], F32)
            nbf = p.sb(pre + "nbf", [16, 1], F32)
            fT = p.sb(pre + "fT", [16, T], F32)
            A = p.sb(pre + "A", [16, T], F32)
            B = p.sb(pre + "B", [16, T], F32)
            hi = p.sb(pre + "hi", [16, T], BF16)
            ps_tr = [p.ps(pre + "ps_tr%d" % j, [128, 512]) for j in range(4)]
            ps_f = [p.ps(pre + "ps_f%d" % j, [128, 512]) for j in range(2)]
            p.op("sp", lambda e: e.dma_start(out=wf[:], in_=win_d[:, 3072:3088].rearrange("(c p) e -> p c e", p=128)), w=["wf"], dma=pre + "wf")
            p.op("sp", lambda e: e.dma_start(out=nbf[:], in_=bf_d.rearrange("(h o) -> h o", o=1)), w=["nbf"], dma=pre + "nbf")
            p.op("dve", lambda e: e.tensor_scalar(nbf[:], nbf[:], -1.0, None, ALU.mult), r=["nbf"], w=["nbf"])
            p.op("pool", lambda e: e.memset(csplit[:], 0.0), w=["csplit"])
            for i in range(NT):
                hti = ht[i % 2]
                p.op("sp", (lambda i, hti: lambda e: e.dma_start(out=hti[:], in_=h_in[i * 128:(i + 1) * 128, :]))(i, hti),
                     w=[K("ht", i % 2)], dma=pre + "ht%d" % (i % 2))
                for half in range(2):
                    pt = ps_tr[(i % 2) * 2 + half]
                    for cc in range(4):
                        c8 = half * 4 + cc
                        p.op("pe", (lambda hti, c8, cc, pt: lambda e: e.transpose(pt[:, cc * 128:(cc + 1) * 128], hti[:, c8 * 128:(c8 + 1) * 128], c["ident"][:]))(hti, c8, cc, pt),
                             r=[K("ht", i % 2), "ident"], w=[K("ps_tr", (i % 2) * 2 + half)])
                    f32t = hF32[i % 2]
                    p.op("act", (lambda half, pt, f32t: lambda e: e.copy(f32t[:, half * 4:(half + 1) * 4, :], pt[:].rearrange("p (c t) -> p c t", c=4)))(half, pt, f32t),
                         r=[K("ps_tr", (i % 2) * 2 + half)], w=[K("hF32", i % 2, half), K("ps_tr", (i % 2) * 2 + half)])
                    p.op("dve", (lambda i, half, pt: lambda e: e.tensor_copy(hFM[:, half * 4:(half + 1) * 4, i * 128:(i + 1) * 128], pt[:].rearrange("p (c t) -> p c t", c=4)))(i, half, pt),
                         r=[K("ps_tr", (i % 2) * 2 + half)], w=[K("hFM", i), K("ps_tr", (i % 2) * 2 + half)])
                if CUT == 1:
                    continue
                pf = ps_f[(i // 4) % 2]
                for c8 in range(8):
                    p.op("pe", (lambda i, c8, pf: lambda e: e.matmul(pf[0:16, (i % 4) * 128:(i % 4 + 1) * 128], wf[:, c8, :], hF32[i % 2][:, c8, :], start=(c8 == 0), stop=(c8 == 7)))(i, c8, pf),
                         r=[K("hF32", i % 2, c8 // 4), "wf"], w=[K("ps_f", (i // 4) % 2)])
                if i % 4 == 3:
                    tb = i // 4
                    p.op("act", (lambda tb, pf: lambda e: e.activation(fT[:, tb * 512:(tb + 1) * 512], pf[0:16, :], AF.Exp, bias=nbf[:], scale=-1.0))(tb, pf),
                         r=[K("ps_f", tb % 2), "nbf"], w=[K("fT", tb), K("ps_f", tb % 2)])
                    p.op("act", (lambda tb: lambda e: e.activation(A[:, tb * 512:(tb + 1) * 512], fT[:, tb * 512:(tb + 1) * 512], AF.Ln, bias=1.0))(tb),
                         r=[K("fT", tb)], w=[id(A)])
            cur, oth = A, B
            s = 1
            while s < T and CUT not in (1, 2):
                p.op("dve", (lambda cur, oth, s: lambda e: e.tensor_tensor(oth[:, s:T], cur[:, s:T], cur[:, 0:T - s], ALU.add))(cur, oth, s),
                     r=[id(cur)], w=[id(oth)])
                p.op("act", (lambda cur, oth, s: lambda e: e.copy(oth[:, 0:s], cur[:, 0:s]))(cur, oth, s),
                     r=[id(cur)], w=[id(oth)])
                cur, oth = oth, cur
                s *= 2
            csum = cur
            for i in range(NT if CUT not in (1, 2, 3) else 0):
                p.op("pe", (lambda i: lambda e: e.transpose(ps_tr[0][:, i * 16:(i + 1) * 16], csum[:, i * 128:(i + 1) * 128], c["ident"][0:16, 0:16]))(i),
                     r=[id(csum), "ident"], w=[K("ps_tr", 0)])
            p.op("act", lambda e: e.copy(ckTM[:].rearrange("p i h -> p (i h)"), ps_tr[0][:, 0:256]), r=[K("ps_tr", 0)], w=["ckTM", K("ps_tr", 0)])
            m8 = oth
            p.op("dve", lambda e: e.tensor_scalar(m8[:], csum[:], -8.0, None, ALU.mult), r=[id(csum)], w=[id(m8)])
            for j in range(3 if CUT not in (1, 2, 3, 4) else 0):
                dst = csplit[32 * j:32 * j + 16, :]
                p.op("act", (lambda dst: lambda e: e.copy(dst, m8[:]))(dst), r=[id(m8)], w=["csplit"])
                if j < 2:
                    p.op("act", lambda e: e.copy(hi[:], m8[:]), r=[id(m8)], w=["hi"])
                    p.op("act", lambda e: e.copy(fT[:], hi[:]), r=["hi"], w=["hif"])
                    p.op("dve", lambda e: e.tensor_tensor(m8[:], m8[:], fT[:], ALU.subtract), r=[id(m8), "hif"], w=[id(m8)])
            if dbg is not None:
                p.op("sp", lambda e: e.dma_start(out=dbg[0:128, 0:256], in_=ckTM[:].rearrange("p i h -> p (i h)")), r=["ckTM"], w=["dbg"], dma="dbg")
                p.op("sp", None, r=["dbg"])
            p.op("sp", None, r=["csplit", "ckTM"] + [K("hFM", i) for i in range(NT)])
            p.emit()
        nc.all_engine_barrier()
        if dbg is not None:
            return
        p.es = outer
        OT = p.sb(pre + "OT", [128, 8, T], BF16)
        wo = p.sb(pre + "wo", [128, 8, D], BF16)
        with ExitStack() as es:
            p.begin(es)
            KTe = [p.sb(pre + "KTe%d" % h, [67, T], BF16) for h in range(HG)]
            QTe = [[p.sb(pre + "QTe%d_%d" % (j, h), [67, 512], BF16) for h in range(HG)] for j in range(2)]
            Vext = p.sb(pre + "Vext", [128, NT, HG, 128], BF16)
            wq = p.sb(pre + "wq", [128, 8, HG * 64], BF16)
            wk = p.sb(pre + "wk", [128, 8, HG * 64], BF16)
            wv = p.sb(pre + "wv", [128, 8, HG * 64], BF16)
            pT = [p.sb(pre + "pT%d" % j, [128, 512], BF16) for j in range(4)]
            rd1 = p.sb(pre + "rd", [64, 512], F32)
            rd = [rd1, rd1]
            ps_s = [p.ps(pre + "ps_s%d" % j, [128, 512]) for j in range(4)]
            ps_o = [p.ps(pre + "ps_o%d" % j, [128, 512]) for j in range(2)]
            ps_p = [p.ps(pre + "ps_p%d" % j, [128, 512]) for j in range(2)]
            for h in range(HG):
                p.op("dve", (lambda h: lambda e: e.memset(KTe[h][64:67, :], 1.0))(h), w=[K("KTones", h)])
            p.op("dve", lambda e: e.memset(Vext[:, :, :, 64:128], 1.0), w=["Vones"])
            def csrows(h, qb):
                base = csplit[h:h + 1, qb * 512:(qb + 1) * 512]
                return bass.AP(base.tensor, base.offset, [[32 * base.ap[0][0], 3]] + [list(x) for x in base.ap[1:]])
            nq = 0
            nsj = 0
            no = 0
            npp = 0
            for g in range(NG if CUT2 == 0 else 1):
                c0 = g * HG * 64
                p.op("pool", (lambda c0: lambda e: e.dma_start(out=wk[:], in_=win_d[:, D + c0:D + c0 + HG * 64].rearrange("(c p) f -> p c f", p=128)))(c0), w=["wk"], dma=pre + "wk")
                p.op("pool", (lambda c0: lambda e: e.dma_start(out=wq[:], in_=win_d[:, c0:c0 + HG * 64].rearrange("(c p) f -> p c f", p=128)))(c0), w=["wq"], dma=pre + "wq")
                p.op("pool", (lambda c0: lambda e: e.dma_start(out=wv[:], in_=win_d[:, 2 * D + c0:2 * D + c0 + HG * 64].rearrange("(c p) f -> p c f", p=128)))(c0), w=["wv"], dma=pre + "wv")
                if g == 0:
                    p.op("pool", lambda e: e.dma_start(out=wo[:], in_=wo_d.rearrange("(j p) f -> p j f", p=128)), w=["wo"], dma=pre + "wo")
                for a_ in range(HG // 2):
                    for tb in range(4):
                        pp = ps_p[npp % 2]
                        kp = K("ps_p", npp % 2)
                        npp += 1
                        for c8 in range(8):
                            p.mm(pp[:], wk[:, c8, a_ * 128:(a_ + 1) * 128], hFM[:, c8, tb * 512:(tb + 1) * 512], c8 == 0, c8 == 7, r=["wk"], w=[kp])
                        for hh_ in range(2):
                            hl = 2 * a_ + hh_
                            p.cp("dve", KTe[hl][0:64, tb * 512:(tb + 1) * 512], pp[hh_ * 64:(hh_ + 1) * 64, :], r=[kp], w=[K("KTe", hl, tb), kp])
                for i in range(NT):
                    pp = ps_p[npp % 2]
                    kp = K("ps_p", npp % 2)
                    npp += 1
                    for c8 in range(8):
                        p.op("pe", (lambda i, c8, pp: lambda e: e.matmul(pp[:, 0:HG * 64], hFM[:, c8, i * 128:(i + 1) * 128], wv[:, c8, :], start=(c8 == 0), stop=(c8 == 7)))(i, c8, pp),
                             r=["wv"], w=[kp])
                    p.op("dve", (lambda i, pp: lambda e: e.tensor_copy(Vext[:, i, :, 0:64], pp[:, 0:HG * 64].rearrange("p (h d) -> p h d", h=HG)))(i, pp),
                         r=[kp], w=[K("V", i), kp])
                for qb in range(4 if CUT2 == 0 else (0 if CUT2 == 1 else 1)):
                    qs = nq % 2
                    nq += 1
                    for hl in range(HG):
                        h = g * HG + hl
                        p.dma("sp", QTe[qs][hl][64:67, :], csplit[:, qb * 512:(qb + 1) * 512].rearrange("(j r) f -> j r f", r=32)[0:3, h, :] if False else csrows(h, qb),
                              pre + "qc%d_%d" % (qs, hl), r=["csplit"], w=[K("QTc", qs, hl)])
                    for a_ in range(HG // 2):
                        pp = ps_p[npp % 2]
                        kp = K("ps_p", npp % 2)
                        npp += 1
                        for c8 in range(8):
                            p.mm(pp[:], wq[:, c8, a_ * 128:(a_ + 1) * 128], hFM[:, c8, qb * 512:(qb + 1) * 512], c8 == 0, c8 == 7, r=["wq"], w=[kp])
                        for hh_ in range(2):
                            hl = 2 * a_ + hh_
                            p.cp("dve", QTe[qs][hl][0:64, :], pp[hh_ * 64:(hh_ + 1) * 64, :], r=[kp], w=[K("QTe", qs, hl), kp])
                    jobs = []
                    for hl in range(HG):
                        h = g * HG + hl
                        nkt = 4 * qb + 4
                        for kt in range(nkt):
                            jobs.append((hl, h, kt, nkt, no % 2))
                        no += 1
                    LA = 3
                    inflight = {}

                    def front(job, ji):
                        hl, h, kt, nkt, oj = job
                        sj = ji % 4
                        d_ = kt - 4 * qb
                        lo = 128 * d_ if d_ > 0 else 0
                        p.mm(ps_s[sj][:, lo:512], KTe[hl][:, kt * 128:(kt + 1) * 128], QTe[qs][hl][:, lo:512], True, True,
                             r=[K("KTe", hl, kt // 4), K("KTones", hl), K("QTe", qs, hl), K("QTc", qs, hl)], w=[K("ps_s", sj)])
                        p.act(pT[sj][:, lo:512], ps_s[sj][:, lo:512], AF.Exp, bias=ckTM[:, kt, h:h + 1], scale=0.125,
                              r=[K("ps_s", sj)], w=[K("pT", sj), K("ps_s", sj)])
                        if d_ >= 0:
                            p.op("pool", (lambda sj, lo, base: lambda e: e.affine_select(out=pT[sj][:, lo:512], in_=pT[sj][:, lo:512], pattern=[[1, 512 - lo]], compare_op=ALU.is_ge, fill=0.0, base=base, channel_multiplier=-1))(sj, lo, qb * 512 + lo - kt * 128),
                                 r=[K("pT", sj)], w=[K("pT", sj)])
                        inflight[ji] = (sj, lo)

                    def back(job, ji):
                        hl, h, kt, nkt, oj = job
                        sj, lo = inflight.pop(ji)
                        po = ps_o[oj]
                        ko = K("ps_o", oj)
                        p.mm(po[:, lo:512], Vext[:, kt, hl, :], pT[sj][:, lo:512], kt == 0, kt == nkt - 1,
                             r=[K("V", kt), "Vones", K("pT", sj)], w=[ko])
                        if kt == nkt - 1:
                            hh, hj = h % 2, h // 2
                            p.rcp(rd[oj][:], po[64:128, :], r=[ko], w=[K("rd", 0), ko])
                            p.tt("dve", OT[hh * 64:(hh + 1) * 64, hj, qb * 512:(qb + 1) * 512], po[0:64, :], rd[oj][:], ALU.mult,
                                 r=[ko, K("rd", 0)], w=[K("OT", qb), ko])
                    nj = len(jobs) if CUT2 not in (1, 2) else 0
                    for ji in range(nj + LA):
                        if ji < nj:
                            front(jobs[ji], nsj + ji)
                        if ji - LA >= 0:
                            back(jobs[ji - LA], nsj + ji - LA)
                    nsj += nj
            p.op("sp", None, r=[K("OT", qb) for qb in range(4)] + ["wo"])
            p.emit()
        nc.all_engine_barrier()
        with ExitStack() as es:
            p.begin(es)
            z4 = [p.sb(pre + "z4_%d" % j, [128, 4, D], F32) for j in range(2)]
            h2t = [p.sb(pre + "h2t%d" % j, [128, D], F32) for j in range(2)]
            sq = [p.sb(pre + "sq%d" % j, [128, D], F32) for j in range(2)]
            st4 = [p.sb(pre + "st4_%d" % j, [128, 8, 4], F32) for j in range(2)]
            gbc = p.sb(pre + "gbc", [128, D], F32)
            bbc = p.sb(pre + "bbc", [128, D], F32)
            ps_p = [p.ps(pre + "ps_p3_%d" % j, [128, 512]) for j in range(3)]
            p.op("sp", lambda e: e.dma_start(out=gbc[:], in_=lng_d.partition_broadcast(128)), w=[K(pre, "gbc")], dma=pre + "gbc")
            p.op("sp", lambda e: e.dma_start(out=bbc[:], in_=lnb_d.partition_broadcast(128)), w=[K(pre, "bbc")], dma=pre + "bbc")
            npp = 0
            outk = []
            pend = []
            for gi in range(NT // 4 if CUT2 in (0, 4) else 0):
                p.capture()
                zg = z4[gi % 2]
                zs, zks, outs = [], [], []
                for t in range(4):
                    i = gi * 4 + t
                    j = i % 2
                    p.dma("sp", h2t[j][:], h_in[i * 128:(i + 1) * 128, :], pre + "h2t%d" % j, w=[K("h2t", j)])
                    zk = K("z4", gi % 2, t)
                    for half in range(2):
                        pp = ps_p[npp % 3]
                        kp = K("ps_p", npp % 3)
                        npp += 1
                        for hj in range(8):
                            p.mm(pp[:], OT[:, hj, i * 128:(i + 1) * 128], wo[:, hj, half * 512:(half + 1) * 512], hj == 0, hj == 7, w=[kp])
                        sl = slice(half * 512, (half + 1) * 512)
                        p.stt("dve", zg[:, t, sl], h2t[j][:, sl], ALPHA, pp[:], ALU.mult, ALU.add, r=[kp, K("h2t", j)], w=[zk, kp])
                    zs.append(zg[:, t, :])
                    zks.append(zk)
                    outs.append(h_out[i * 128:(i + 1) * 128, :] if acc is None else ("sb", acc[:, i, :]))
                la = p.end_capture()
                p.replay_merged(la, pend)
                p.capture()
                outk += ln_group(p, pre, zs, zks, gbc, bbc, outs, gi, st4[gi % 2], sq)
                pend = p.end_capture()
            p.replay_merged(pend)
            p.op("sp", None, r=outk)
            p.emit()
        nc.all_engine_barrier()


def bc_load(p, name, src_1d, n=128):
    t = p.sb(name, [n, D], F32)
    p.dma("sp", t[:], src_1d.partition_broadcast(n), "ld_" + name, w=[name])
    return t


def stage_rwkv1(p, pre, xT_d, W, S):
    nc = p.nc
    with ExitStack() as es:
        p.begin(es)
        c = consts(p)
        wr = p.sb("wr", [128, 8, D], BF16)
        wk = p.sb("wk", [128, 8, D], BF16)
        wv = p.sb("wv", [128, 8, D], BF16)
        for t_, d_, n_ in ((wr, W["wr"], "wr"), (wk, W["wk"], "wk"), (wv, W["wv"], "wv")):
            p.dma("pool", t_[:], d_.rearrange("(c p) f -> p c f", p=128), "ld_" + n_, w=[n_])
        w1 = p.sb("w1", [128, 8, 64], BF16)
        a1 = p.sb("a1", [128, 8, 64], BF16)
        g1 = p.sb("g1", [128, 8, 128], BF16)
        w2 = p.sb("w2", [64, D], BF16)
        a2 = p.sb("a2", [64, D], BF16)
        g2 = p.sb("g2", [128, D], BF16)
        for t_, d_, n_ in ((w1, W["w1"], "w1"), (a1, W["a1"], "a1"), (g1, W["g1"], "g1")):
            p.dma("pool", t_[:], d_.rearrange("(c p) f -> p c f", p=128), "ld_" + n_, w=[n_])
        for t_, d_, n_ in ((w2, W["w2"], "w2"), (a2, W["a2"], "a2"), (g2, W["g2"], "g2")):
            p.dma("pool", t_[:], d_, "ld_" + n_, w=[n_])
        W0 = bc_load(p, "W0", W["w0"])
        A0 = bc_load(p, "A0", W["a0"])
        KKp = bc_load(p, "KKp", W["kk"])
        KAp = bc_load(p, "KAp", W["ka"])
        RKp = bc_load(p, "RKp", W["rk"].rearrange("h n -> (h n)"))
        mix = p.sb("mix", [128, 6, 8], F32)
        p.dma("sp", mix[:], W["mix"].rearrange("m (c p) -> p m c", p=128), "ld_mix", w=["mix"], slow=True)
        triu = p.sb("triu", [128, 128], F32)
        ones = p.sb("ones", [128, 128], F32)
        p.op("pool", lambda e: e.memset(triu[:], 1.0), w=["triu"])
        p.op("pool", lambda e: e.affine_select(out=triu[:], in_=triu[:], pattern=[[1, 128]], compare_op=ALU.is_ge, fill=0.0, base=0, channel_multiplier=-1), r=["triu"], w=["triu"])
        p.op("pool", lambda e: e.memset(ones[:], 1.0), w=["ones"])

        xt = [p.sb("xt%d" % j, [128, 8, 129], F32) for j in range(2)]
        xx = p.sb("xx", [128, 8, 128], F32)
        xm = [[p.sb("xm%d_%d" % (s_, j), [128, 8, 128], BF16) for j in range(6)] for s_ in range(2)]
        dbl = lambda n_: [p.sb("%s%d" % (n_, j), [128, D], F32) for j in range(2)]
        R, K0, V32, Aa, LDP = dbl("R"), dbl("K0"), dbl("V32"), dbl("Aa"), dbl("LDP")
        KKn = p.sb("KKn", [128, D], F32)
        KP = p.sb("KP", [128, D], F32)
        Bb = p.sb("Bb", [128, D], F32)
        C32 = p.sb("C32", [128, D], F32)
        T1 = p.sb("T1", [128, D], F32)
        T2 = p.sb("T2", [128, D], F32)
        T3 = p.sb("T3", [128, D], F32)
        Gg = p.sb("Gg", [128, D], F32)
        sm = p.sb("sm", [128, 4, 16], F32)
        ET = p.sb("ET", [64, 16], F32)
        hid = p.sb("hid", [128, 3, 128], F32)
        hidb = p.sb("hidb", [128, 3, 128], BF16)
        ob = {n_: p.sb("o_" + n_, [128, D], BF16) for n_ in ("RT", "KKT", "KH", "BH", "KG", "BG", "Vb")}
        psT = [[p.ps("psT%d_%d" % (j, h), [128, 512]) for h in range(2)] for j in range(3)]
        ps_l = p.ps("ps_l", [128, 512])
        ps_e = p.ps("ps_e", [128, 512])
        nps = [0]

        def tm_ps():
            j = nps[0] % 2
            nps[0] += 1
            return psT[j], [K("psT", j, 0), K("psT", j, 1)]

        def proj_tm(lhs_tile, lk, w_t, wkey, kc):
            pt, pk = tm_ps()
            for half in range(2):
                if kc is None:
                    p.mm(pt[half][:], lhs_tile, w_t[:, half * 512:(half + 1) * 512], True, True, r=[lk, wkey], w=[pk[half]])
                else:
                    for c8 in range(kc):
                        p.mm(pt[half][:], lhs_tile[:, c8, :], w_t[:, c8, half * 512:(half + 1) * 512], c8 == 0, c8 == kc - 1, r=[lk, wkey], w=[pk[half]])
            return pt, pk

        def halves(pt, pk, fn):
            for half in range(2):
                fn(slice(half * 512, (half + 1) * 512), pt[half], pk[half])

        xTv = xT_d.rearrange("(c p) t -> p c t", p=128)
        hn = lambda t_: t_[:].rearrange("p (h n) -> p h n", h=16)

        def load_xt(j):
            s_ = j % 2
            x_ = xt[s_]
            kx = K("xt", s_)
            if j == 0:
                p.op("pool", lambda e: e.memset(x_[:, :, 0:1], 0.0), w=[K("xt0c", 0)])
                p.dma("sp", x_[:, :, 1:129], xTv[:, :, 0:128], "ld_xt0", w=[kx])
            else:
                p.dma("sp", x_[:, :, :], xTv[:, :, j * 128 - 1:j * 128 + 128], "ld_xt%d" % s_, w=[kx, K("xt0c", 0)] if s_ == 0 else [kx])
        load_xt(0)
        load_xt(1)

        def P1(j):
            s_ = j % 2
            x_ = xt[s_]
            kx = K("xt", s_)
            kx_all = [kx, K("xt0c", 0)] if s_ == 0 else [kx]
            p.tt("dve", xx[:], x_[:, :, 0:128], x_[:, :, 1:129], ALU.subtract, r=kx_all, w=["xx"])
            xm_ = xm[s_]
            for m in (0, 2, 3, 1, 4, 5):
                for c8 in range(8):
                    p.stt("dve", xm_[m][:, c8, :], xx[:, c8, :], mix[:, m, c8:c8 + 1], x_[:, c8, 1:129], ALU.mult, ALU.add,
                          r=["xx", "mix"] + kx_all, w=[K("xm", s_, m)])
            if j + 2 < NT:
                load_xt(j + 2)
            pt, pk = proj_tm(xm_[0], K("xm", s_, 0), wr, "wr", 8)
            halves(pt, pk, lambda sl, ps_, k_: p.cp("act", R[s_][:, sl], ps_[:], r=[k_], w=[K("R", s_), k_]))
            pt, pk = proj_tm(xm_[2], K("xm", s_, 2), wk, "wk", 8)
            halves(pt, pk, lambda sl, ps_, k_: p.cp("act", K0[s_][:, sl], ps_[:], r=[k_], w=[K("K0", s_), k_]))
            pt, pk = proj_tm(xm_[3], K("xm", s_, 3), wv, "wv", 8)
            halves(pt, pk, lambda sl, ps_, k_: p.cp("act", V32[s_][:, sl], ps_[:], r=[k_], w=[K("V32", s_), k_]))
            halves(pt, pk, lambda sl, ps_, k_: p.cp("act", ob["Vb"][:, sl], ps_[:], r=[k_], w=["o_Vb", k_]))
            p.dma("sp", S["Vb"][j * 128:(j + 1) * 128, :], ob["Vb"][:], "st_Vb", r=["o_Vb"], w=[K("S_Vb", j)])
            for c8 in range(8):
                p.mm(ps_l[0:64, 0:128], w1[:, c8, :], xm_[1][:, c8, :], c8 == 0, c8 == 7, r=["w1", K("xm", s_, 1)], w=["ps_l"])
            for c8 in range(8):
                p.mm(ps_l[0:64, 128:256], a1[:, c8, :], xm_[4][:, c8, :], c8 == 0, c8 == 7, r=["a1", K("xm", s_, 4)], w=["ps_l"])
            for c8 in range(8):
                p.mm(ps_l[:, 256:384], g1[:, c8, :], xm_[5][:, c8, :], c8 == 0, c8 == 7, r=["g1", K("xm", s_, 5)], w=["ps_l"])
            p.act(hid[0:64, 0, :], ps_l[0:64, 0:128], AF.Exp, scale=2.0, r=["ps_l"], w=["hid0", "ps_l"])
            p.ts("dve", hid[0:64, 0, :], hid[0:64, 0, :], 1.0, None, ALU.add, r=["hid0"], w=["hid0"])
            p.rcp(hid[0:64, 0, :], hid[0:64, 0, :], r=["hid0"], w=["hid0"])
            p.ts("dve", hidb[0:64, 0, :], hid[0:64, 0, :], -2.0, 1.0, ALU.mult, ALU.add, r=["hid0"], w=["hidb0"])
            p.cp("act", hidb[0:64, 1, :], ps_l[0:64, 128:256], r=["ps_l"], w=["hidb1", "ps_l"])
            p.act(hid[:, 2, :], ps_l[:, 256:384], AF.Exp, scale=-1.0, r=["ps_l"], w=["hid2", "ps_l"])
            p.act(hid[:, 2, :], hid[:, 2, :], AF.Ln, bias=1.0, r=["hid2"], w=["hid2"])
            p.act(hidb[:, 2, :], hid[:, 2, :], AF.Exp, scale=-1.0, r=["hid2"], w=["hidb2"])
            L_ = LDP[s_]
            kL = K("LDP", s_)
            pt, pk = proj_tm(hidb[0:64, 0, :], "hidb0", w2, "w2", None)
            halves(pt, pk, lambda sl, ps_, k_: p.tt("dve", L_[:, sl], ps_[:], W0[:, sl], ALU.add, r=[k_, "W0"], w=[kL, k_]))
            p.act(L_[:], L_[:], AF.Exp, scale=-1.0, r=[kL], w=[kL])
            p.act(L_[:], L_[:], AF.Ln, bias=1.0, r=[kL], w=[kL])
            p.act(L_[:], L_[:], AF.Exp, scale=-1.0, bias=-0.5, r=[kL], w=[kL])
            A_ = Aa[s_]
            kA = K("Aa", s_)
            pt, pk = proj_tm(hidb[0:64, 1, :], "hidb1", a2, "a2", None)
            halves(pt, pk, lambda sl, ps_, k_: p.tt("dve", A_[:, sl], ps_[:], A0[:, sl], ALU.add, r=[k_, "A0"], w=[kA, k_]))
            p.act(A_[:], A_[:], AF.Exp, scale=-1.0, r=[kA], w=[kA])
            p.act(A_[:], A_[:], AF.Ln, bias=1.0, r=[kA], w=[kA])
            p.act(A_[:], A_[:], AF.Exp, scale=-1.0, r=[kA], w=[kA])
            pt, pk = proj_tm(hidb[:, 2, :], "hidb2", g2, "g2", None)
            halves(pt, pk, lambda sl, ps_, k_: p.cp("act", Gg[:, sl], ps_[:], r=[k_], w=["Gg", k_]))
            p.dma("sp", S["Gg"][j * 128:(j + 1) * 128, :], Gg[:], "st_Gg", r=["Gg"], w=[K("S_Gg", j)])

        def P2(j):
            s_ = j % 2
            R_, K_, V_, A_, L_ = R[s_], K0[s_], V32[s_], Aa[s_], LDP[s_]
            kR, kK, kV, kA, kL = K("R", s_), K("K0", s_), K("V32", s_), K("Aa", s_), K("LDP", s_)
            p.tt("dve", KKn[:], K_[:], KKp[:], ALU.mult, r=[kK, "KKp"], w=["KKn"])
            p.act(T3[:], KKn[:], AF.Square, r=["KKn"], w=["T3"])
            p.red(sm[:, 0, :], hn(T3), r=["T3"], w=["sm0"])
            p.ts("dve", sm[:, 0, :], sm[:, 0, :], 1e-24, None, ALU.max, r=["sm0"], w=["sm0"])
            p.act(sm[:, 0, :], sm[:, 0, :], AF.Ln, r=["sm0"], w=["sm0"])
            p.act(sm[:, 0, :], sm[:, 0, :], AF.Exp, scale=-0.5, r=["sm0"], w=["sm0"])
            p.tt("dve", hn(KKn), hn(KKn), sm[:, 0, :].unsqueeze(2).to_broadcast([128, 16, 64]), ALU.mult, r=["KKn", "sm0"], w=["KKn"])
            p.stt("dve", T3[:], A_[:], -1.0, KAp[:], ALU.add, ALU.mult, r=[kA, "KAp"], w=["T3"])
            p.stt("dve", KP[:], T3[:], 1.0, K_[:], ALU.add, ALU.mult, r=["T3", kK], w=["KP"])
            p.tt("pool", Bb[:], KKn[:], A_[:], ALU.mult, r=["KKn", kA], w=["Bb"])
            p.tt("pool", T3[:], R_[:], KP[:], ALU.mult, r=[kR, "KP"], w=["T3"])
            p.tt("pool", T3[:], T3[:], RKp[:], ALU.mult, r=["T3", "RKp"], w=["T3"])
            p.red(sm[:, 1, :], hn(T3), r=["T3"], w=["sm1"])
            p.tt("dve", hn(T3), hn(V_), sm[:, 1, :].unsqueeze(2).to_broadcast([128, 16, 64]), ALU.mult, r=[kV, "sm1"], w=["T3"])
            p.dma("sp", S["BV"][j * 128:(j + 1) * 128, :], T3[:], "st_BV", r=["T3"], w=[K("S_BV", j), "T3dma"])
            pc, pkc = psT[2], [K("psT", 2, 0), K("psT", 2, 1)]
            for half in range(2):
                sl = slice(half * 512, (half + 1) * 512)
                p.mm(pc[half][:], triu[:], L_[:, sl], True, True, r=["triu", kL], w=[pkc[half]])
            halves(pc, pkc, lambda sl, ps_, k_: p.cp("act", C32[:, sl], ps_[:], r=[k_], w=["C32", k_]))
            for half in range(2):
                sl = slice(half * 512, (half + 1) * 512)
                p.mm(pc[half][:], ones[:], L_[:, sl], True, True, r=["ones", kL], w=[pkc[half]])
            for h in range(16):
                p.mm(ps_e[0:64, h:h + 1], L_[:, h * 64:(h + 1) * 64], ones[:, 0:1], True, True, r=[kL, "ones"], w=["ps_e"])
            p.act(ET[:], ps_e[0:64, 0:16], AF.Exp, scale=-1.0, r=["ps_e"], w=["ET", "ps_e"])
            p.dma("sp", S["ET"][j], ET[:], "st_ET", r=["ET"], w=[K("S_ET", j)])
            halves(pc, pkc, lambda sl, ps_, k_: p.tt("dve", T2[:, sl], ps_[:], C32[:, sl], ALU.subtract, r=[k_, "C32", "T2"], w=["T2", k_]))
            p.act(T2[:], T2[:], AF.Exp, scale=-1.0, r=["T2"], w=["T2"])
            p.tt("dve", ob["KG"][:], KP[:], T2[:], ALU.mult, r=["KP", "T2"], w=["o_KG"])
            p.tt("pool", ob["BG"][:], Bb[:], T2[:], ALU.mult, r=["Bb", "T2"], w=["o_BG"])
            p.act(T1[:], C32[:], AF.Exp, scale=-1.0, r=["C32"], w=["T1"])
            p.tt("dve", ob["RT"][:], R_[:], T1[:], ALU.mult, r=[kR, "T1"], w=["o_RT"])
            p.act(T2[:], C32[:], AF.Exp, r=["C32", "T2"], w=["T2"])
            p.tt("dve", ob["KH"][:], KP[:], T2[:], ALU.mult, r=["KP", "T2"], w=["o_KH"])
            p.tt("pool", ob["BH"][:], Bb[:], T2[:], ALU.mult, r=["Bb", "T2"], w=["o_BH"])
            p.tt("dve", T1[:], C32[:], L_[:], ALU.subtract, r=["C32", kL, "T1"], w=["T1"])
            p.act(T1[:], T1[:], AF.Exp, scale=-1.0, r=["T1"], w=["T1"])
            p.tt("pool", ob["KKT"][:], KKn[:], T1[:], ALU.mult, r=["KKn", "T1"], w=["o_KKT"])
            for n_ in ("RT", "KKT", "KH", "BH", "KG", "BG"):
                p.dma("sp", S[n_][j * 128:(j + 1) * 128, :], ob[n_][:], "st_" + n_, r=["o_" + n_], w=[K("S_" + n_, j)])

        P1(0)
        for j in range(NT):
            p.capture()
            P2(j)
            la = p.end_capture()
            lb = []
            if j + 1 < NT:
                p.capture()
                P1(j + 1)
                lb = p.end_capture()
            p.replay_merged(la, lb)
        p.fence([K("S_" + n_, j) for n_ in ("RT", "KKT", "KH", "BH", "KG", "BG", "Vb", "Gg", "BV", "ET") for j in range(NT)])
        p.emit()
    nc.all_engine_barrier()


def stage_rwkv2(p, pre, W, S):
    nc = p.nc
    with ExitStack() as es:
        p.begin(es)
        c = consts(p)
        ident, identb = c["ident"], c["identb"]
        GNg = bc_load(p, "GNg", W["gn_g"])
        GNb = bc_load(p, "GNb", W["gn_b"])
        MASK4 = p.sb("MASK4", [128, 4, 128], F32)
        MASKL = p.sb("MASKL", [128, 4, 128], F32)
        p.op("pool", lambda e: e.memset(MASK4[:], 1.0), w=["MASK4"])
        p.op("pool", lambda e: e.memset(MASKL[:], 1.0), w=["MASKL"])
        for q4 in range(4):
            strict = q4 % 2 == 0
            p.op("pool", (lambda q4, strict: lambda e: e.affine_select(out=MASK4[:, q4, :], in_=MASK4[:, q4, :], pattern=[[1, 128]], compare_op=ALU.is_ge, fill=0.0, base=(-1 if strict else 0), channel_multiplier=-1))(q4, strict), r=["MASK4"], w=["MASK4"])
            p.op("pool", (lambda q4: lambda e: e.affine_select(out=MASKL[:, q4, :], in_=MASKL[:, q4, :], pattern=[[-1, 128]], compare_op=ALU.is_ge, fill=0.0, base=-1, channel_multiplier=1))(q4), r=["MASKL"], w=["MASKL"])
        names = ("RT", "KH", "BH", "KG", "BG", "Vb")
        inb = {n_: [p.sb("i_%s%d" % (n_, j), [128, D], BF16) for j in range(2)] for n_ in names}
        Rhs = [p.sb("Rhs%d" % j, [128, 16, 128], BF16) for j in range(2)]
        Gg = [p.sb("Gg%d" % j, [128, D], F32) for j in range(2)]
        BV = [p.sb("BV%d" % j, [128, D], F32) for j in range(2)]
        ET = [p.sb("ET%d" % j, [64, 16], F32) for j in range(2)]
        KHT = p.sb("KHT", [64, 16, 128], BF16)
        BHT = p.sb("BHT", [64, 16, 128], BF16)
        KR = p.sb("KR", [64, 16, 256], BF16)
        ATB = p.sb("ATB", [128, 16, 512], BF16)
        Lb = p.sb("Lb", [128, 16, 128], BF16)
        Nn = [p.sb("Nn%d" % j, [128, 16, 128], BF16) for j in range(2)]
        NTn = [p.sb("NTn%d" % j, [128, 16, 128], BF16) for j in range(2)]
        Yb = [p.sb("Yb%d" % j, [128, 16, 128], BF16) for j in range(2)]
        PUn = p.sb("PUn", [128, 16, 128], BF16)
        MTp = p.sb("MTp", [64, 16, 64], BF16)
        QT = p.sb("QT", [64, 16, 128], BF16)
        Gf = p.sb("Gf", [64, 16, 64], F32)
        H = p.sb("H", [64, 16, 64], F32)
        HG = p.sb("HG", [64, 16, 64], F32)
        Hb = p.sb("Hb", [64, 16, 64], BF16)
        Tn = [p.sb("Tn%d" % j, [128, D], F32) for j in range(2)]
        sm = p.sb("sm", [128, 6, 16], F32)
        YG = [p.sb("YG%d" % j, [128, D], BF16) for j in range(2)]
        TRb = [p.ps("TRb%d" % j, [128, 1024], BF16) for j in range(2)]
        PB = [p.ps("PB%d" % j, [128, 512]) for j in range(6)]
        nb = [0]

        def bank():
            j = nb[0] % 4
            nb[0] += 1
            return PB[j], K("PB", j)
        PY = [(PB[4], K("PB", 4)), (PB[5], K("PB", 5))]
        pendingB = []
        ntr = [0]
        nev = [0]

        def evq():
            nev[0] += 1
            return "dve" if nev[0] % 3 == 0 else "act"

        p.op("pool", lambda e: e.memset(H[:], 0.0), w=["H"])
        p.op("pool", lambda e: e.memset(Hb[:], 0.0), w=["Hb"])

        def loads(j):
            s = j % 2
            rows = slice(j * 128, (j + 1) * 128)
            for n_ in names:
                p.dma("sp", inb[n_][s][:], S[n_][rows, :], "ld_%s%d" % (n_, s), w=[K("i", n_, s)])
            p.dma("sp", Rhs[s][:, :, 0:64], S["KKT"][rows, :].rearrange("t (h n) -> t h n", h=16), "ld_KKT%d" % s, w=[K("RhsA", s)])
            p.dma("sp", ET[s][:], S["ET"][j], "ld_ET%d" % s, w=[K("ET", s)])
        loads(0)

        for j in range(NT):
            p.capture()
            s = j % 2
            rows = slice(j * 128, (j + 1) * 128)
            if j + 1 < NT:
                loads(j + 1)
            p.dma("sp", Gg[s][:], S["Gg"][rows, :], "ld_Gg%d" % s, w=[K("Gg", s)])
            p.dma("sp", BV[s][:], S["BV"][rows, :], "ld_BV%d" % s, w=[K("BV", s)])
            RT_, KH_, BH_, KG_, BG_, Vb_ = (inb[n_][s] for n_ in names)
            kRT, kKH, kBH, kKG, kBG, kVb = (K("i", n_, s) for n_ in names)
            def transp(src, rk, dst, wk_, pair_view=None):
                tb_ = TRb[ntr[0] % 2]
                tk = K("TRb", ntr[0] % 2)
                ntr[0] += 1
                for a_ in range(8):
                    p.tr(tb_[:, a_ * 128:(a_ + 1) * 128], src(a_), identb[:], r=[rk, "identb"], w=[tk])
                for par in range(2):
                    p.cp(evq(), dst(par), tb_[par * 64:(par + 1) * 64, :].rearrange("p (a t) -> p a t", a=8), r=[tk], w=[wk_, tk])
            ev = lambda t_, lo, hi: (lambda par: t_[:].rearrange("p (a two) t -> p a two t", two=2)[:, :, par, lo:hi])
            transp(lambda a_: KH_[:, a_ * 128:(a_ + 1) * 128], kKH, ev(KHT, 0, 128), "KHT")
            transp(lambda a_: BH_[:, a_ * 128:(a_ + 1) * 128], kBH, ev(BHT, 0, 128), "BHT")
            for hh in range(2):
                tb_ = TRb[ntr[0] % 2]
                tk = K("TRb", ntr[0] % 2)
                ntr[0] += 1
                for h8 in range(8):
                    p.tr(tb_[0:64, h8 * 128:(h8 + 1) * 128], Rhs[s][:, hh * 8 + h8, 0:64], identb[:], r=[K("RhsA", s), "identb"], w=[tk])
                p.cp(evq(), KR[:, hh * 8:(hh + 1) * 8, 0:128], tb_[0:64, :].rearrange("p (h t) -> p h t", h=8), r=[tk], w=["KR", tk])
            transp(lambda a_: RT_[:, a_ * 128:(a_ + 1) * 128], kRT, ev(KR, 128, 256), "KR")
            for h in range(16):
                b_, bk = bank()
                p.mm(b_[:, 0:256], KHT[:, h, :], KR[:, h, :], True, True, r=["KHT", "KR"], w=[bk])
                p.mm(b_[:, 256:512], BHT[:, h, :], KR[:, h, :], True, True, r=["BHT", "KR"], w=[bk])
                p.tt("dve", ATB[:, h, :], b_[:], MASK4[:].rearrange("p a t -> p (a t)"), ALU.mult, r=[bk, "MASK4"], w=[K("ATB", h // 4), bk])
            for g4 in range(4):
                b_, bk = bank()
                for hq in range(4):
                    h = g4 * 4 + hq
                    p.mm(b_[:, hq * 128:(hq + 1) * 128], KR[:, h, 0:128], BHT[:, h, :], True, True, r=["KR", "BHT"], w=[bk])
                p.tt("dve", Lb[:, g4 * 4:(g4 + 1) * 4, :], b_[:].rearrange("p (a t) -> p a t", a=4), MASKL[:], ALU.mult, r=[bk, "MASKL"], w=[K("Lb", g4), bk])
            ycur = 0
            for g4 in range(4):
                hs = slice(g4 * 4, (g4 + 1) * 4)
                p.tt("pool", Yb[0][:, hs, :], identb[:].unsqueeze(1).to_broadcast([128, 4, 128]), ATB[:, hs, 256:384], ALU.subtract,
                     r=["identb", K("ATB", g4)], w=[K("Yb", 0, g4)])
            Ncur = lambda h: Lb[:, h, :]
            NTcur = lambda h: ATB[:, h, 256:384]
            nkey = lambda g4: K("Lb", g4)
            ntkey = lambda g4: K("ATB", g4)
            for lvl in range(6):
                pp_ = lvl % 2
                last = lvl == 5
                for g4 in range(4):
                    hs = slice(g4 * 4, (g4 + 1) * 4)
                    bA, kA = bank()
                    for hq in range(4):
                        h = g4 * 4 + hq
                        p.mm(bA[:, hq * 128:(hq + 1) * 128], NTcur(h), Ncur(h), True, True, r=[nkey(g4), ntkey(g4)], w=[kA])
                    p.cp(evq(), Nn[pp_][:, hs, :], bA[:].rearrange("p (a t) -> p a t", a=4), r=[kA], w=[K("Nn", pp_, g4), kA])
                    if not last:
                        bB, kB = bank()
                        for hq in range(4):
                            h = g4 * 4 + hq
                            p.mm(bB[:, hq * 128:(hq + 1) * 128], Ncur(h), NTcur(h), True, True, r=[nkey(g4), ntkey(g4)], w=[kB])
                        p.cp(evq(), NTn[pp_][:, hs, :], bB[:].rearrange("p (a t) -> p a t", a=4), r=[kB], w=[K("NTn", pp_, g4), kB])
                for g4 in range(4):
                    hs = slice(g4 * 4, (g4 + 1) * 4)
                    bC, kC = bank()
                    p.mm(bC[:], identb[:], Yb[ycur][:, hs, :].rearrange("p a t -> p (a t)"), True, False, r=["identb", K("Yb", ycur, g4)], w=[kC])
                    for hq in range(4):
                        h = g4 * 4 + hq
                        osl = bC[:, hq * 128:(hq + 1) * 128]
                        p.mm(osl, Nn[pp_][:, h, :], Yb[ycur][:, h, :], False, hq == 3, r=[K("Nn", pp_, g4), K("Yb", ycur, g4)], w=[kC])
                    p.cp(evq(), Yb[1 - ycur][:, hs, :], bC[:].rearrange("p (a t) -> p a t", a=4), r=[kC], w=[K("Yb", 1 - ycur, g4), kC])
                ycur = 1 - ycur
                Ncur = (lambda pp_: lambda h: Nn[pp_][:, h, :])(pp_)
                NTcur = (lambda pp_: lambda h: NTn[pp_][:, h, :])(pp_)
                nkey = (lambda pp_: lambda g4: K("Nn", pp_, g4))(pp_)
                ntkey = (lambda pp_: lambda g4: K("NTn", pp_, g4))(pp_)
            Y = Yb[ycur]
            for g8 in range(2):
                b_, bk = bank()
                for h8 in range(8):
                    h = g8 * 8 + h8
                    p.mm(b_[:, h8 * 64:(h8 + 1) * 64], ATB[:, h, 0:128], Vb_[:, h * 64:(h + 1) * 64], True, True, r=[K("ATB", h // 4), kVb], w=[bk])
                p.cp(evq(), Rhs[s][:, g8 * 8:(g8 + 1) * 8, 64:128], b_[:].rearrange("p (h n) -> p h n", h=8), r=[bk], w=[K("RhsB", s, g8), bk])
            for g4 in range(4):
                b_, bk = bank()
                for hq in range(4):
                    h = g4 * 4 + hq
                    p.mm(b_[:, hq * 128:(hq + 1) * 128], Y[:, h, :], Rhs[s][:, h, :], True, True, r=[K("Yb", ycur, g4), K("RhsA", s), K("RhsB", s, h // 8)], w=[bk])
                p.op("act", (lambda b_, g4: lambda e: e.mul(PUn[:, g4 * 4:(g4 + 1) * 4, :], b_[:].rearrange("p (a t) -> p a t", a=4), -1.0))(b_, g4), r=[bk], w=[K("PUn", g4), bk])
            for g8 in range(2):
                b_, bk = bank()
                for h8 in range(8):
                    h = g8 * 8 + h8
                    p.mm(b_[0:64, h8 * 64:(h8 + 1) * 64], PUn[:, h, 0:64], BG_[:, h * 64:(h + 1) * 64], True, True, r=[K("PUn", h // 4), kBG], w=[bk])
                p.cp(evq(), MTp[:, g8 * 8:(g8 + 1) * 8, :], b_[0:64, :].rearrange("p (h n) -> p h n", h=8), r=[bk], w=[K("MTp", g8), bk])
            for g4 in range(4):
                b_, bk = bank()
                for hq in range(4):
                    h = g4 * 4 + hq
                    osl = b_[0:64, hq * 128:(hq + 1) * 128]
                    p.mm(osl, identb[0:64, 0:64], KR[:, h, 128:256], True, False, r=["identb", "KR"], w=[bk])
                    p.mm(osl, PUn[:, h, 0:64], ATB[:, h, 384:512], False, True, r=[K("PUn", g4), K("ATB", g4)], w=[bk])
                p.cp(evq(), QT[:, g4 * 4:(g4 + 1) * 4, :], b_[0:64, :].rearrange("p (a t) -> p a t", a=4), r=[bk], w=[K("QT", g4), bk])
            for g8 in range(2):
                b_, bk = bank()
                for h8 in range(8):
                    h = g8 * 8 + h8
                    osl = b_[0:64, h8 * 64:(h8 + 1) * 64]
                    p.mm(osl, KG_[:, h * 64:(h + 1) * 64], Vb_[:, h * 64:(h + 1) * 64], True, False, r=[kKG, kVb], w=[bk])
                    p.mm(osl, BG_[:, h * 64:(h + 1) * 64], PUn[:, h, 64:128], False, True, r=[kBG, K("PUn", h // 4)], w=[bk])
                p.cp(evq(), Gf[:, g8 * 8:(g8 + 1) * 8, :], b_[0:64, :].rearrange("p (h n) -> p h n", h=8), r=[bk], w=[K("Gf", g8), bk])
            ybanks = []
            for g8 in range(2):
                b_, bk = PY[g8]
                ybanks.append((b_, bk))
                for h8 in range(8):
                    h = g8 * 8 + h8
                    osl = b_[:, h8 * 64:(h8 + 1) * 64]
                    p.mm(osl, ATB[:, h, 128:256], Vb_[:, h * 64:(h + 1) * 64], True, False, r=[K("ATB", h // 4), kVb], w=[bk])
                    p.mm(osl, ATB[:, h, 384:512], PUn[:, h, 64:128], False, False, r=[K("ATB", h // 4), K("PUn", h // 4)], w=[bk])
                    p.mm(osl, QT[:, h, :], Hb[:, h, :], False, True, r=[K("QT", h // 4), "Hb"], w=[bk])
            hb_ = []
            for g8 in range(2):
                b_, bk = bank()
                hb_.append((b_, bk))
                for h8 in range(8):
                    h = g8 * 8 + h8
                    p.mm(b_[0:64, h8 * 64:(h8 + 1) * 64], MTp[:, h, :], Hb[:, h, :], True, True, r=[K("MTp", g8), "Hb"], w=[bk])
            p.tt("pool", HG[:], H[:], ET[s][:].unsqueeze(2).to_broadcast([64, 16, 64]), ALU.mult, r=["H", K("ET", s)], w=["HG"])
            p.tt("pool", HG[:], HG[:], Gf[:], ALU.add, r=["HG", K("Gf", 0), K("Gf", 1)], w=["HG"])
            for g8 in range(2):
                b_, bk = hb_[g8]
                p.tt("dve", H[:, g8 * 8:(g8 + 1) * 8, :], b_[0:64, :].rearrange("p (h n) -> p h n", h=8), HG[:, g8 * 8:(g8 + 1) * 8, :], ALU.add, r=[bk, "HG"], w=["H", bk])
            p.cp("act", Hb[:], H[:], r=["H"], w=["Hb"])
            la = p.end_capture()
            nf = min(len(la), 100000)
            p.replay_merged(la[:nf], pendingB)
            p.replay_merged(la[nf:])
            p.capture()
            T_ = Tn[s]
            kT = K("Tn", s)
            for g8 in range(2):
                b_, bk = ybanks[g8]
                sl = slice(g8 * 512, (g8 + 1) * 512)
                p.red(sm[:, 0, g8 * 8:(g8 + 1) * 8], b_[:].rearrange("p (h n) -> p h n", h=8), r=[bk], w=["sm0", bk])
                p.act(T_[:, sl], b_[:], AF.Square, r=[bk], w=[kT, bk])
            p.red(sm[:, 1, :], T_[:].rearrange("p (h n) -> p h n", h=16), r=[kT], w=["sm1"])
            p.ts("dve", sm[:, 0, :], sm[:, 0, :], 1.0 / 64, None, ALU.mult, r=["sm0"], w=["sm0"])
            p.tt("dve", sm[:, 2, :], sm[:, 0, :], sm[:, 0, :], ALU.mult, r=["sm0"], w=["sm2"])
            p.stt("dve", sm[:, 3, :], sm[:, 1, :], 1.0 / 64, sm[:, 2, :], ALU.mult, ALU.subtract, r=["sm1", "sm2"], w=["sm3"])
            p.ts("dve", sm[:, 3, :], sm[:, 3, :], GN_EPS, None, ALU.add, r=["sm3"], w=["sm3"])
            p.act(sm[:, 3, :], sm[:, 3, :], AF.Ln, r=["sm3"], w=["sm3"])
            p.act(sm[:, 3, :], sm[:, 3, :], AF.Exp, scale=-0.5, r=["sm3"], w=["sm3"])
            for g8 in range(2):
                b_, bk = ybanks[g8]
                sl = slice(g8 * 512, (g8 + 1) * 512)
                p.tt("dve", T_[:, sl].rearrange("p (h n) -> p h n", h=8), b_[:].rearrange("p (h n) -> p h n", h=8),
                     sm[:, 0, g8 * 8:(g8 + 1) * 8].unsqueeze(2).to_broadcast([128, 8, 64]), ALU.subtract, r=[bk, "sm0"], w=[kT, bk])
            p.tt("dve", T_[:].rearrange("p (h n) -> p h n", h=16), T_[:].rearrange("p (h n) -> p h n", h=16),
                 sm[:, 3, :].unsqueeze(2).to_broadcast([128, 16, 64]), ALU.mult, r=[kT, "sm3"], w=[kT])
            p.tt("pool", T_[:], T_[:], GNg[:], ALU.mult, r=[kT, "GNg"], w=[kT])
            p.tt("pool", T_[:], T_[:], GNb[:], ALU.add, r=[kT, "GNb"], w=[kT])
            p.tt("dve", T_[:], T_[:], BV[s][:], ALU.add, r=[kT, K("BV", s)], w=[kT])
            p.tt("pool", YG[s][:], T_[:], Gg[s][:], ALU.mult, r=[kT, K("Gg", s)], w=[K("YG", s)])
            p.dma("sp", S["YG"][rows, :], YG[s][:], "st_YG%d" % s, r=[K("YG", s)], w=[K("S_YG", j)])
            pendingB = p.end_capture()
        p.replay_merged(pendingB)
        p.fence([K("S_YG", j) for j in range(NT)])
        p.emit()
    nc.all_engine_barrier()


def stage_rwkv3(p, pre, x_d, W, S, lng_d, lnb_d, h_out, acc=None):
    nc = p.nc
    with ExitStack() as es:
        p.begin(es)
        c = consts(p)
        identb = c["identb"]
        wo = p.sb("wo3", [128, 8, D], BF16)
        p.dma("pool", wo[:], W["wo"].rearrange("(c p) f -> p c f", p=128), "ld_wo3", w=["wo"])
        gbc = p.sb(pre + "gbc", [128, D], F32)
        bbc = p.sb(pre + "bbc", [128, D], F32)
        p.dma("sp", gbc[:], lng_d.partition_broadcast(128), pre + "gbc", w=[K(pre, "gbc")])
        p.dma("sp", bbc[:], lnb_d.partition_broadcast(128), pre + "bbc", w=[K(pre, "bbc")])
        yg = [p.sb("yg%d" % j, [128, D], BF16) for j in range(2)]
        ygT = [p.sb("ygT%d" % j, [128, 8, 128], BF16) for j in range(2)]
        xt = [p.sb("x3_%d" % j, [128, D], F32) for j in range(2)]
        z4 = [p.sb("z43_%d" % j, [128, 4, D], F32) for j in range(2)]
        sq = [p.sb("sq3_%d" % j, [128, D], F32) for j in range(2)]
        st4 = [p.sb("st43_%d" % j, [128, 8, 4], F32) for j in range(2)]
        TRb = [p.ps("TR3_%d" % j, [128, 1024], BF16) for j in range(2)]
        PZ = [p.ps("PZ%d" % j, [128, 512]) for j in range(6)]
        outk = []
        pend = []
        for gi in range(NT // 4):
            p.capture()
            zg = z4[gi % 2]
            zs, zks, outs = [], [], []
            for t in range(4):
                i = gi * 4 + t
                s = i % 2
                rows = slice(i * 128, (i + 1) * 128)
                p.dma("sp", yg[s][:], S["YG"][rows, :], "ld_yg%d" % s, w=[K("yg", s)])
                p.dma("sp", xt[s][:], x_d[rows, :], "ld_x3%d" % s, w=[K("x3", s)])
                for c8 in range(8):
                    p.tr(TRb[s][:, c8 * 128:(c8 + 1) * 128], yg[s][:, c8 * 128:(c8 + 1) * 128], identb[:], r=[K("yg", s), "identb"], w=[K("TR3", s)])
                p.cp("act", ygT[s][:], TRb[s][:].rearrange("p (c t) -> p c t", c=8), r=[K("TR3", s)], w=[K("ygT", s), K("TR3", s)])
                zk = K("z43", gi % 2, t)
                for half in range(2):
                    pz = PZ[(i * 2 + half) % 6]
                    kz = K("PZ", (i * 2 + half) % 6)
                    for c8 in range(8):
                        p.mm(pz[:], ygT[s][:, c8, :], wo[:, c8, half * 512:(half + 1) * 512], c8 == 0, c8 == 7, r=[K("ygT", s), "wo"], w=[kz])
                    sl = slice(half * 512, (half + 1) * 512)
                    p.stt("dve", zg[:, t, sl], xt[s][:, sl], ALPHA, pz[:], ALU.mult, ALU.add, r=[kz, K("x3", s)], w=[zk, kz])
                zs.append(zg[:, t, :])
                zks.append(zk)
                outs.append(h_out[rows, :] if acc is None else ("sb", acc[:, i, :]))
            la = p.end_capture()
            p.replay_merged(la, pend)
            p.capture()
            outk += ln_group(p, pre, zs, zks, gbc, bbc, outs, gi, st4[gi % 2], sq)
            pend = p.end_capture()
        p.replay_merged(pend)
        p.fence(outk)
        p.emit()
    nc.all_engine_barrier()


RW_SHAPES = (("mix", [6, D]), ("wr", [D, D]), ("wk", [D, D]), ("wv", [D, D]), ("wo", [D, D]), ("w0", [D]), ("w1", [D, 64]), ("w2", [64, D]),
             ("a0", [D]), ("a1", [D, 64]), ("a2", [64, D]), ("g1", [D, 128]), ("g2", [128, D]), ("kk", [D]), ("ka", [D]), ("rk", [16, 64]),
             ("gn_g", [D]), ("gn_b", [D]))
BNAMES = ("RT", "KKT", "KH", "BH", "KG", "BG", "Vb", "YG")


def build_full():
    nc = bass.Bass("TRN2", target_bir_lowering=False)

    def dt(n, s, kind="ExternalInput", t=F32):
        if kind is None:
            return nc.dram_tensor(n, s, t).ap()
        return nc.dram_tensor(n, s, t, kind=kind).ap()
    x_d = dt("x", [T, D])
    xT_d = dt("xT", [D, T])
    W = {n_: dt("rw_" + n_, shp) for n_, shp in RW_SHAPES}
    win = dt("fx_w_in", [D, 3088])
    bf = dt("fx_b_f", [16])
    fwo = dt("fx_wo", [D, D])
    rw = dt("router_w", [D, 16])
    rb = dt("router_bias", [16])
    wg = dt("moe_w_gate", [2, 16, D, 512])
    wu = dt("moe_w_up", [2, 16, D, 512])
    wd = dt("moe_w_down", [2, 16, 512, D])
    lng = dt("ln_g", [2, 2, D])
    lnb = dt("ln_b", [2, 2, D])
    out = dt("out", [T, D], "ExternalOutput")
    S = {n_: dt("S_" + n_, [T, D], None, BF16) for n_ in BNAMES}
    S["Gg"] = dt("S_Gg", [T, D], None)
    S["BV"] = dt("S_BV", [T, D], None)
    S["ET"] = dt("S_ET", [NT, 64, 16], None)
    h1 = dt("S_h1", [T, D], None)
    h2 = dt("S_h2", [T, D], None)
    h3 = dt("S_h3", [T, D], None)
    p = Prog(nc)
    stage_rwkv1(p, "r1", xT_d, W, S)
    stage_rwkv2(p, "r2", W, S)
    with ExitStack() as ea:
        p.es = ea
        acc0 = p.sb("acc_r", [128, NT, D], F32)
        stage_rwkv3(p, "r3", x_d, W, S, lng[0, 0], lnb[0, 0], h1, acc=acc0)
        stage_moe(p, "m0", h1, h2, rw, rb, wg[0], wu[0], wd[0], lng[0, 1], lnb[0, 1], acc=acc0)
    with ExitStack() as eb:
        p.es = eb
        acc1 = p.sb("acc_f", [128, NT, D], F32)
        stage_fox(p, "fx", h2, h3, win, bf, fwo, lng[1, 0], lnb[1, 0], acc=acc1)
        stage_moe(p, "m1", h3, out, rw, rb, wg[1], wu[1], wd[1], lng[1, 1], lnb[1, 1], acc=acc1)
    return nc


def kernel(**inputs):
    x = np.asarray(inputs["x"], dtype=np.float32)
    B = x.shape[0]
    shared = {}
    for n_, _ in RW_SHAPES:
        shared["rw_" + n_] = np.ascontiguousarray(np.asarray(inputs["rw_" + n_], dtype=np.float32)[0])
    shared["fx_w_in"] = np.ascontiguousarray(np.asarray(inputs["fx_w_in"], dtype=np.float32)[0])
    shared["fx_b_f"] = np.ascontiguousarray(np.asarray(inputs["fx_b_f"], dtype=np.float32)[0])
    shared["fx_wo"] = np.ascontiguousarray(np.asarray(inputs["fx_wo"], dtype=np.float32)[0])
    for n_ in ("router_w", "router_bias", "moe_w_gate", "moe_w_up", "moe_w_down", "ln_g", "ln_b"):
        shared[n_] = np.ascontiguousarray(np.asarray(inputs[n_], dtype=np.float32))
    nc = build_full()
    in_maps = []
    for b in range(B):
        m = dict(shared)
        m["x"] = np.ascontiguousarray(x[b])
        m["xT"] = np.ascontiguousarray(x[b].T)
        in_maps.append(m)
    res = run_bass_kernel_spmd(nc, in_maps, core_ids=list(range(B)))
    return np.stack([np.asarray(r["out"], dtype=np.float32) for r in res.results], axis=0)
```
